# Optimizing a Trainium2 kernel written in Bass

```python
import math
import jax, jax.numpy as jnp
from jax import lax
import numpy as np

D_MODEL = 1024
BATCH = 8
SEQ = 4096
DEPTH = 4

GRID_W = 64
CTX_LEN = 256
BRANCH_W = 512
N_BRANCH = 3
ML_HEADS = 4
ML_DH = 128
ML_W = ML_HEADS * ML_DH
ML_CHUNK = 64
ML_CONV = 3
DF_HEADS = 4
DF_DQK = 64
DF_DV = 128
DF_W = DF_HEADS * DF_DV
DF_QBLOCK = 128
ROPE_BASE = 10000.0
NA_HEADS = 8
NA_DH = 64
NA_W = NA_HEADS * NA_DH
NA_KH = 8
NA_KW = 16
NA_KBW = 2 * NA_KW
D_FF = 2816
EPS = 1e-6
IN_SIZES = (ML_W, ML_W, ML_W, ML_W, 4 * ML_HEADS,
            2 * DF_HEADS * DF_DQK, 2 * DF_HEADS * DF_DQK, DF_W,
            NA_W, NA_W, NA_W,
            N_BRANCH * D_MODEL)
D_IN = sum(IN_SIZES)

kernel_name = "hybrid_mlstm_diffattn_natten_macaron_dit"


def rmsnorm(x, g):
    xf = x.astype(jnp.float32)
    y = xf * lax.rsqrt(jnp.mean(xf * xf, axis=-1, keepdims=True) + EPS)
    return (y * g.astype(jnp.float32)).astype(x.dtype)


def modulate(h, shift, scale):
    return h * (1 + scale) + shift


def swiglu(h, w1, w2):
    a, b = jnp.split(h @ w1, 2, axis=-1)
    return (jax.nn.silu(a) * b) @ w2


def ffn_half(x, mod, g, w1, w2):
    shift, scale, gate = mod
    return x + 0.5 * gate * swiglu(modulate(rmsnorm(x, g), shift, scale), w1, w2)


def head_rmsnorm(h, g):
    B, T, H, dh = h.shape
    return rmsnorm(h, g.reshape(H, dh)).reshape(B, T, H * dh)


def dwconv_centred(x, w, b):
    C = x.shape[-1]
    y = lax.conv_general_dilated(x, w[:, None, :].astype(x.dtype), window_strides=(1,),
                                 padding='SAME', dimension_numbers=('NWC', 'WIO', 'NWC'),
                                 feature_group_count=C)
    return y + b


def rope_2d(S):
    t = jnp.arange(S)
    row = (t // GRID_W).astype(jnp.float32)
    col = (t % GRID_W).astype(jnp.float32)
    half = DF_DQK // 2
    freqs = ROPE_BASE ** (-jnp.arange(0, half, 2, dtype=jnp.float32) / half)
    ang = jnp.concatenate([row[:, None] * freqs, col[:, None] * freqs], axis=-1)
    return jnp.cos(ang), jnp.sin(ang)


def apply_rope(x, cos, sin):
    half = DF_DQK // 2
    x1, x2 = x[..., :half], x[..., half:]
    return jnp.concatenate([x1 * cos - x2 * sin, x1 * sin + x2 * cos], axis=-1).astype(x.dtype)


def mlstm_chunk_scan(q, k, v, log_i, log_f, state):
    B, H, T, dh = q.shape
    L = ML_CHUNK
    nc = T // L

    def to_chunks(a):
        return jnp.moveaxis(a.reshape(B, H, nc, L, *a.shape[3:]), 2, 0)

    xs = tuple(to_chunks(a) for a in (q, k, v, log_i, log_f))
    tri = jnp.tril(jnp.ones((L, L), dtype=bool))

    def step(carry, inp):
        C, n, m = carry
        qc, kc, vc, li, lf = inp
        b = jnp.cumsum(lf, axis=-1)
        dmat = jnp.where(tri, b[..., :, None] - b[..., None, :] + li[..., None, :], -jnp.inf)
        inter = b + m[..., None]
        m_t = jnp.maximum(inter, jnp.max(dmat, axis=-1))
        w = jnp.exp(dmat - m_t[..., None])
        s_inter = jnp.exp(inter - m_t)
        qk = jnp.einsum('bhtd,bhsd->bhts', qc, kc) * w
        num = (jnp.einsum('bhts,bhse->bhte', qk, vc)
               + s_inter[..., None] * jnp.einsum('bhed,bhtd->bhte', C, qc))
        den = jnp.sum(qk, axis=-1) + s_inter * jnp.einsum('bhd,bhtd->bht', n, qc)
        h = num / jnp.maximum(jnp.abs(den), jnp.exp(-m_t))[..., None]
        b_last = b[..., -1]
        wk = b_last[..., None] - b + li
        m_new = jnp.maximum(b_last + m, jnp.max(wk, axis=-1))
        decay = jnp.exp(b_last + m - m_new)
        wk = jnp.exp(wk - m_new[..., None])
        C_new = decay[..., None, None] * C + jnp.einsum('bhs,bhse,bhsd->bhed', wk, vc, kc)
        n_new = decay[..., None] * n + jnp.einsum('bhs,bhsd->bhd', wk, kc)
        return (C_new, n_new, m_new), h

    state, hs = lax.scan(step, state, xs)
    return jnp.moveaxis(hs, 0, 2).reshape(B, H, T, dh), state


def mlstm_inputs(q, k, v, o, g, conv_w, conv_b, gate_b):
    qk = jax.nn.silu(dwconv_centred(jnp.concatenate([q, k], axis=-1), conv_w, conv_b))
    q, k = jnp.split(qk, 2, axis=-1)
    B, T, _ = q.shape

    def heads(a):
        return a.reshape(B, T, ML_HEADS, ML_DH).transpose(0, 2, 1, 3).astype(jnp.float32)

    gates = (g + gate_b).astype(jnp.float32).reshape(B, T, 2, 2, ML_HEADS).transpose(2, 3, 0, 4, 1)
    log_i = gates[:, 0]
    log_f = jax.nn.log_sigmoid(gates[:, 1])
    return heads(q) * ML_DH ** -0.5, heads(k), heads(v), jax.nn.sigmoid(o), log_i, log_f


def mlstm_mixer(lat, ctx, conv_w, conv_b, gate_b, norm_g, ctx_out):
    ql, kl, vl, ol, il, fl = mlstm_inputs(*lat, conv_w, conv_b, gate_b)
    qc, kc, vc, oc, ic, fc = mlstm_inputs(*ctx, conv_w, conv_b, gate_b)
    B = ql.shape[0]
    zero = (jnp.zeros((B, ML_HEADS, ML_DH, ML_DH), jnp.float32),
            jnp.zeros((B, ML_HEADS, ML_DH), jnp.float32),
            jnp.zeros((B, ML_HEADS), jnp.float32))

    def flip(a):
        return jnp.flip(a, axis=2)

    hcf, st_f = mlstm_chunk_scan(qc, kc, vc, ic[0], fc[0], zero)
    hlf, _ = mlstm_chunk_scan(ql, kl, vl, il[0], fl[0], st_f)
    hcb, st_b = mlstm_chunk_scan(flip(qc), flip(kc), flip(vc), flip(ic[1]), flip(fc[1]), zero)
    hlb, _ = mlstm_chunk_scan(flip(ql), flip(kl), flip(vl), flip(il[1]), flip(fl[1]), st_b)

    def finish(h, o):
        return (o * head_rmsnorm(h.transpose(0, 2, 1, 3).astype(o.dtype), norm_g)).astype(o.dtype)

    y_lat = finish(hlf + flip(hlb), ol)
    y_ctx = finish(hcf + flip(hcb), oc) if ctx_out else None
    return y_lat, y_ctx


def diff_mixer(q_lat, k_lat, v_lat, q_ctx, k_ctx, v_ctx, lam_vecs, norm_g, lam_init, ctx_out):
    B, S, _ = q_lat.shape
    scale = DF_DQK ** -0.5

    def qk_heads(a):
        T = a.shape[1]
        a = a.reshape(B, T, DF_HEADS, 2, DF_DQK).transpose(3, 0, 2, 1, 4)
        return a[0], a[1]

    def v_heads(a):
        return a.reshape(B, a.shape[1], DF_HEADS, DF_DV).transpose(0, 2, 1, 3)

    cos, sin = rope_2d(S)
    q1l, q2l = [apply_rope(t, cos, sin) for t in qk_heads(q_lat)]
    k1l, k2l = [apply_rope(t, cos, sin) for t in qk_heads(k_lat)]
    q1c, q2c = qk_heads(q_ctx)
    k1c, k2c = qk_heads(k_ctx)
    vl, vc = v_heads(v_lat), v_heads(v_ctx)
    lv = lam_vecs.astype(jnp.float32)
    lam = jnp.exp(jnp.sum(lv[0] * lv[1])) - jnp.exp(jnp.sum(lv[2] * lv[3])) + lam_init

    def attend(q1, q2, k1, k2, v):
        s1 = (jnp.einsum('bhqd,bhkd->bhqk', q1, k1) * scale).astype(jnp.float32)
        s2 = (jnp.einsum('bhqd,bhkd->bhqk', q2, k2) * scale).astype(jnp.float32)
        p = jax.nn.softmax(s1, axis=-1) - lam * jax.nn.softmax(s2, axis=-1)
        return jnp.einsum('bhqk,bhkd->bhqd', p.astype(v.dtype), v)

    k1a = jnp.concatenate([k1l, k1c], axis=2)
    k2a = jnp.concatenate([k2l, k2c], axis=2)
    va = jnp.concatenate([vl, vc], axis=2)
    nqb = S // DF_QBLOCK

    def blocks(a):
        return jnp.moveaxis(a.reshape(B, DF_HEADS, nqb, DF_QBLOCK, DF_DQK), 2, 0)

    o = lax.map(lambda qq: attend(qq[0], qq[1], k1a, k2a, va), (blocks(q1l), blocks(q2l)))
    o = jnp.moveaxis(o, 0, 2).reshape(B, DF_HEADS, S, DF_DV)

    def finish(h):
        return head_rmsnorm(h.transpose(0, 2, 1, 3), norm_g) * (1.0 - lam_init)

    y_lat = finish(o)
    y_ctx = finish(attend(q1c, q2c, k1c, k2c, vc)) if ctx_out else None
    return y_lat, y_ctx


def na_mixer(q_lat, k_lat, v_lat, q_ctx, k_ctx, v_ctx, rel_bias, ctx_out):
    B, S, _ = q_lat.shape
    rows = S // GRID_W
    kh = min(NA_KH, rows)
    nb = GRID_W // NA_KW
    scale = NA_DH ** -0.5

    def grid(a):
        return a.reshape(B, rows, GRID_W, NA_HEADS, NA_DH)

    qg = jnp.moveaxis(grid(q_lat), 1, 0)
    kg, vg = grid(k_lat), grid(v_lat)
    kc = k_ctx.reshape(B, -1, NA_HEADS, NA_DH)
    vc = v_ctx.reshape(B, -1, NA_HEADS, NA_DH)
    qcol = np.arange(GRID_W).reshape(nb, NA_KW)
    qstart = np.clip(qcol - NA_KW // 2, 0, GRID_W - NA_KW)
    kstart = np.clip(np.arange(nb) * NA_KW - NA_KW // 2, 0, GRID_W - NA_KBW)
    kcol = kstart[:, None] + np.arange(NA_KBW)
    col_ok = ((kcol[:, None, :] >= qstart[:, :, None])
              & (kcol[:, None, :] < qstart[:, :, None] + NA_KW))
    dc_idx = np.clip(kcol[:, None, :] - qcol[:, :, None] + NA_KW - 1, 0, 2 * NA_KW - 2)
    bias_cols = rel_bias[:, :, dc_idx].astype(jnp.float32)
    n_lat = kh * NA_KBW

    def row_fn(args):
        r, q_row = args
        rs = jnp.clip(r - kh // 2, 0, rows - kh)
        kb = lax.dynamic_slice_in_dim(kg, rs, kh, axis=1)[:, :, kcol]
        vb = lax.dynamic_slice_in_dim(vg, rs, kh, axis=1)[:, :, kcol]
        qb = q_row.reshape(B, nb, NA_KW, NA_HEADS, NA_DH)
        dr_idx = rs + jnp.arange(kh) - r + NA_KH - 1
        bias = jnp.take(bias_cols, dr_idx, axis=1).transpose(0, 2, 3, 1, 4)
        s_lat = (jnp.einsum('bnqhd,bknwhd->bhnqkw', qb, kb) * scale).astype(jnp.float32) + bias
        s_lat = jnp.where(col_ok[:, :, None, :], s_lat, -jnp.inf)
        s_ctx = (jnp.einsum('bnqhd,bchd->bhnqc', qb, kc) * scale).astype(jnp.float32)
        s = jnp.concatenate([s_lat.reshape(B, NA_HEADS, nb, NA_KW, n_lat), s_ctx], axis=-1)
        p = jax.nn.softmax(s, axis=-1).astype(vb.dtype)
        p_lat = p[..., :n_lat].reshape(B, NA_HEADS, nb, NA_KW, kh, NA_KBW)
        o = (jnp.einsum('bhnqkw,bknwhd->bnqhd', p_lat, vb)
             + jnp.einsum('bhnqc,bchd->bnqhd', p[..., n_lat:], vc))
        return o.reshape(B, GRID_W, NA_W)

    out = lax.map(row_fn, (jnp.arange(rows), qg))
    y_lat = jnp.moveaxis(out, 0, 1).reshape(B, S, NA_W)
    y_ctx = None
    if ctx_out:
        qc = q_ctx.reshape(B, -1, NA_HEADS, NA_DH)
        s = (jnp.einsum('bqhd,bkhd->bhqk', qc, kc) * scale).astype(jnp.float32)
        p = jax.nn.softmax(s, axis=-1).astype(vc.dtype)
        y_ctx = jnp.einsum('bhqk,bkhd->bqhd', p, vc).reshape(B, -1, NA_W)
    return y_lat, y_ctx


def merge_branches(gate_pre, outs, w_branch, w_out):
    gates = jnp.split(jax.nn.sigmoid(gate_pre.astype(jnp.float32)).astype(gate_pre.dtype), N_BRANCH, axis=-1)
    y = sum(g * (o @ w_branch[i]) for i, (g, o) in enumerate(zip(gates, outs)))
    return y @ w_out


def token_mixers(h_lat, h_ctx, ml_conv_w, ml_conv_b, ml_gate_b, ml_norm_g, df_lambda, df_norm_g,
                 na_rel_bias, w_branch, w_out, lam_init, ctx_out):
    offs = np.cumsum(IN_SIZES)[:-1].tolist()
    (ml_q, ml_k, ml_v, ml_o, ml_g, df_q, df_k, df_v, na_q, na_k, na_v, gate_l) = jnp.split(h_lat, offs, axis=-1)
    (mc_q, mc_k, mc_v, mc_o, mc_g, dc_q, dc_k, dc_v, nc_q, nc_k, nc_v, gate_c) = jnp.split(h_ctx, offs, axis=-1)
    a_lat, a_ctx = mlstm_mixer((ml_q, ml_k, ml_v, ml_o, ml_g), (mc_q, mc_k, mc_v, mc_o, mc_g),
                               ml_conv_w, ml_conv_b, ml_gate_b, ml_norm_g, ctx_out)
    b_lat, b_ctx = diff_mixer(df_q, df_k, df_v, dc_q, dc_k, dc_v, df_lambda, df_norm_g, lam_init, ctx_out)
    c_lat, c_ctx = na_mixer(na_q, na_k, na_v, nc_q, nc_k, nc_v, na_rel_bias, ctx_out)
    y_lat = merge_branches(gate_l, (a_lat, b_lat, c_lat), w_branch, w_out)
    y_ctx = merge_branches(gate_c, (a_ctx, b_ctx, c_ctx), w_branch, w_out) if ctx_out else None
    return y_lat, y_ctx


def setup_inputs(seed: int = 0) -> dict:
    key = jax.random.key(seed)
    ks = jax.random.split(key, 24)
    D = D_MODEL

    def nrm(k, shape, s):
        return jax.random.normal(k, shape, jnp.float32) * s

    ib = nrm(ks[12], (DEPTH, 2, 1, ML_HEADS), 0.1)
    fb = 3.0 + 3.0 * jax.random.uniform(ks[13], (DEPTH, 2, 1, ML_HEADS), jnp.float32)
    return {
        'x': nrm(ks[0], (BATCH, SEQ, D), 1.0),
        'c': nrm(ks[1], (BATCH, D), 1.0),
        'ctx': nrm(ks[2], (BATCH, CTX_LEN, D), 1.0),
        'c_ctx': nrm(ks[3], (D,), 1.0),
        'w_ada': nrm(ks[4], (DEPTH, D, 9 * D), D ** -0.5),
        'b_ada': nrm(ks[5], (DEPTH, 9 * D), 0.02),
        'norm_g': 1.0 + nrm(ks[6], (DEPTH, 3, D), 0.05),
        'ffn_w1': nrm(ks[7], (DEPTH, 2, D, 2 * D_FF), D ** -0.5),
        'ffn_w2': nrm(ks[8], (DEPTH, 2, D_FF, D), D_FF ** -0.5),
        'w_in': nrm(ks[9], (DEPTH, D, D_IN), D ** -0.5),
        'ml_conv_w': nrm(ks[10], (DEPTH, ML_CONV, 2 * ML_W), ML_CONV ** -0.5),
        'ml_conv_b': nrm(ks[11], (DEPTH, 2 * ML_W), 0.02),
        'ml_gate_b': jnp.concatenate([ib, fb], axis=2).reshape(DEPTH, 4 * ML_HEADS),
        'ml_norm_g': 1.0 + nrm(ks[14], (DEPTH, ML_W), 0.05),
        'df_lambda': nrm(ks[15], (DEPTH, 4, DF_DQK), 0.1),
        'df_norm_g': 1.0 + nrm(ks[16], (DEPTH, DF_W), 0.05),
        'na_rel_bias': nrm(ks[17], (DEPTH, NA_HEADS, 2 * NA_KH - 1, 2 * NA_KW - 1), 0.1),
        'w_branch': nrm(ks[18], (DEPTH, N_BRANCH, BRANCH_W, D), BRANCH_W ** -0.5),
        'w_out': nrm(ks[19], (DEPTH, D, D), D ** -0.5),
        'final_g': 1.0 + nrm(ks[20], (D,), 0.05),
    }


def reference(x, c, ctx, c_ctx, w_ada, b_ada, norm_g, ffn_w1, ffn_w2, w_in, ml_conv_w, ml_conv_b,
              ml_gate_b, ml_norm_g, df_lambda, df_norm_g, na_rel_bias, w_branch, w_out, final_g):
    for l in range(DEPTH):
        ctx_out = l < DEPTH - 1
        lam_init = 0.8 - 0.6 * math.exp(-0.3 * l)
        m_lat = jnp.split((jax.nn.silu(c) @ w_ada[l] + b_ada[l])[:, None, :], 9, axis=-1)
        m_ctx = jnp.split((jax.nn.silu(c_ctx) @ w_ada[l] + b_ada[l])[None, None, :], 9, axis=-1)
        x = ffn_half(x, m_lat[0:3], norm_g[l, 0], ffn_w1[l, 0], ffn_w2[l, 0])
        ctx = ffn_half(ctx, m_ctx[0:3], norm_g[l, 0], ffn_w1[l, 0], ffn_w2[l, 0])
        h_lat = modulate(rmsnorm(x, norm_g[l, 1]), m_lat[3], m_lat[4]) @ w_in[l]
        h_ctx = modulate(rmsnorm(ctx, norm_g[l, 1]), m_ctx[3], m_ctx[4]) @ w_in[l]
        y_lat, y_ctx = token_mixers(h_lat, h_ctx, ml_conv_w[l], ml_conv_b[l], ml_gate_b[l], ml_norm_g[l],
                                    df_lambda[l], df_norm_g[l], na_rel_bias[l], w_branch[l], w_out[l],
                                    lam_init, ctx_out)
        x = x + m_lat[5] * y_lat
        x = ffn_half(x, m_lat[6:9], norm_g[l, 2], ffn_w1[l, 1], ffn_w2[l, 1])
        if ctx_out:
            ctx = ctx + m_ctx[5] * y_ctx
            ctx = ffn_half(ctx, m_ctx[6:9], norm_g[l, 2], ffn_w1[l, 1], ffn_w2[l, 1])
    return rmsnorm(x, final_g)
```

```python
import math
from contextlib import ExitStack
import numpy as np
import concourse.bass as bass
import concourse.mybir as mybir
from concourse.bass_utils import run_bass_kernel_spmd

F32 = mybir.dt.float32
BF16 = mybir.dt.bfloat16
AF = mybir.ActivationFunctionType
ALU = mybir.AluOpType
KQ = 8
EPS = 1e-6
NEG = -30000.0


class Cfg:
    def __init__(s, rows=64, ctx=256, depth=4, dff=2816, ncores=8):
        s.ROWS = rows; s.GW = 64; s.S = rows * 64; s.CTX = ctx; s.T = s.S + ctx
        s.DEPTH = depth; s.DFF = dff; s.NJ = dff // 128; s.D = 1024; s.NB = s.T // 128
        s.NCB = ctx // 128; s.G = rows // 8; s.DIN = 5136 + 3072; s.NC = ncores
        s.tiles = [(0, ctx, True)] + [(ctx + i * 512, 512, False) for i in range(s.S // 512)]


class Buf:
    __slots__ = ("w", "r", "wf")

    def __init__(s):
        s.w = {}; s.r = {}; s.wf = {}


def _merge(d, e):
    for k, v in e.items():
        if d.get(k, 0) < v:
            d[k] = v


class Mach:
    def __init__(s, nc):
        s.nc = nc; s.es = ExitStack(); s.sems = []; s.bufs = {}; s.eng = {}
        for nm, h in (("pe", nc.tensor), ("act", nc.scalar), ("dve", nc.vector), ("pool", nc.gpsimd), ("sp", nc.sync)):
            s.eng[nm] = {"h": h, "si": s.newsem("e_" + nm), "cnt": 0, "seen": {}}
        s.dq = {q: {"sis": [s.newsem("d_%s%d" % (q, i)) for i in range(KQ)], "n": 0} for q in ("sp", "pool")}
        s.ninst = 0

    def newsem(s, name):
        s.sems.append(s.es.enter_context(s.nc.semaphore(name)))
        return len(s.sems) - 1

    def buf(s, ap):
        k = ap.tensor.name
        b = s.bufs.get(k)
        if b is None:
            b = s.bufs[k] = Buf()
        return b

    def wait(s, en, evs):
        E = s.eng[en]
        for si, v in evs.items():
            if en == "pe" and si == E["si"]:
                continue
            if E["seen"].get(si, 0) < v:
                E["h"].wait_ge(s.sems[si], v); E["seen"][si] = v; s.ninst += 1

    def _deps(s, outs, ins, part):
        evs = {}
        for ap in ins:
            _merge(evs, s.buf(ap).w)
        for ap in outs:
            b = s.buf(ap); _merge(evs, b.r)
            _merge(evs, b.wf if part else b.w)
        return evs

    def _rec(s, outs, ins, ev, part=False):
        for ap in ins:
            _merge(s.buf(ap).r, ev)
        for ap in outs:
            _merge(s.buf(ap).w, ev)
            if not part:
                _merge(s.buf(ap).wf, ev)

    def op(s, en, name, outs, ins, *a, part=False, **kw):
        E = s.eng[en]
        s.wait(en, s._deps(outs, ins, part))
        inst = getattr(E["h"], name)(*a, **kw)
        E["cnt"] += 1; s.ninst += 1
        inst.then_inc(s.sems[E["si"]], 1)
        s._rec(outs, ins, {E["si"]: E["cnt"]}, part)

    def dma(s, q, out, in_, part=False, **kw):
        Q = s.dq[q]; slot = Q["n"] % KQ; gen = Q["n"] // KQ; si = Q["sis"][slot]
        evs = s._deps([out], [in_], part)
        if gen > 0:
            _merge(evs, {si: 16 * gen})
        s.wait(q, evs)
        s.eng[q]["h"].dma_start(out=out, in_=in_, **kw).then_inc(s.sems[si], 16)
        Q["n"] += 1; s.ninst += 1
        s._rec([out], [in_], {si: 16 * (gen + 1)}, part)

    def barrier(s):
        evs = {E["si"]: E["cnt"] for E in s.eng.values() if E["cnt"] > 0}
        for Q in s.dq.values():
            for i, si in enumerate(Q["sis"]):
                n = (Q["n"] - i + KQ - 1) // KQ
                if n > 0:
                    evs[si] = 16 * n
        for en in s.eng:
            s.wait(en, evs)

    def mm(s, out, lhsT, rhs, start=True, stop=True):
        s.op("pe", "matmul", [out], [lhsT, rhs], out, lhsT=lhsT, rhs=rhs, start=start, stop=stop)

    def tr(s, out, in_, ident):
        s.op("pe", "transpose", [out], [in_, ident], out, in_, ident)

    def act(s, out, in_, func, bias=None, scale=None, accum_out=None, part=False):
        kw = {}; ins = [in_]; outs = [out]
        if bias is not None:
            kw["bias"] = bias
            if not isinstance(bias, float):
                ins.append(bias)
        if scale is not None:
            kw["scale"] = scale
            if not isinstance(scale, float):
                ins.append(scale)
        if accum_out is not None:
            kw["accum_out"] = accum_out; outs.append(accum_out)
        s.op("act", "activation", outs, ins, out=out, in_=in_, func=func, part=part, **kw)

    def tt(s, en, out, in0, in1, op, part=False):
        s.op(en, "tensor_tensor", [out], [in0, in1], out=out, in0=in0, in1=in1, op=op, part=part)

    def ts(s, en, out, in0, s1, op0, s2=None, op1=None, part=False):
        ins = [in0] + [x for x in (s1, s2) if x is not None and not isinstance(x, float)]
        kw = {"op1": op1} if op1 is not None else {}
        s.op(en, "tensor_scalar", [out], ins, out=out, in0=in0, scalar1=s1, scalar2=s2, op0=op0, part=part, **kw)

    def stt(s, out, in0, scalar, in1, op0, op1, part=False):
        ins = [in0, in1] + ([] if isinstance(scalar, float) else [scalar])
        s.op("dve", "scalar_tensor_tensor", [out], ins, out=out, in0=in0, scalar=scalar, in1=in1, op0=op0, op1=op1, part=part)

    def copy(s, en, out, in_, part=False):
        if en == "act":
            s.op("act", "activation", [out], [in_], out=out, in_=in_, func=AF.Copy, part=part)
        else:
            s.op(en, "tensor_copy", [out], [in_], out=out, in_=in_, part=part)

    def memset(s, en, ap, val, part=False):
        s.op(en, "memset", [ap], [], ap, val, part=part)

    def recip(s, out, in_, part=False):
        s.op("dve", "reciprocal", [out], [in_], out=out, in_=in_, part=part)


def pipeline(n, stages, look):
    for j in range(min(look, n)):
        stages[0](j)
    for j in range(n):
        if j + look < n:
            stages[0](j + look)
        for f in stages[1:]:
            f(j)


def build(cfg):
    nc = bass.Bass("TRN2", target_bir_lowering=False)
    T, S, CTX, D, DFF, NJ, NB, DEPTH = cfg.T, cfg.S, cfg.CTX, cfg.D, cfg.DFF, cfg.NJ, cfg.NB, cfg.DEPTH

    def din(name, shape, dt=F32):
        return nc.dram_tensor(name, list(shape), dt, kind="ExternalInput").ap()

    def dscr(name, shape, dt):
        return nc.dram_tensor(name, list(shape), dt, kind="Internal").ap()

    x_in = din("x", [S, D]); ctx_in = din("ctx", [CTX, D]); cv_in = din("cv", [128, 24])
    w_ada = din("w_ada", [DEPTH, D, 9 * D]); pv_in = din("pv", [DEPTH, 128, 136]); gb_in = din("gb", [DEPTH, 128, 16])
    ffn_w1 = din("ffn_w1", [DEPTH, 2, D, 2 * DFF]); ffn_w2 = din("ffn_w2", [DEPTH, 2, DFF, D])
    w_in = din("w_in", [DEPTH, D, cfg.DIN]); dfl_in = din("dfl", [DEPTH, 1, 256]); rbT_in = din("rbT", [DEPTH, 32, 120])
    w_br = din("w_branch", [DEPTH, 3, 512, D]); w_out = din("w_out", [DEPTH, D, D])
    cmat_in = din("cmat", [128, 3, 128]); rope_in = din("rope", [2, 128, S]); ind_in = din("ind", [32, 4096])
    y_out = nc.dram_tensor("y", [S, D], F32, kind="ExternalOutput").ap()

    xTd = dscr("xTd", [D, T], F32).rearrange("(k p) t -> p k t", p=128)
    fm = {n: dscr("fm_" + n, [r, T], BF16) for n, r in (("mlq", 512), ("mlk", 512), ("mlo", 512), ("dfq", 512), ("dfk", 512),
                                                        ("naq", 512), ("nak", 512), ("gate", 3072), ("mly", 512), ("dfy", 512), ("nay", 512))}
    tm = {n: dscr("tm_" + n, [T, c], dt) for n, c, dt in (("mlv", 512, BF16), ("dfv", 512, BF16), ("nav", 512, BF16), ("mlg", 16, F32))}
    trev = dscr("trev", [120, 4096], BF16)

    M = Mach(nc)
    gs_ = ExitStack()

    _nm = [0]

    def sb(es, name, shape, dt):
        _nm[0] += 1
        return es.enter_context(nc.sbuf_tensor("%s_%d" % (name, _nm[0]), list(shape), dt))

    PS = [gs_.enter_context(nc.psum_tensor("ps%d" % i, [128, 512], F32)) for i in range(8)]
    cm = sb(gs_, "cmat", [128, 3, 128], F32)
    ones_f = sb(gs_, "ones_f", [128, 128], F32); ones_b = sb(gs_, "ones_b", [128, 128], BF16)
    epsc = sb(gs_, "epsc", [128, 2], F32)
    cvs = sb(gs_, "cvs", [128, 24], F32)
    prm = sb(gs_, "prm", [128, DEPTH * 2 * 9 * 8], F32)
    pvs = sb(gs_, "pvs", [128, DEPTH, 136], F32)
    ident = cm[:, 0, :]; triF = cm[:, 1, :]; triB = cm[:, 2, :]
    eps_ap = epsc[:, 0:1]; one_ap = epsc[:, 1:2]

    def P(l, ci, j):
        o = ((l * 2 + ci) * 9 + j) * 8
        return prm[:, o:o + 8]

    M.dma("sp", cm[:, :, :], cmat_in[:, :, :]); M.dma("sp", cvs[:, :], cv_in[:, :])
    for l in range(DEPTH):
        M.dma("sp", pvs[:, l, :], pv_in[l, :, :], part=True)
    M.memset("dve", ones_f[:, :], 1.0); M.memset("dve", ones_b[:, :], 1.0)
    M.memset("dve", epsc[:, 0:1], EPS); M.memset("dve", epsc[:, 1:2], 1.0, part=True)
    with ExitStack() as es:
        scT = sb(es, "scT", [128, 8, 2], F32)
        wa = [sb(es, "wa%d" % i, [128, 8, 1024], F32) for i in range(2)]
        mods = sb(es, "mods", [128, 72, 2], F32)
        for i in range(2):
            M.act(scT[:, :, i], cvs[:, i * 8:(i + 1) * 8], AF.Silu, part=True)
        it = 0
        for l in range(DEPTH):
            wv = w_ada[l].rearrange("(k p) n -> p k n", p=128)
            pm = PS[l % 2]
            for grp in range(9):
                w = wa[it % 2]; it += 1
                for k in range(8):
                    M.dma("sp", w[:, k, :], wv[:, k, grp * 1024:(grp + 1) * 1024], part=True)
                for fc in range(8):
                    c0 = (grp * 8 + fc) * 2
                    for k in range(8):
                        M.mm(pm[:, c0:c0 + 2], w[:, k, fc * 128:(fc + 1) * 128], scT[:, k, :], start=(k == 0), stop=(k == 7))
            pmv = pm[:, 0:144].rearrange("p (j i) -> p j i", i=2)
            for i in range(2):
                M.tt("dve", mods[:, :, i], pmv[:, :, i], pvs[:, l, 24:96], ALU.add, part=True)
            for ci in range(2):
                for sub in range(3):
                    mo = sub * 24
                    ng = pvs[:, l, sub * 8:(sub + 1) * 8]
                    M.stt(P(l, ci, sub * 3 + 0), mods[:, mo + 8:mo + 16, ci], 1.0, ng, ALU.add, ALU.mult, part=True)
                    M.copy("dve", P(l, ci, sub * 3 + 1), mods[:, mo:mo + 8, ci], part=True)
                    M.ts("dve", P(l, ci, sub * 3 + 2), mods[:, mo + 16:mo + 24, ci], 0.5 if sub != 1 else 1.0, ALU.mult, part=True)
    M.barrier()

    with ExitStack() as es:
        xin = [sb(es, "xin%d" % i, [128, D], F32) for i in range(2)]
        xtb = [sb(es, "xtb%d" % i, [128, 8, 128], F32) for i in range(2)]
        for b in range(NB):
            src = ctx_in[b * 128:(b + 1) * 128, :] if b < cfg.NCB else x_in[(b - cfg.NCB) * 128:(b - cfg.NCB + 1) * 128, :]
            xi = xin[b % 2]; xo = xtb[b % 2]
            M.dma("sp", xi[:, :], src)
            for k in range(8):
                pb = PS[(b % 2) * 2 + k // 4]
                M.tr(pb[:, (k % 4) * 128:(k % 4 + 1) * 128], xi[:, k * 128:(k + 1) * 128], ident)
            M.copy("dve", xo[:, 0:4, :], PS[(b % 2) * 2][:, :].rearrange("p (k t) -> p k t", k=4), part=True)
            M.copy("act", xo[:, 4:8, :], PS[(b % 2) * 2 + 1][:, :].rearrange("p (k t) -> p k t", k=4), part=True)
            M.dma("sp", xTd[:, :, b * 128:(b + 1) * 128], xo[:, :, :])
    M.barrier()

    def rms_mod(xT, hT, N, gsv, shv, sqb, rstd, tmpb):
        pss = PS[7]
        if not callable(xT):
            xt_ = xT
            xT = lambda k: xt_[:, k, :N]
        for k in range(8):
            s_ = sqb[k % 2]
            M.act(s_[:, :N], xT(k), AF.Square)
            M.mm(pss[:, :N], ones_f[:, :], s_[:, :N], start=(k == 0), stop=(k == 7))
        M.act(rstd[:, :N], pss[:, :N], AF.Sqrt, bias=eps_ap, scale=1.0 / D)
        M.recip(rstd[:, :N], rstd[:, :N])
        for k in range(8):
            t_ = tmpb[k % 2]
            M.stt(t_[:, :N], xT(k), gsv[:, k:k + 1], rstd[:, :N], ALU.mult, ALU.mult)
            M.act(hT[:, k, :N], t_[:, :N], AF.Identity, bias=shv[:, k:k + 1], part=True)

    def ffn_stage(l, j, tiles):
        sub = 0 if j == 0 else 2
        with ExitStack() as es:
            W1 = sb(es, "W1", [128, 8, 2 * DFF], BF16); W2 = sb(es, "W2", [128, NJ, D], BF16)
            xTk = [sb(es, "xTk%d" % k, [128, 512], F32) for k in range(8)]
            hT = sb(es, "hT", [128, 8, 512], BF16); uT = sb(es, "uT", [128, NJ, 512], BF16)
            sqb = [sb(es, "sq%d" % i, [128, 512], F32) for i in range(2)]
            rstd = sb(es, "rstd", [128, 512], F32)
            w1v = ffn_w1[l, j].rearrange("(k p) n -> p k n", p=128); w2v = ffn_w2[l, j].rearrange("(k p) n -> p k n", p=128)
            cw = 2 * DFF // 4
            for k in range(8):
                for c in range(4):
                    M.dma("pool", W1[:, k, c * cw:(c + 1) * cw], w1v[:, k, c * cw:(c + 1) * cw], part=True)
            for k in range(NJ):
                M.dma("pool", W2[:, k, :], w2v[:, k, :], part=True)
            for k in range(8):
                M.dma("sp", xTk[k][:, 0:tiles[0][1]], xTd[:, k, tiles[0][0]:tiles[0][0] + tiles[0][1]])
            for ix, (t0, N, isctx) in enumerate(tiles):
                ci = 1 if isctx else 0
                rms_mod(lambda k: xTk[k][:, :N], hT, N, P(l, ci, sub * 3), P(l, ci, sub * 3 + 1), sqb, rstd, sqb)
                for jj in range(NJ):
                    pa = PS[(jj % 2) * 2]; pb = PS[(jj % 2) * 2 + 1]; sl = sqb[jj % 2]
                    for k in range(8):
                        M.mm(pa[:, :N], W1[:, k, jj * 128:(jj + 1) * 128], hT[:, k, :N], start=(k == 0), stop=(k == 7))
                    for k in range(8):
                        M.mm(pb[:, :N], W1[:, k, DFF + jj * 128:DFF + (jj + 1) * 128], hT[:, k, :N], start=(k == 0), stop=(k == 7))
                    M.act(sl[:, :N], pa[:, :N], AF.Silu)
                    M.tt("dve", uT[:, jj, :N], sl[:, :N], pb[:, :N], ALU.mult, part=True)
                hg = P(l, ci, sub * 3 + 2)
                for m in range(8):
                    po = PS[4 + m % 2]
                    for jj in range(NJ):
                        M.mm(po[:, :N], W2[:, jj, m * 128:(m + 1) * 128], uT[:, jj, :N], start=(jj == 0), stop=(jj == NJ - 1))
                    M.stt(xTk[m][:, :N], po[:, :N], hg[:, m:m + 1], xTk[m][:, :N], ALU.mult, ALU.add)
                    M.dma("sp", xTd[:, m, t0:t0 + N], xTk[m][:, 0:N])
                    if ix + 1 < len(tiles):
                        tn, Nn, _ = tiles[ix + 1]
                        M.dma("sp", xTk[m][:, 0:Nn], xTd[:, m, tn:tn + Nn])
        M.barrier()

    FMG = [("mlq", 0, 4, None), ("mlk", 512, 4, None), ("mlo", 1536, 4, None), ("dfq", 2064, 4, 0.125), ("dfk", 2576, 4, None),
           ("naq", 3600, 4, 0.125), ("nak", 4112, 4, None), ("gate", 5136, 24, None)]
    TMG = [("mlv", 1024, 512), ("dfv", 3088, 512), ("nav", 4624, 512), ("mlg", 2048, 16)]

    def mixin_stage(l):
        with ExitStack() as es:
            W = sb(es, "Win", [128, 8, cfg.DIN], BF16); Wr = sb(es, "Wrot", [128, 8, 1024], BF16)
            xT = sb(es, "xT", [128, 8, 512], F32); hT = sb(es, "hT", [128, 8, 512], BF16)
            sqb = [sb(es, "sq%d" % i, [128, 512], F32) for i in range(2)]
            rstd = sb(es, "rstd", [128, 512], F32)
            stg = [sb(es, "stg%d" % i, [128, 4, 512], BF16) for i in range(2)]
            stt_ = [sb(es, "stt%d" % i, [128, 512], BF16) for i in range(2)]
            stf = sb(es, "stf", [128, 16], F32)
            rC = sb(es, "rC", [128, 512], F32); rS = sb(es, "rS", [128, 512], F32)
            wv = w_in[l].rearrange("(k p) n -> p k n", p=128)
            cw = cfg.DIN // 6
            for k in range(8):
                for c in range(6):
                    M.dma("pool", W[:, k, c * cw:(c + 1) * cw], wv[:, k, c * cw:(c + 1) * cw], part=True)
            for k in range(8):
                sv = W[:, k, 2064:3088].rearrange("p (b h d) -> p b h d", b=16, h=2, d=32)
                dv = Wr[:, k, :].rearrange("p (b h d) -> p b h d", b=16, h=2, d=32)
                M.ts("dve", dv[:, :, 0, :], sv[:, :, 1, :], -1.0, ALU.mult, part=True)
                M.copy("pool", dv[:, :, 1, :], sv[:, :, 0, :], part=True)
            si = 0; ti = 0; ev = 0
            M.dma("sp", xT[:, :, 0:cfg.tiles[0][1]], xTd[:, :, cfg.tiles[0][0]:cfg.tiles[0][0] + cfg.tiles[0][1]])
            for ix, (t0, N, isctx) in enumerate(cfg.tiles):
                ci = 1 if isctx else 0
                if not isctx:
                    M.dma("sp", rC[:, :N], rope_in[0, :, t0 - CTX:t0 - CTX + N]); M.dma("sp", rS[:, :N], rope_in[1, :, t0 - CTX:t0 - CTX + N])
                rms_mod(xT, hT, N, P(l, ci, 3), P(l, ci, 4), sqb, rstd, sqb)
                if ix + 1 < len(cfg.tiles):
                    tn, Nn, _ = cfg.tiles[ix + 1]
                    M.dma("sp", xT[:, :, 0:Nn], xTd[:, :, tn:tn + Nn])
                for (name, c0, nch, scl) in FMG:
                    rope = name in ("dfq", "dfk") and not isctx
                    for c in range(nch):
                        st = stg[si % 2]
                        pa = PS[(ev % 2) * 2]; pb = PS[(ev % 2) * 2 + 1]; ev += 1
                        col = c0 + c * 128
                        for k in range(8):
                            M.mm(pa[:, :N], W[:, k, col:col + 128], hT[:, k, :N], start=(k == 0), stop=(k == 7))
                        if rope:
                            rc = col - 2064
                            for k in range(8):
                                M.mm(pb[:, :N], Wr[:, k, rc:rc + 128], hT[:, k, :N], start=(k == 0), stop=(k == 7))
                            t1 = sqb[0]; t2 = sqb[1]
                            M.stt(t1[:, :N], pa[:, :N], float(scl or 1.0), rC[:, :N], ALU.mult, ALU.mult)
                            M.stt(t2[:, :N], pb[:, :N], float(scl or 1.0), rS[:, :N], ALU.mult, ALU.mult)
                            M.tt("pool", st[:, c % 4, :N], t1[:, :N], t2[:, :N], ALU.add, part=True)
                        elif ev % 2 == 0:
                            M.act(st[:, c % 4, :N], pa[:, :N], AF.Copy, scale=float(scl or 1.0), part=True)
                        else:
                            M.ts("dve", st[:, c % 4, :N], pa[:, :N], float(scl or 1.0), ALU.mult, part=True)
                        if c % 4 == 3:
                            r0 = (c - 3) * 128
                            M.dma("sp", fm[name][r0:r0 + 512, t0:t0 + N].rearrange("(c p) t -> p c t", p=128), st[:, :, 0:N])
                            si += 1
                for tb in range(N // 128):
                    for (name, c0, ncol) in TMG:
                        pa = PS[4 + ti % 2]
                        for k in range(8):
                            M.mm(pa[:, :ncol], hT[:, k, tb * 128:(tb + 1) * 128], W[:, k, c0:c0 + ncol], start=(k == 0), stop=(k == 7))
                        dst = tm[name][t0 + tb * 128:t0 + (tb + 1) * 128, :]
                        if name == "mlg":
                            M.copy("dve", stf[:, :], pa[:, 0:16]); M.dma("sp", dst, stf[:, :])
                        else:
                            so = stt_[ti % 2]
                            if ti % 2 == 0:
                                M.copy("act", so[:, :], pa[:, :])
                            else:
                                M.copy("dve", so[:, :], pa[:, :])
                            M.dma("sp", dst, so[:, :])
                        ti += 1
        M.barrier()

    def diff_stage(l, ctx_out):
        lam_init = 0.8 - 0.6 * math.exp(-0.3 * l)
        with ExitStack() as es:
            lv = sb(es, "lv", [1, 256], F32); lt = sb(es, "lt", [1, 8], F32)
            nlam = sb(es, "nlam", [128, 1], F32); ngd = sb(es, "ngd", [128, 4], F32)
            kzs = [[sb(es, "kz%d_%d" % (i, jb), [128, T], BF16) for i in range(2)] for jb in range(2)]
            Vs = [sb(es, "V%d" % jb, [128, NB, 128], BF16) for jb in range(2)]
            qT = [sb(es, "qT%d" % i, [128, 512], BF16) for i in range(2)]
            pt = [sb(es, "pt%d" % i, [128, 512], BF16) for i in range(4)]
            acc = [sb(es, "acc%d" % i, [128, 512], F32) for i in range(2)]
            for jb in range(2):
                M.memset("pool", kzs[jb][0][64:128, :], 0.0); M.memset("pool", kzs[jb][1][0:64, :], 0.0)

            def ldh(h):
                for m in range(2):
                    M.dma("sp", kzs[h % 2][m][m * 64:(m + 1) * 64, :], fm["dfk"][h * 128 + m * 64:h * 128 + (m + 1) * 64, :], part=True)
                M.dma("sp", Vs[h % 2][:, :, :], tm["dfv"][:, h * 128:(h + 1) * 128].rearrange("(b p) c -> p b c", p=128))
            r1 = sb(es, "r1", [128, 512], F32); r2 = sb(es, "r2", [128, 512], F32)
            t1 = sb(es, "t1", [128, 512], F32); t2 = sb(es, "t2", [128, 512], F32)
            yb = [sb(es, "yb%d" % i, [128, 512], BF16) for i in range(2)]
            M.dma("sp", lv[:, :], dfl_in[l, :, :])
            lp = sb(es, "lp", [1, 128], F32)
            for i in range(2):
                M.tt("dve", lp[:, 64 * i:64 * i + 64], lv[:, 128 * i:128 * i + 64], lv[:, 128 * i + 64:128 * i + 128], ALU.mult, part=True)
            M.op("dve", "tensor_reduce", [lt[:, 0:2]], [lp[:, :]], out=lt[:, 0:2], in_=lp[:, :].rearrange("p (a b) -> p a b", a=2),
                 axis=mybir.AxisListType.X, op=ALU.add)
            M.act(lt[:, 2:4], lt[:, 0:2], AF.Exp)
            M.tt("dve", lt[:, 4:5], lt[:, 3:4], lt[:, 2:3], ALU.subtract)
            M.ts("dve", lt[:, 5:6], lt[:, 4:5], -lam_init, ALU.add)
            M.mm(PS[7][:, 0:1], ones_f[0:1, :], lt[0:1, 5:6])
            M.copy("dve", nlam[:, :], PS[7][:, 0:1])
            M.ts("dve", ngd[:, :], pvs[:, l, 132:136], 1.0 - lam_init, ALU.mult)
            qi = 0
            items = [(h, t0, N, isctx) for h in range(4) for (t0, N, isctx) in cfg.tiles if ctx_out or not isctx]

            def ldq(ii):
                hh, tt0, NN, _ = items[ii]
                M.dma("sp", qT[ii % 2][:, :NN], fm["dfq"][hh * 128:(hh + 1) * 128, tt0:tt0 + NN])

            ldh(0); ldq(0)
            for ii, (h, t0, N, isctx) in enumerate(items):
                if True:
                    if (ii == 0 or items[ii - 1][0] != h) and h + 1 < 4:
                        ldh(h + 1)
                    kz = kzs[h % 2]; V = Vs[h % 2]
                    if ii + 1 < len(items):
                        ldq(ii + 1)
                    q = qT[qi % 2]; yo = yb[qi % 2]; qi += 1
                    kbs = list(range(cfg.NCB)) if isctx else list(range(NB))
                    O = [PS[0], PS[1]]
                    its = [(i, kb, m) for i, kb in enumerate(kbs) for m in range(2)]
                    nk = len(kbs)

                    def qk(j):
                        i, kb, m = its[j]
                        M.mm(PS[2 + j % 4][:, :N], kz[m][:, kb * 128:(kb + 1) * 128], q[:, :N])

                    def ex(j):
                        M.act(pt[j % 4][:, :N], PS[2 + j % 4][:, :N], AF.Exp)

                    def pv(j):
                        i, kb, m = its[j]
                        M.mm(O[m][:, :N], V[:, kb, :], pt[j % 4][:, :N], start=(i == 0), stop=(i == nk - 1))
                        if m == 0:
                            M.mm(PS[6][:, :N], ones_b[:, :], pt[j % 4][:, :N], start=(i == 0), stop=(i == nk - 1))
                        elif i == 0:
                            M.copy("dve", acc[1][:, :N], pt[j % 4][:, :N])
                        else:
                            M.tt("dve", acc[1][:, :N], acc[1][:, :N], pt[j % 4][:, :N], ALU.add)

                    pipeline(len(its), [qk, ex, pv], 3)
                    M.recip(r1[:, :N], PS[6][:, :N])
                    M.mm(PS[7][:, :N], ones_f[:, :], acc[1][:, :N])
                    M.recip(r2[:, :N], PS[7][:, :N])
                    M.tt("dve", t1[:, :N], O[0][:, :N], r1[:, :N], ALU.mult)
                    M.tt("dve", t2[:, :N], O[1][:, :N], r2[:, :N], ALU.mult)
                    M.stt(t1[:, :N], t2[:, :N], nlam[:, 0:1], t1[:, :N], ALU.mult, ALU.add)
                    M.act(t2[:, :N], t1[:, :N], AF.Square)
                    M.mm(PS[7][:, :N], ones_f[:, :], t2[:, :N])
                    M.act(r1[:, :N], PS[7][:, :N], AF.Sqrt, bias=eps_ap, scale=1.0 / 128)
                    M.recip(r1[:, :N], r1[:, :N])
                    M.stt(yo[:, :N], t1[:, :N], ngd[:, h:h + 1], r1[:, :N], ALU.mult, ALU.mult)
                    M.dma("sp", fm["dfy"][h * 128:(h + 1) * 128, t0:t0 + N], yo[:, :N])
        M.barrier()

    def na_stage(l, ctx_out):
        ROWS, G = cfg.ROWS, cfg.G
        with ExitStack() as es:
            A = sb(es, "A", [32, 120], F32); Ind = sb(es, "Ind", [32, 4096], F32); Tsb = sb(es, "Tsb", [128, 4096], BF16)
            M.dma("sp", A[:, :], rbT_in[l, :, :]); M.dma("sp", Ind[:, :], ind_in[:, :])
            for cc in range(8):
                ps = PS[cc % 2]
                M.mm(ps[0:120, :], A[:, :], Ind[:, cc * 512:(cc + 1) * 512])
                M.copy("dve" if cc % 2 else "act", Tsb[0:120, cc * 512:(cc + 1) * 512], ps[0:120, :], part=True)
            M.dma("sp", trev[:, :], Tsb[0:120, :])
        M.barrier()
        with ExitStack() as es:
            BTs = [sb(es, "BT%d" % i, [128, 48, 512], BF16) for i in range(2)]
            kzs = [[sb(es, "kz%d_%d" % (i, jb), [128, T], BF16) for i in range(2)] for jb in range(2)]
            Vzs = [[sb(es, "Vz%d_%d" % (i, jb), [128, NB, 128], BF16) for i in range(2)] for jb in range(2)]
            oz = [sb(es, "oz%d" % i, [128, 128], BF16) for i in range(2)]
            qT = [sb(es, "qT%d" % i, [128, 512], BF16) for i in range(2)]
            pt = [sb(es, "pt%d" % i, [128, 512], BF16) for i in range(4)]
            sf = [sb(es, "sf%d" % i, [128, 512], F32) for i in range(4)]
            r1 = sb(es, "r1", [128, 512], F32)
            yb = [sb(es, "yb%d" % i, [128, 512], BF16) for i in range(2)]
            for e in range(2):
                for jb in range(2):
                    M.memset("pool", kzs[jb][e][(1 - e) * 64:(2 - e) * 64, :], 0.0)
                    M.memset("pool", Vzs[jb][e][:, :, :], 0.0)
                M.memset("pool", oz[e][:, :], 0.0); M.memset("pool", oz[e][:, e * 64:(e + 1) * 64], 1.0)

            def ldc(c):
                kz = kzs[c % 2]; Vz = Vzs[c % 2]; BT = BTs[c % 2]
                for e in range(2):
                    M.dma("sp", kz[e][e * 64:(e + 1) * 64, :], fm["nak"][c * 128 + e * 64:c * 128 + (e + 1) * 64, :], part=True)
                    M.dma("sp", Vz[e][:, :, e * 64:(e + 1) * 64],
                          tm["nav"][:, c * 128 + e * 64:c * 128 + (e + 1) * 64].rearrange("(b p) c -> p b c", p=128), part=True)
                M.memset("pool", BT[:, :, :], NEG)
                for pat in range(3):
                    g = (0, 1, G - 1)[pat]
                    rs0 = min(max(8 * g - 4, 0), ROWS - 16)
                    for e in range(2):
                        h = 2 * c + e
                        for kb in range(8):
                            for kr2 in range(2):
                                kr = rs0 + 2 * kb + kr2
                                val = [qr for qr in range(8) if 0 <= kr - min(max(8 * g + qr - 4, 0), ROWS - 8) < 8]
                                if not val:
                                    continue
                                qa, qb = val[0], val[-1] + 1
                                assert val == list(range(qa, qb))
                                ja = 7 - kr + 8 * g + qa
                                assert 0 <= ja and ja + (qb - qa) - 1 <= 14
                                src = trev[h * 15 + ja:h * 15 + ja + (qb - qa), :].rearrange("j (cp c) -> cp j c", c=64)
                                dst = BT[kr2 * 64:(kr2 + 1) * 64, (pat * 2 + e) * 8 + kb, qa * 64:qb * 64].rearrange("p (j c) -> p j c", c=64)
                                M.dma("sp", dst, src, part=True)

            qi = 0
            ldc(0)
            for c in range(4):
                if c + 1 < 4:
                    ldc(c + 1)
                kz = kzs[c % 2]; Vz = Vzs[c % 2]; BT = BTs[c % 2]
                qtiles = [(CTX + g * 512, 512, g) for g in range(G)] + ([(0, CTX, -1)] if ctx_out else [])
                for qx, (t0, N, g) in enumerate(qtiles):
                    if qx == 0 and c == 0:
                        M.dma("sp", qT[qi % 2][:, :N], fm["naq"][c * 128:(c + 1) * 128, t0:t0 + N])
                    nx = (c, qx + 1) if qx + 1 < len(qtiles) else ((c + 1, 0) if c + 1 < 4 else None)
                    if nx is not None:
                        tn, Nn, _ = qtiles[nx[1]]
                        M.dma("sp", qT[(qi + 1) % 2][:, :Nn], fm["naq"][nx[0] * 128:(nx[0] + 1) * 128, tn:tn + Nn])
                    q = qT[qi % 2]; yo = yb[qi % 2]; qi += 1
                    if g >= 0:
                        pat = 0 if g == 0 else (2 if g == G - 1 else 1)
                        rs0 = min(max(8 * g - 4, 0), ROWS - 16)
                        blks = [((CTX + rs0 * 64) // 128 + kb, kb) for kb in range(8)] + [(b, -1) for b in range(cfg.NCB)]
                    else:
                        blks = [(b, -1) for b in range(cfg.NCB)]
                    O = PS[0]; Dn = PS[1]
                    its = [(e, i, blk, kb) for e in range(2) for i, (blk, kb) in enumerate(blks)]
                    nk = len(blks)

                    def qk(j):
                        e, i, blk, kb = its[j]
                        M.mm(PS[2 + j % 6][:, :N], kz[e][:, blk * 128:(blk + 1) * 128], q[:, :N])

                    def ex(j):
                        e, i, blk, kb = its[j]
                        ps = PS[2 + j % 6]; p_ = pt[j % 4]; s_ = sf[j % 4]
                        if kb >= 0:
                            M.tt("dve", s_[:, :N], ps[:, :N], BT[:, (pat * 2 + e) * 8 + kb, :N], ALU.add)
                            M.act(p_[:, :N], s_[:, :N], AF.Exp)
                        else:
                            M.act(p_[:, :N], ps[:, :N], AF.Exp)

                    def pv(j):
                        e, i, blk, kb = its[j]
                        p_ = pt[j % 4]
                        first = (j == 0); last = (j == len(its) - 1)
                        M.mm(O[:, :N], Vz[e][:, blk, :], p_[:, :N], start=first, stop=last)
                        M.mm(Dn[:, :N], oz[e][:, :], p_[:, :N], start=first, stop=last)

                    pipeline(len(its), [qk, ex, pv], 3)
                    M.recip(r1[:, :N], Dn[:, :N])
                    M.tt("dve", yo[:, :N], O[:, :N], r1[:, :N], ALU.mult)
                    M.dma("sp", fm["nay"][c * 128:(c + 1) * 128, t0:t0 + N], yo[:, :N])
        M.barrier()

    def mlstm_stage(l, ctx_out):
        with ExitStack() as es:
            Gt = sb(es, "Gt", [128, NB, 16], F32); gbs = sb(es, "gbs", [128, 16], F32)
            LF = sb(es, "LF", [128, NB, 2, 4], F32); Bc = sb(es, "Bc", [128, NB, 2, 4], F32)
            eL = sb(es, "eL", [128, NB, 2, 4], F32); EK = sb(es, "EK", [128, NB, 2, 4], F32); EMB = sb(es, "EMB", [128, NB, 2, 4], F32)
            raw = sb(es, "raw", [128, T], BF16); cvb = sb(es, "cvb", [128, T], F32); k32 = sb(es, "k32", [128, T], F32)
            qT = sb(es, "qT", [128, T], BF16); kT = sb(es, "kT", [128, T], BF16); sigo = sb(es, "sigo", [128, T], BF16)
            Va = sb(es, "Va", [128, NB, 129], BF16); hF = sb(es, "hF", [128, NB, 128], F32); ybuf = sb(es, "ybuf", [128, T], BF16)
            CT = sb(es, "CT", [128, 129], F32); CTb = sb(es, "CTb", [128, 129], BF16); tmc = sb(es, "tmc", [128, 129], F32)
            ktd = [sb(es, "ktd%d" % i, [128, 128], BF16) for i in range(2)]
            Sm = [sb(es, "Sm%d" % i, [128, 128], BF16) for i in range(2)]
            hs = [sb(es, "hs%d" % i, [128, 128], F32) for i in range(2)]
            hn = [sb(es, "hn%d" % i, [128, 128], F32) for i in range(2)]
            smv = [sb(es, "sm%d" % i, [128, 8], F32) for i in range(2)]
            sqh = sb(es, "sqh", [128, NB, 128], F32); ssq = sb(es, "ssq", [128, NB], F32)
            M.dma("sp", Gt[:, :, :], tm["mlg"].rearrange("(b p) c -> p b c", p=128)); M.dma("sp", gbs[:, :], gb_in[l, :, :])
            for j in range(16):
                M.ts("dve", Gt[:, :, j], Gt[:, :, j], gbs[:, j:j + 1], ALU.add, part=True)
            for d in range(2):
                M.act(LF[:, :, d, :], Gt[:, :, d * 8 + 4:d * 8 + 8], AF.Exp, scale=-1.0, part=True)
            M.act(LF[:, :, :, :], LF[:, :, :, :], AF.Ln, bias=one_ap)
            M.ts("dve", LF[:, :, :, :], LF[:, :, :, :], -1.0, ALU.mult)
            M.mm(PS[0][:, 0:NB * 4], triF, LF[:, :, 0, :]); M.mm(PS[1][:, 0:NB * 4], triB, LF[:, :, 1, :])
            M.mm(PS[2][:, 0:NB * 8], ones_f[:, :], LF[:, :, :, :])
            M.act(eL[:, :, :, :], PS[2][:, 0:NB * 8].rearrange("p (b d h) -> p b d h", d=2, h=4), AF.Exp)
            for d in range(2):
                pv_ = PS[d][:, 0:NB * 4].rearrange("p (b h) -> p b h", h=4)
                M.copy("dve", Bc[:, :, d, :], pv_, part=True)
                M.tt("dve", EK[:, :, d, :], Gt[:, :, d * 8:d * 8 + 4], pv_, ALU.subtract, part=True)
            M.act(EK[:, :, :, :], EK[:, :, :, :], AF.Exp)
            M.act(EMB[:, :, :, :], Bc[:, :, :, :], AF.Exp, scale=-1.0)
            segs = ((0, CTX), (CTX, T))
            for h in range(4):
                for (name, chunk, isq) in (("mlq", h, True), ("mlk", 4 + h, False)):
                    M.dma("sp", raw[:, :], fm[name][h * 128:(h + 1) * 128, :])
                    cwv = lambda j: pvs[:, l, 96 + j * 8 + chunk:96 + j * 8 + chunk + 1]
                    for (a, b) in segs:
                        M.ts("dve", cvb[:, a:b], raw[:, a:b], cwv(1), ALU.mult, pvs[:, l, 120 + chunk:121 + chunk], ALU.add, part=True)
                        M.stt(cvb[:, a + 1:b], raw[:, a:b - 1], cwv(0), cvb[:, a + 1:b], ALU.mult, ALU.add, part=True)
                        M.stt(cvb[:, a:b - 1], raw[:, a + 1:b], cwv(2), cvb[:, a:b - 1], ALU.mult, ALU.add, part=True)
                    if isq:
                        M.act(k32[:, :], cvb[:, :], AF.Silu)
                        M.ts("dve", qT[:, :], k32[:, :], 128.0 ** -0.5, ALU.mult)
                    else:
                        M.act(k32[:, :], cvb[:, :], AF.Silu)
                        M.copy("pool", kT[:, :], k32[:, :])
                M.dma("sp", raw[:, :], fm["mlo"][h * 128:(h + 1) * 128, :])
                M.act(sigo[:, :], raw[:, :], AF.Sigmoid)
                M.dma("sp", Va[:, :, 0:128], tm["mlv"][:, h * 128:(h + 1) * 128].rearrange("(b p) c -> p b c", p=128))
                M.memset("pool", Va[:, :, 128:129], 1.0, part=True)
                for d in range(2):
                    order = list(range(NB)) if d == 0 else (list(range(cfg.NCB - 1, -1, -1)) + list(range(NB - 1, cfg.NCB - 1, -1)))
                    tri = triF if d == 0 else triB
                    M.memset("pool", CT[:, :], 0.0); M.memset("pool", CTb[:, :], 0.0)

                    def stA(j, d=d, order=order, tri=tri):
                        blk = order[j]; cs = slice(blk * 128, (blk + 1) * 128)
                        ek = EK[:, blk, d, h:h + 1]
                        pk = PS[j % 2]; pU = PS[2 + j % 2]
                        M.tr(pk[:, 0:128], k32[:, cs], ident)
                        M.ts("dve", ktd[j % 2][:, :], pk[:, 0:128], ek, ALU.mult)
                        M.mm(pU[:, 0:129], ktd[j % 2][:, :], Va[:, blk, :])

                    def stB(j, d=d, order=order, tri=tri):
                        blk = order[j]; cs = slice(blk * 128, (blk + 1) * 128)
                        pO = PS[5 + j % 2]; pU = PS[2 + j % 2]; pT = PS[7]; pS = PS[4]
                        hs_ = hs[j % 2]; hn_ = hn[j % 2]; sm_ = smv[j % 2]
                        M.mm(pS[:, 0:128], kT[:, cs], qT[:, cs])
                        M.stt(Sm[j % 2][:, :], pS[:, 0:128], EK[:, blk, d, h:h + 1], tri, ALU.mult, ALU.mult)
                        M.mm(pO[:, 0:129], Sm[j % 2][:, :], Va[:, blk, :], start=True, stop=False)
                        M.mm(pO[:, 0:129], qT[:, cs], CTb[:, :], start=False, stop=True)
                        M.tt("dve", tmc[:, :], pU[:, 0:129], CT[:, :], ALU.add)
                        M.ts("dve", CT[:, :], tmc[:, :], eL[:, blk, d, h:h + 1], ALU.mult)
                        M.copy("pool", CTb[:, :], CT[:, :])
                        M.ts("dve", sm_[:, 0:1], pO[:, 128:129], -1.0, ALU.mult, EMB[:, blk, d, h:h + 1], ALU.max)
                        M.tt("dve", sm_[:, 0:1], sm_[:, 0:1], pO[:, 128:129], ALU.max)
                        M.recip(sm_[:, 1:2], sm_[:, 0:1])
                        if d == 0:
                            M.ts("dve", hF[:, blk, :], pO[:, 0:128], sm_[:, 1:2], ALU.mult, part=True)
                        else:
                            M.stt(hF[:, blk, :], pO[:, 0:128], sm_[:, 1:2], hF[:, blk, :], ALU.mult, ALU.add, part=True)

                    pipeline(NB, [stA, stB], 1)
                b0 = 0 if ctx_out else cfg.NCB
                M.act(sqh[:, b0:NB, :], hF[:, b0:NB, :], AF.Square)
                M.op("dve", "tensor_reduce", [ssq[:, b0:NB]], [sqh[:, b0:NB, :]], out=ssq[:, b0:NB], in_=sqh[:, b0:NB, :],
                     axis=mybir.AxisListType.X, op=ALU.add)
                M.act(ssq[:, b0:NB], ssq[:, b0:NB], AF.Sqrt, bias=eps_ap, scale=1.0 / 128)
                M.recip(ssq[:, b0:NB], ssq[:, b0:NB])
                for blk in range(b0, NB):
                    cs = slice(blk * 128, (blk + 1) * 128)
                    hn_ = hn[blk % 2]; pT = PS[blk % 2]
                    M.ts("dve", hn_[:, :], hF[:, blk, :], ssq[:, blk:blk + 1], ALU.mult)
                    M.tr(pT[:, 0:128], hn_[:, :], ident)
                    M.stt(ybuf[:, cs], pT[:, 0:128], pvs[:, l, 128 + h:129 + h], sigo[:, cs], ALU.mult, ALU.mult, part=True)
                a0 = 0 if ctx_out else CTX
                M.dma("sp", fm["mly"][h * 128:(h + 1) * 128, a0:T], ybuf[:, a0:T])
        M.barrier()

    def merge_stage(l, tiles):
        with ExitStack() as es:
            Wb = sb(es, "Wb", [128, 12, D], BF16); Wo = sb(es, "Wo", [128, 8, D], BF16)
            xTs = [sb(es, "xT%d" % i, [128, 8, 512], F32) for i in range(2)]
            brs = [sb(es, "br%d" % i, [128, 12, 512], BF16) for i in range(2)]
            gts = [sb(es, "gt%d" % i, [128, 24, 512], BF16) for i in range(2)]
            yT = sb(es, "yT", [128, 8, 512], BF16)

            def ldt(ix):
                t0, N, isctx = tiles[ix]
                M.dma("sp", xTs[ix % 2][:, :, 0:N], xTd[:, :, t0:t0 + N])
                for i, nm in enumerate(("mly", "dfy", "nay")):
                    M.dma("sp", brs[ix % 2][:, i * 4:(i + 1) * 4, 0:N], fm[nm][:, t0:t0 + N].rearrange("(c p) t -> p c t", p=128), part=True)
                M.dma("sp", gts[ix % 2][:, :, 0:N], fm["gate"][:, t0:t0 + N].rearrange("(c p) t -> p c t", p=128))
            ta = [sb(es, "ta%d" % i, [128, 512], F32) for i in range(2)]; tb_ = [sb(es, "tb%d" % i, [128, 512], F32) for i in range(2)]
            wbv = w_br[l].rearrange("i (k p) n -> p i k n", p=128); wov = w_out[l].rearrange("(k p) n -> p k n", p=128)
            for i in range(3):
                for k in range(4):
                    M.dma("pool", Wb[:, i * 4 + k, :], wbv[:, i, k, :], part=True)
            for k in range(8):
                M.dma("pool", Wo[:, k, :], wov[:, k, :], part=True)
            ldt(0)
            for ix, (t0, N, isctx) in enumerate(tiles):
                ci = 1 if isctx else 0
                if ix + 1 < len(tiles):
                    ldt(ix + 1)
                xT = xTs[ix % 2]; br = brs[ix % 2]; gt = gts[ix % 2]
                M.act(gt[:, :, 0:N], gt[:, :, 0:N], AF.Sigmoid)
                for m in range(8):
                    a_ = ta[m % 2]; b_ = tb_[m % 2]
                    for i in range(3):
                        pp = PS[(m % 2) * 3 + i]
                        for k in range(4):
                            M.mm(pp[:, :N], Wb[:, i * 4 + k, m * 128:(m + 1) * 128], br[:, i * 4 + k, :N], start=(k == 0), stop=(k == 3))
                    M.tt("dve", a_[:, :N], PS[(m % 2) * 3][:, :N], gt[:, m, :N], ALU.mult)
                    M.tt("dve", b_[:, :N], PS[(m % 2) * 3 + 1][:, :N], gt[:, 8 + m, :N], ALU.mult)
                    M.tt("pool", a_[:, :N], a_[:, :N], b_[:, :N], ALU.add)
                    M.tt("dve", b_[:, :N], PS[(m % 2) * 3 + 2][:, :N], gt[:, 16 + m, :N], ALU.mult)
                    M.tt("pool", yT[:, m, :N], a_[:, :N], b_[:, :N], ALU.add, part=True)
                g5 = P(l, ci, 5)
                for m2 in range(8):
                    po = PS[6 + m2 % 2]
                    for m in range(8):
                        M.mm(po[:, :N], Wo[:, m, m2 * 128:(m2 + 1) * 128], yT[:, m, :N], start=(m == 0), stop=(m == 7))
                    M.stt(xT[:, m2, :N], po[:, :N], g5[:, m2:m2 + 1], xT[:, m2, :N], ALU.mult, ALU.add, part=True)
                M.dma("sp", xTd[:, :, t0:t0 + N], xT[:, :, 0:N])
        M.barrier()

    def final_stage():
        with ExitStack() as es:
            xT = sb(es, "xT", [128, 8, 512], F32); xn = sb(es, "xn", [128, 8, 512], F32)
            sqb = [sb(es, "sq%d" % i, [128, 512], F32) for i in range(2)]
            rstd = sb(es, "rstd", [128, 512], F32)
            ob = [sb(es, "ob%d" % i, [128, D], F32) for i in range(2)]
            oi = 0
            for (t0, N, isctx) in cfg.tiles:
                if isctx:
                    continue
                M.dma("sp", xT[:, :, 0:N], xTd[:, :, t0:t0 + N])
                for k in range(8):
                    M.act(sqb[k % 2][:, :N], xT[:, k, :N], AF.Square)
                    M.mm(PS[7][:, :N], ones_f[:, :], sqb[k % 2][:, :N], start=(k == 0), stop=(k == 7))
                M.act(rstd[:, :N], PS[7][:, :N], AF.Sqrt, bias=eps_ap, scale=1.0 / D)
                M.recip(rstd[:, :N], rstd[:, :N])
                for k in range(8):
                    M.stt(xn[:, k, :N], xT[:, k, :N], cvs[:, 16 + k:17 + k], rstd[:, :N], ALU.mult, ALU.mult, part=True)
                for tb in range(N // 128):
                    o_ = ob[oi % 2]
                    for k in range(8):
                        pb = PS[(oi % 2) * 2 + k // 4]
                        M.tr(pb[:, (k % 4) * 128:(k % 4 + 1) * 128], xn[:, k, tb * 128:(tb + 1) * 128], ident)
                    M.copy("dve", o_[:, 0:512], PS[(oi % 2) * 2][:, :], part=True)
                    M.copy("act", o_[:, 512:1024], PS[(oi % 2) * 2 + 1][:, :], part=True)
                    r0 = t0 - CTX + tb * 128
                    M.dma("sp", y_out[r0:r0 + 128, :], o_[:, :])
                    oi += 1
        M.barrier()

    for l in range(DEPTH):
        ctx_out = l < DEPTH - 1
        ffn_stage(l, 0, cfg.tiles)
        mixin_stage(l)
        mlstm_stage(l, ctx_out)
        diff_stage(l, ctx_out)
        na_stage(l, ctx_out)
        t2 = cfg.tiles if ctx_out else [t for t in cfg.tiles if not t[2]]
        merge_stage(l, t2)
        ffn_stage(l, 1, t2)
    final_stage()
    gs_.close()
    return nc, M


def host_consts(cfg):
    S = cfg.S
    cmat = np.zeros((128, 3, 128), np.float32)
    i = np.arange(128)
    cmat[:, 0, :] = (i[:, None] == i[None, :]); cmat[:, 1, :] = (i[:, None] <= i[None, :]); cmat[:, 2, :] = (i[:, None] >= i[None, :])
    t = np.arange(S); row = (t // 64).astype(np.float32); col = (t % 64).astype(np.float32)
    freqs = (10000.0 ** (-np.arange(0, 32, 2, dtype=np.float32) / 32)).astype(np.float32)
    ang = np.concatenate([row[:, None] * freqs, col[:, None] * freqs], -1).astype(np.float32)
    rope = np.zeros((2, 128, S), np.float32)
    rope[0] = np.tile(np.cos(ang).T, (4, 1)); rope[1] = np.tile(np.sin(ang).T, (4, 1))
    ind = np.zeros((32, 64, 64), np.float32)
    cp = np.arange(64)[:, None]; c = np.arange(64)[None, :]
    for d in range(31):
        ind[d] = (cp - c + 15 == d)
    qstart = np.clip(c - 8, 0, 48)
    ind[31] = np.where((cp >= qstart) & (cp < qstart + 16), 0.0, NEG)
    return cmat, rope, ind.reshape(32, 4096)


def make_in_maps(cfg, inp):
    cmat, rope, ind = host_consts(cfg)
    DEPTH = cfg.DEPTH
    f = lambda a: np.ascontiguousarray(np.asarray(a, np.float32))
    pv = np.zeros((DEPTH, 128, 136), np.float32)
    for l in range(DEPTH):
        pv[l, :, 0:24] = inp["norm_g"][l].reshape(24, 128).T
        pv[l, :, 24:96] = inp["b_ada"][l].reshape(72, 128).T
        pv[l, :, 96:120] = inp["ml_conv_w"][l].reshape(24, 128).T
        pv[l, :, 120:128] = inp["ml_conv_b"][l].reshape(8, 128).T
        pv[l, :, 128:132] = inp["ml_norm_g"][l].reshape(4, 128).T
        pv[l, :, 132:136] = inp["df_norm_g"][l].reshape(4, 128).T
    gb = np.broadcast_to(np.asarray(inp["ml_gate_b"], np.float32)[:, None, :], (DEPTH, 128, 16))
    rbT = np.ones((DEPTH, 32, 120), np.float32)
    rb = np.asarray(inp["na_rel_bias"], np.float32)
    rbT[:, 0:31, :] = rb[:, :, ::-1, :].reshape(DEPTH, 120, 31).transpose(0, 2, 1)
    shared = {"w_ada": f(inp["w_ada"]), "pv": pv, "gb": f(gb), "ffn_w1": f(inp["ffn_w1"]), "ffn_w2": f(inp["ffn_w2"]),
              "w_in": f(inp["w_in"]), "dfl": f(np.asarray(inp["df_lambda"]).reshape(DEPTH, 1, 256)), "rbT": f(rbT),
              "w_branch": f(inp["w_branch"]), "w_out": f(inp["w_out"]), "cmat": cmat, "rope": rope, "ind": ind}
    maps = []
    for b in range(cfg.NC):
        cv = np.zeros((128, 24), np.float32)
        cv[:, 0:8] = np.asarray(inp["c"][b]).reshape(8, 128).T
        cv[:, 8:16] = np.asarray(inp["c_ctx"]).reshape(8, 128).T
        cv[:, 16:24] = np.asarray(inp["final_g"]).reshape(8, 128).T
        m = dict(shared); m["x"] = f(inp["x"][b]); m["ctx"] = f(inp["ctx"][b]); m["cv"] = cv
        maps.append(m)
    return maps


def kernel(**inputs):
    cfg = Cfg()
    nc, M = build(cfg)
    maps = make_in_maps(cfg, inputs)
    res = run_bass_kernel_spmd(nc, maps, core_ids=list(range(cfg.NC)))
    return np.stack([np.asarray(r["y"], np.float32) for r in res.results], 0)
```

```python
import math
from contextlib import ExitStack
import numpy as np
import concourse.bass as bass
import concourse.mybir as mybir
from concourse.bass_utils import run_bass_kernel_spmd

F32 = mybir.dt.float32
BF16 = mybir.dt.bfloat16
AF = mybir.ActivationFunctionType
ALU = mybir.AluOpType
KQ = 8
EPS = 1e-6
NEG = -30000.0


class Cfg:
    def __init__(s, rows=64, ctx=256, depth=4, dff=2816, ncores=8):
        s.ROWS = rows; s.GW = 64; s.S = rows * 64; s.CTX = ctx; s.T = s.S + ctx
        s.DEPTH = depth; s.DFF = dff; s.NJ = dff // 128; s.D = 1024; s.NB = s.T // 128
        s.NCB = ctx // 128; s.G = rows // 8; s.DIN = 5136 + 3072; s.NC = ncores
        s.tiles = [(0, ctx, True)] + [(ctx + i * 512, 512, False) for i in range(s.S // 512)]


class Buf:
    __slots__ = ("w", "r", "wf")

    def __init__(s):
        s.w = {}; s.r = {}; s.wf = {}


def _merge(d, e):
    for k, v in e.items():
        if d.get(k, 0) < v:
            d[k] = v


class Mach:
    def __init__(s, nc):
        s.nc = nc; s.es = ExitStack(); s.sems = []; s.bufs = {}; s.eng = {}
        for nm, h in (("pe", nc.tensor), ("act", nc.scalar), ("dve", nc.vector), ("pool", nc.gpsimd), ("sp", nc.sync)):
            s.eng[nm] = {"h": h, "si": s.newsem("e_" + nm), "cnt": 0, "seen": {}}
        s.dq = {q: {"sis": [s.newsem("d_%s%d" % (q, i)) for i in range(KQ)], "n": 0} for q in ("sp", "pool")}
        s.ninst = 0

    def newsem(s, name):
        s.sems.append(s.es.enter_context(s.nc.semaphore(name)))
        return len(s.sems) - 1

    def buf(s, ap):
        k = ap.tensor.name
        b = s.bufs.get(k)
        if b is None:
            b = s.bufs[k] = Buf()
        return b

    def wait(s, en, evs):
        E = s.eng[en]
        for si, v in evs.items():
            if en == "pe" and si == E["si"]:
                continue
            if E["seen"].get(si, 0) < v:
                E["h"].wait_ge(s.sems[si], v); E["seen"][si] = v; s.ninst += 1

    def _deps(s, outs, ins, part):
        evs = {}
        for ap in ins:
            _merge(evs, s.buf(ap).w)
        for ap in outs:
            b = s.buf(ap); _merge(evs, b.r)
            _merge(evs, b.wf if part else b.w)
        return evs

    def _rec(s, outs, ins, ev, part=False):
        for ap in ins:
            _merge(s.buf(ap).r, ev)
        for ap in outs:
            _merge(s.buf(ap).w, ev)
            if not part:
                _merge(s.buf(ap).wf, ev)

    def op(s, en, name, outs, ins, *a, part=False, **kw):
        E = s.eng[en]
        s.wait(en, s._deps(outs, ins, part))
        inst = getattr(E["h"], name)(*a, **kw)
        E["cnt"] += 1; s.ninst += 1
        inst.then_inc(s.sems[E["si"]], 1)
        s._rec(outs, ins, {E["si"]: E["cnt"]}, part)

    def dma(s, q, out, in_, part=False, **kw):
        Q = s.dq[q]; slot = Q["n"] % KQ; gen = Q["n"] // KQ; si = Q["sis"][slot]
        evs = s._deps([out], [in_], part)
        if gen > 0:
            _merge(evs, {si: 16 * gen})
        s.wait(q, evs)
        s.eng[q]["h"].dma_start(out=out, in_=in_, **kw).then_inc(s.sems[si], 16)
        Q["n"] += 1; s.ninst += 1
        s._rec([out], [in_], {si: 16 * (gen + 1)}, part)

    def barrier(s):
        evs = {E["si"]: E["cnt"] for E in s.eng.values() if E["cnt"] > 0}
        for Q in s.dq.values():
            for i, si in enumerate(Q["sis"]):
                n = (Q["n"] - i + KQ - 1) // KQ
                if n > 0:
                    evs[si] = 16 * n
        for en in s.eng:
            s.wait(en, evs)

    def mm(s, out, lhsT, rhs, start=True, stop=True):
        s.op("pe", "matmul", [out], [lhsT, rhs], out, lhsT=lhsT, rhs=rhs, start=start, stop=stop)

    def tr(s, out, in_, ident):
        s.op("pe", "transpose", [out], [in_, ident], out, in_, ident)

    def act(s, out, in_, func, bias=None, scale=None, accum_out=None, part=False):
        kw = {}; ins = [in_]; outs = [out]
        if bias is not None:
            kw["bias"] = bias
            if not isinstance(bias, float):
                ins.append(bias)
        if scale is not None:
            kw["scale"] = scale
            if not isinstance(scale, float):
                ins.append(scale)
        if accum_out is not None:
            kw["accum_out"] = accum_out; outs.append(accum_out)
        s.op("act", "activation", outs, ins, out=out, in_=in_, func=func, part=part, **kw)

    def tt(s, en, out, in0, in1, op, part=False):
        s.op(en, "tensor_tensor", [out], [in0, in1], out=out, in0=in0, in1=in1, op=op, part=part)

    def ts(s, en, out, in0, s1, op0, s2=None, op1=None, part=False):
        ins = [in0] + [x for x in (s1, s2) if x is not None and not isinstance(x, float)]
        kw = {"op1": op1} if op1 is not None else {}
        s.op(en, "tensor_scalar", [out], ins, out=out, in0=in0, scalar1=s1, scalar2=s2, op0=op0, part=part, **kw)

    def stt(s, out, in0, scalar, in1, op0, op1, part=False):
        ins = [in0, in1] + ([] if isinstance(scalar, float) else [scalar])
        s.op("dve", "scalar_tensor_tensor", [out], ins, out=out, in0=in0, scalar=scalar, in1=in1, op0=op0, op1=op1, part=part)

    def copy(s, en, out, in_, part=False):
        if en == "act":
            s.op("act", "activation", [out], [in_], out=out, in_=in_, func=AF.Copy, part=part)
        else:
            s.op(en, "tensor_copy", [out], [in_], out=out, in_=in_, part=part)

    def memset(s, en, ap, val, part=False):
        s.op(en, "memset", [ap], [], ap, val, part=part)

    def recip(s, out, in_, part=False):
        s.op("dve", "reciprocal", [out], [in_], out=out, in_=in_, part=part)


def pipeline(n, stages, look):
    for j in range(min(look, n)):
        stages[0](j)
    for j in range(n):
        if j + look < n:
            stages[0](j + look)
        for f in stages[1:]:
            f(j)


def build(cfg):
    nc = bass.Bass("TRN2", target_bir_lowering=False)
    T, S, CTX, D, DFF, NJ, NB, DEPTH = cfg.T, cfg.S, cfg.CTX, cfg.D, cfg.DFF, cfg.NJ, cfg.NB, cfg.DEPTH

    def din(name, shape, dt=F32):
        return nc.dram_tensor(name, list(shape), dt, kind="ExternalInput").ap()

    def dscr(name, shape, dt):
        return nc.dram_tensor(name, list(shape), dt, kind="Internal").ap()

    x_in = din("x", [S, D]); ctx_in = din("ctx", [CTX, D]); cv_in = din("cv", [128, 24])
    w_ada = din("w_ada", [DEPTH, D, 9 * D]); pv_in = din("pv", [DEPTH, 128, 136]); gb_in = din("gb", [DEPTH, 128, 16])
    ffn_w1 = din("ffn_w1", [DEPTH, 2, D, 2 * DFF]); ffn_w2 = din("ffn_w2", [DEPTH, 2, DFF, D])
    w_in = din("w_in", [DEPTH, D, cfg.DIN]); dfl_in = din("dfl", [DEPTH, 1, 256]); rbT_in = din("rbT", [DEPTH, 32, 120])
    w_br = din("w_branch", [DEPTH, 3, 512, D]); w_out = din("w_out", [DEPTH, D, D])
    cmat_in = din("cmat", [128, 3, 128]); rope_in = din("rope", [2, 128, S]); ind_in = din("ind", [32, 4096])
    y_out = nc.dram_tensor("y", [S, D], F32, kind="ExternalOutput").ap()

    xTd = dscr("xTd", [D, T], F32).rearrange("(k p) t -> p k t", p=128)
    fm = {n: dscr("fm_" + n, [r, T], BF16) for n, r in (("mlq", 512), ("mlk", 512), ("mlo", 512), ("dfq", 512), ("dfk", 512),
                                                        ("naq", 512), ("nak", 512), ("gate", 3072), ("mly", 512), ("dfy", 512), ("nay", 512))}
    tm = {n: dscr("tm_" + n, [T, c], dt) for n, c, dt in (("mlv", 512, BF16), ("dfv", 512, BF16), ("nav", 512, BF16), ("mlg", 16, F32))}
    trev_all = dscr("trev", [DEPTH, 120, 4096], BF16)

    M = Mach(nc)
    gs_ = ExitStack()

    _nm = [0]

    def sb(es, name, shape, dt):
        _nm[0] += 1
        return es.enter_context(nc.sbuf_tensor("%s_%d" % (name, _nm[0]), list(shape), dt))

    PS = [gs_.enter_context(nc.psum_tensor("ps%d" % i, [128, 512], F32)) for i in range(8)]
    cm = sb(gs_, "cmat", [128, 3, 128], F32)
    ones_f = sb(gs_, "ones_f", [128, 128], F32); ones_b = sb(gs_, "ones_b", [128, 128], BF16)
    epsc = sb(gs_, "epsc", [128, 2], F32)
    cvs = sb(gs_, "cvs", [128, 24], F32)
    prm = sb(gs_, "prm", [128, DEPTH * 2 * 9 * 8], F32)
    pvs = sb(gs_, "pvs", [128, DEPTH, 136], F32)
    ident = cm[:, 0, :]; triF = cm[:, 1, :]; triB = cm[:, 2, :]
    eps_ap = epsc[:, 0:1]; one_ap = epsc[:, 1:2]

    def P(l, ci, j):
        o = ((l * 2 + ci) * 9 + j) * 8
        return prm[:, o:o + 8]

    M.dma("sp", cm[:, :, :], cmat_in[:, :, :]); M.dma("sp", cvs[:, :], cv_in[:, :])
    for l in range(DEPTH):
        M.dma("sp", pvs[:, l, :], pv_in[l, :, :], part=True)
    M.memset("dve", ones_f[:, :], 1.0); M.memset("dve", ones_b[:, :], 1.0)
    M.memset("dve", epsc[:, 0:1], EPS); M.memset("dve", epsc[:, 1:2], 1.0, part=True)
    with ExitStack() as es:
        scT = sb(es, "scT", [128, 8, 2], F32)
        wa = [sb(es, "wa%d" % i, [128, 8, 1024], F32) for i in range(2)]
        mods = sb(es, "mods", [128, 72, 2], F32)
        for i in range(2):
            M.act(scT[:, :, i], cvs[:, i * 8:(i + 1) * 8], AF.Silu, part=True)
        it = 0
        for l in range(DEPTH):
            wv = w_ada[l].rearrange("(k p) n -> p k n", p=128)
            pm = PS[l % 2]
            for grp in range(9):
                w = wa[it % 2]; it += 1
                for k in range(8):
                    M.dma("sp", w[:, k, :], wv[:, k, grp * 1024:(grp + 1) * 1024], part=True)
                for fc in range(8):
                    c0 = (grp * 8 + fc) * 2
                    for k in range(8):
                        M.mm(pm[:, c0:c0 + 2], w[:, k, fc * 128:(fc + 1) * 128], scT[:, k, :], start=(k == 0), stop=(k == 7))
            pmv = pm[:, 0:144].rearrange("p (j i) -> p j i", i=2)
            for i in range(2):
                M.tt("dve", mods[:, :, i], pmv[:, :, i], pvs[:, l, 24:96], ALU.add, part=True)
            for ci in range(2):
                for sub in range(3):
                    mo = sub * 24
                    ng = pvs[:, l, sub * 8:(sub + 1) * 8]
                    M.stt(P(l, ci, sub * 3 + 0), mods[:, mo + 8:mo + 16, ci], 1.0, ng, ALU.add, ALU.mult, part=True)
                    M.copy("dve", P(l, ci, sub * 3 + 1), mods[:, mo:mo + 8, ci], part=True)
                    M.ts("dve", P(l, ci, sub * 3 + 2), mods[:, mo + 16:mo + 24, ci], 0.5 if sub != 1 else 1.0, ALU.mult, part=True)
    M.barrier()
    with ExitStack() as es:
        Ind = sb(es, "Ind", [32, 4096], F32)
        At = [sb(es, "A%d" % i, [32, 120], F32) for i in range(2)]
        Tsb = [sb(es, "Tsb%d" % i, [128, 4096], BF16) for i in range(2)]
        M.dma("sp", Ind[:, :], ind_in[:, :])
        for l in range(DEPTH):
            A = At[l % 2]; Tb = Tsb[l % 2]
            M.dma("sp", A[:, :], rbT_in[l, :, :])
            for cc in range(8):
                ps = PS[2 + cc % 2]
                M.mm(ps[0:120, :], A[:, :], Ind[:, cc * 512:(cc + 1) * 512])
                M.copy("dve" if cc % 2 else "act", Tb[0:120, cc * 512:(cc + 1) * 512], ps[0:120, :], part=True)
            M.dma("sp", trev_all[l, :, :], Tb[0:120, :])
    M.barrier()

    with ExitStack() as es:
        xin = [sb(es, "xin%d" % i, [128, D], F32) for i in range(2)]
        xtb = [sb(es, "xtb%d" % i, [128, 8, 128], F32) for i in range(2)]
        for b in range(NB):
            src = ctx_in[b * 128:(b + 1) * 128, :] if b < cfg.NCB else x_in[(b - cfg.NCB) * 128:(b - cfg.NCB + 1) * 128, :]
            xi = xin[b % 2]; xo = xtb[b % 2]
            M.dma("sp", xi[:, :], src)
            for k in range(8):
                pb = PS[(b % 2) * 2 + k // 4]
                M.tr(pb[:, (k % 4) * 128:(k % 4 + 1) * 128], xi[:, k * 128:(k + 1) * 128], ident)
            M.copy("dve", xo[:, 0:4, :], PS[(b % 2) * 2][:, :].rearrange("p (k t) -> p k t", k=4), part=True)
            M.copy("act", xo[:, 4:8, :], PS[(b % 2) * 2 + 1][:, :].rearrange("p (k t) -> p k t", k=4), part=True)
            M.dma("sp", xTd[:, :, b * 128:(b + 1) * 128], xo[:, :, :])
    M.barrier()

    def rms_mod(xT, hT, N, gsv, shv, sqb, rstd, tmpb):
        pss = PS[7]
        if not callable(xT):
            xt_ = xT
            xT = lambda k: xt_[:, k, :N]
        for k in range(8):
            s_ = sqb[k % 2]
            M.act(s_[:, :N], xT(k), AF.Square)
            M.mm(pss[:, :N], ones_f[:, :], s_[:, :N], start=(k == 0), stop=(k == 7))
        M.act(rstd[:, :N], pss[:, :N], AF.Sqrt, bias=eps_ap, scale=1.0 / D)
        M.recip(rstd[:, :N], rstd[:, :N])
        for k in range(8):
            t_ = tmpb[k % 2]
            M.stt(t_[:, :N], xT(k), gsv[:, k:k + 1], rstd[:, :N], ALU.mult, ALU.mult)
            M.act(hT[:, k, :N], t_[:, :N], AF.Identity, bias=shv[:, k:k + 1], part=True)

    def ffn_stage(l, j, tiles):
        sub = 0 if j == 0 else 2
        with ExitStack() as es:
            NG = min(4, NJ)
            gsz = [NJ // NG + (1 if g < NJ % NG else 0) for g in range(NG)]
            gj0 = [sum(gsz[:g]) for g in range(NG)]
            W1g = [sb(es, "W1g%d" % g, [128, 8, 2, gsz[g] * 128], BF16) for g in range(NG)]
            jg = [(g, jj - gj0[g]) for g in range(NG) for jj in range(gj0[g], gj0[g] + gsz[g])]
            W2 = sb(es, "W2", [128, NJ, D], BF16)
            xTk = [sb(es, "xTk%d" % k, [128, 512], F32) for k in range(8)]
            hT = sb(es, "hT", [128, 8, 512], BF16); uT = sb(es, "uT", [128, NJ, 512], BF16)
            sqb = [sb(es, "sq%d" % i, [128, 512], F32) for i in range(2)]
            rstd = sb(es, "rstd", [128, 512], F32)
            w1v = ffn_w1[l, j].rearrange("(k p) n -> p k n", p=128); w2v = ffn_w2[l, j].rearrange("(k p) n -> p k n", p=128)
            for g in range(NG):
                for k in range(8):
                    for ab in range(2):
                        c0 = ab * DFF + gj0[g] * 128
                        M.dma("pool", W1g[g][:, k, ab, :], w1v[:, k, c0:c0 + gsz[g] * 128], part=True)
            for k in range(NJ):
                M.dma("pool", W2[:, k, :], w2v[:, k, :], part=True)
            for k in range(8):
                M.dma("sp", xTk[k][:, 0:tiles[0][1]], xTd[:, k, tiles[0][0]:tiles[0][0] + tiles[0][1]])
            for ix, (t0, N, isctx) in enumerate(tiles):
                ci = 1 if isctx else 0
                rms_mod(lambda k: xTk[k][:, :N], hT, N, P(l, ci, sub * 3), P(l, ci, sub * 3 + 1), sqb, rstd, sqb)
                for jj in range(NJ):
                    pa = PS[(jj % 2) * 2]; pb = PS[(jj % 2) * 2 + 1]; sl = sqb[jj % 2]
                    g_, jo = jg[jj]
                    for k in range(8):
                        M.mm(pa[:, :N], W1g[g_][:, k, 0, jo * 128:(jo + 1) * 128], hT[:, k, :N], start=(k == 0), stop=(k == 7))
                    for k in range(8):
                        M.mm(pb[:, :N], W1g[g_][:, k, 1, jo * 128:(jo + 1) * 128], hT[:, k, :N], start=(k == 0), stop=(k == 7))
                    M.act(sl[:, :N], pa[:, :N], AF.Silu)
                    M.tt("dve", uT[:, jj, :N], sl[:, :N], pb[:, :N], ALU.mult, part=True)
                hg = P(l, ci, sub * 3 + 2)
                for m in range(8):
                    po = PS[4 + m % 2]
                    for jj in range(NJ):
                        M.mm(po[:, :N], W2[:, jj, m * 128:(m + 1) * 128], uT[:, jj, :N], start=(jj == 0), stop=(jj == NJ - 1))
                    M.stt(xTk[m][:, :N], po[:, :N], hg[:, m:m + 1], xTk[m][:, :N], ALU.mult, ALU.add)
                    M.dma("sp", xTd[:, m, t0:t0 + N], xTk[m][:, 0:N])
                    if ix + 1 < len(tiles):
                        tn, Nn, _ = tiles[ix + 1]
                        M.dma("sp", xTk[m][:, 0:Nn], xTd[:, m, tn:tn + Nn])
        M.barrier()

    FMG = [("mlq", 0, 4, None), ("mlk", 512, 4, None), ("mlo", 1536, 4, None), ("dfq", 2064, 4, 0.125), ("dfk", 2576, 4, None),
           ("naq", 3600, 4, 0.125), ("nak", 4112, 4, None), ("gate", 5136, 24, None)]
    TMG = [("mlv", 1024, 512), ("dfv", 3088, 512), ("nav", 4624, 512), ("mlg", 2048, 16)]

    def mixin_stage(l):
        with ExitStack() as es:
            W = sb(es, "Win", [128, 8, cfg.DIN], BF16); Wr = sb(es, "Wrot", [128, 8, 1024], BF16)
            xT = sb(es, "xT", [128, 8, 512], F32); hT = sb(es, "hT", [128, 8, 512], BF16)
            sqb = [sb(es, "sq%d" % i, [128, 512], F32) for i in range(2)]
            rstd = sb(es, "rstd", [128, 512], F32)
            stg = [sb(es, "stg%d" % i, [128, 4, 512], BF16) for i in range(2)]
            stt_ = [sb(es, "stt%d" % i, [128, 512], BF16) for i in range(2)]
            stf = sb(es, "stf", [128, 16], F32)
            rC = sb(es, "rC", [128, 512], F32); rS = sb(es, "rS", [128, 512], F32)
            wv = w_in[l].rearrange("(k p) n -> p k n", p=128)
            cw = cfg.DIN // 6
            for k in range(8):
                for c in range(6):
                    M.dma("pool", W[:, k, c * cw:(c + 1) * cw], wv[:, k, c * cw:(c + 1) * cw], part=True)
            for k in range(8):
                sv = W[:, k, 2064:3088].rearrange("p (b h d) -> p b h d", b=16, h=2, d=32)
                dv = Wr[:, k, :].rearrange("p (b h d) -> p b h d", b=16, h=2, d=32)
                M.ts("dve", dv[:, :, 0, :], sv[:, :, 1, :], -1.0, ALU.mult, part=True)
                M.copy("pool", dv[:, :, 1, :], sv[:, :, 0, :], part=True)
            si = 0; ti = 0; ev = 0
            M.dma("sp", xT[:, :, 0:cfg.tiles[0][1]], xTd[:, :, cfg.tiles[0][0]:cfg.tiles[0][0] + cfg.tiles[0][1]])
            for ix, (t0, N, isctx) in enumerate(cfg.tiles):
                ci = 1 if isctx else 0
                if not isctx:
                    M.dma("sp", rC[:, :N], rope_in[0, :, t0 - CTX:t0 - CTX + N]); M.dma("sp", rS[:, :N], rope_in[1, :, t0 - CTX:t0 - CTX + N])
                rms_mod(xT, hT, N, P(l, ci, 3), P(l, ci, 4), sqb, rstd, sqb)
                if ix + 1 < len(cfg.tiles):
                    tn, Nn, _ = cfg.tiles[ix + 1]
                    M.dma("sp", xT[:, :, 0:Nn], xTd[:, :, tn:tn + Nn])
                for (name, c0, nch, scl) in FMG:
                    rope = name in ("dfq", "dfk") and not isctx
                    for c in range(nch):
                        st = stg[si % 2]
                        pa = PS[(ev % 2) * 2]; pb = PS[(ev % 2) * 2 + 1]; ev += 1
                        col = c0 + c * 128
                        for k in range(8):
                            M.mm(pa[:, :N], W[:, k, col:col + 128], hT[:, k, :N], start=(k == 0), stop=(k == 7))
                        if rope:
                            rc = col - 2064
                            for k in range(8):
                                M.mm(pb[:, :N], Wr[:, k, rc:rc + 128], hT[:, k, :N], start=(k == 0), stop=(k == 7))
                            t1 = sqb[0]; t2 = sqb[1]
                            M.stt(t1[:, :N], pa[:, :N], float(scl or 1.0), rC[:, :N], ALU.mult, ALU.mult)
                            M.stt(t2[:, :N], pb[:, :N], float(scl or 1.0), rS[:, :N], ALU.mult, ALU.mult)
                            M.tt("pool", st[:, c % 4, :N], t1[:, :N], t2[:, :N], ALU.add, part=True)
                        elif ev % 2 == 0:
                            M.act(st[:, c % 4, :N], pa[:, :N], AF.Copy, scale=float(scl or 1.0), part=True)
                        else:
                            M.ts("dve", st[:, c % 4, :N], pa[:, :N], float(scl or 1.0), ALU.mult, part=True)
                        if c % 4 == 3:
                            r0 = (c - 3) * 128
                            M.dma("sp", fm[name][r0:r0 + 512, t0:t0 + N].rearrange("(c p) t -> p c t", p=128), st[:, :, 0:N])
                            si += 1
                for tb in range(N // 128):
                    for (name, c0, ncol) in TMG:
                        pa = PS[4 + ti % 2]
                        for k in range(8):
                            M.mm(pa[:, :ncol], hT[:, k, tb * 128:(tb + 1) * 128], W[:, k, c0:c0 + ncol], start=(k == 0), stop=(k == 7))
                        dst = tm[name][t0 + tb * 128:t0 + (tb + 1) * 128, :]
                        if name == "mlg":
                            M.copy("dve", stf[:, :], pa[:, 0:16]); M.dma("sp", dst, stf[:, :])
                        else:
                            so = stt_[ti % 2]
                            if ti % 2 == 0:
                                M.copy("act", so[:, :], pa[:, :])
                            else:
                                M.copy("dve", so[:, :], pa[:, :])
                            M.dma("sp", dst, so[:, :])
                        ti += 1
        M.barrier()

    def diff_stage(l, ctx_out):
        lam_init = 0.8 - 0.6 * math.exp(-0.3 * l)
        with ExitStack() as es:
            lv = sb(es, "lv", [1, 256], F32); lt = sb(es, "lt", [1, 8], F32)
            nlam = sb(es, "nlam", [128, 1], F32); ngd = sb(es, "ngd", [128, 4], F32)
            kzs = [[sb(es, "kz%d_%d" % (i, jb), [128, T], BF16) for i in range(2)] for jb in range(2)]
            Vs = [sb(es, "V%d" % jb, [128, NB, 128], BF16) for jb in range(2)]
            qT = [sb(es, "qT%d" % i, [128, 512], BF16) for i in range(2)]
            pt = [sb(es, "pt%d" % i, [128, 512], BF16) for i in range(4)]
            acc = [sb(es, "acc%d" % i, [128, 512], F32) for i in range(2)]
            for jb in range(2):
                M.memset("pool", kzs[jb][0][64:128, :], 0.0); M.memset("pool", kzs[jb][1][0:64, :], 0.0)

            def ldh(h):
                for m in range(2):
                    M.dma("sp", kzs[h % 2][m][m * 64:(m + 1) * 64, :], fm["dfk"][h * 128 + m * 64:h * 128 + (m + 1) * 64, :], part=True)
                M.dma("sp", Vs[h % 2][:, :, :], tm["dfv"][:, h * 128:(h + 1) * 128].rearrange("(b p) c -> p b c", p=128))
            r1 = sb(es, "r1", [128, 512], F32); r2 = sb(es, "r2", [128, 512], F32)
            t1 = sb(es, "t1", [128, 512], F32); t2 = sb(es, "t2", [128, 512], F32)
            yb = [sb(es, "yb%d" % i, [128, 512], BF16) for i in range(2)]
            M.dma("sp", lv[:, :], dfl_in[l, :, :])
            lp = sb(es, "lp", [1, 128], F32)
            for i in range(2):
                M.tt("dve", lp[:, 64 * i:64 * i + 64], lv[:, 128 * i:128 * i + 64], lv[:, 128 * i + 64:128 * i + 128], ALU.mult, part=True)
            M.op("dve", "tensor_reduce", [lt[:, 0:2]], [lp[:, :]], out=lt[:, 0:2], in_=lp[:, :].rearrange("p (a b) -> p a b", a=2),
                 axis=mybir.AxisListType.X, op=ALU.add)
            M.act(lt[:, 2:4], lt[:, 0:2], AF.Exp)
            M.tt("dve", lt[:, 4:5], lt[:, 3:4], lt[:, 2:3], ALU.subtract)
            M.ts("dve", lt[:, 5:6], lt[:, 4:5], -lam_init, ALU.add)
            M.mm(PS[7][:, 0:1], ones_f[0:1, :], lt[0:1, 5:6])
            M.copy("dve", nlam[:, :], PS[7][:, 0:1])
            M.ts("dve", ngd[:, :], pvs[:, l, 132:136], 1.0 - lam_init, ALU.mult)
            qi = 0
            items = [(h, t0, N, isctx) for h in range(4) for (t0, N, isctx) in cfg.tiles if ctx_out or not isctx]

            def ldq(ii):
                hh, tt0, NN, _ = items[ii]
                M.dma("sp", qT[ii % 2][:, :NN], fm["dfq"][hh * 128:(hh + 1) * 128, tt0:tt0 + NN])

            ldh(0); ldq(0)
            for ii, (h, t0, N, isctx) in enumerate(items):
                if True:
                    if (ii == 0 or items[ii - 1][0] != h) and h + 1 < 4:
                        ldh(h + 1)
                    kz = kzs[h % 2]; V = Vs[h % 2]
                    if ii + 1 < len(items):
                        ldq(ii + 1)
                    q = qT[qi % 2]; yo = yb[qi % 2]; qi += 1
                    kbs = list(range(cfg.NCB)) if isctx else list(range(NB))
                    O = [PS[0], PS[1]]
                    its = [(i, kb, m) for i, kb in enumerate(kbs) for m in range(2)]
                    nk = len(kbs)

                    def qk(j):
                        i, kb, m = its[j]
                        M.mm(PS[2 + j % 5][:, :N], kz[m][:, kb * 128:(kb + 1) * 128], q[:, :N])

                    def ex(j):
                        M.act(pt[j % 4][:, :N], PS[2 + j % 5][:, :N], AF.Exp)

                    def pv(j):
                        i, kb, m = its[j]
                        M.mm(O[m][:, :N], V[:, kb, :], pt[j % 4][:, :N], start=(i == 0), stop=(i == nk - 1))
                        en = "pool" if m == 0 else "dve"
                        if i == 0:
                            M.copy(en, acc[m][:, :N], pt[j % 4][:, :N])
                        else:
                            M.tt(en, acc[m][:, :N], acc[m][:, :N], pt[j % 4][:, :N], ALU.add)

                    pipeline(len(its), [qk, ex, pv], 3)
                    for m in range(2):
                        M.mm(PS[7][:, :N], ones_f[:, :], acc[m][:, :N])
                        M.recip((r1, r2)[m][:, :N], PS[7][:, :N])
                    M.tt("dve", t1[:, :N], O[0][:, :N], r1[:, :N], ALU.mult)
                    M.tt("dve", t2[:, :N], O[1][:, :N], r2[:, :N], ALU.mult)
                    M.stt(t1[:, :N], t2[:, :N], nlam[:, 0:1], t1[:, :N], ALU.mult, ALU.add)
                    M.act(t2[:, :N], t1[:, :N], AF.Square)
                    M.mm(PS[7][:, :N], ones_f[:, :], t2[:, :N])
                    M.act(r1[:, :N], PS[7][:, :N], AF.Sqrt, bias=eps_ap, scale=1.0 / 128)
                    M.recip(r1[:, :N], r1[:, :N])
                    M.stt(yo[:, :N], t1[:, :N], ngd[:, h:h + 1], r1[:, :N], ALU.mult, ALU.mult)
                    M.dma("sp", fm["dfy"][h * 128:(h + 1) * 128, t0:t0 + N], yo[:, :N])
        M.barrier()

    def na_stage(l, ctx_out):
        ROWS, G = cfg.ROWS, cfg.G
        trev = trev_all[l]
        with ExitStack() as es:
            BTs = [sb(es, "BT%d" % i, [128, 48, 512], BF16) for i in range(2)]
            kzs = [[sb(es, "kz%d_%d" % (i, jb), [128, T], BF16) for i in range(2)] for jb in range(2)]
            Vzs = [[sb(es, "Vz%d_%d" % (i, jb), [128, NB, 128], BF16) for i in range(2)] for jb in range(2)]
            oz = [sb(es, "oz%d" % i, [128, 128], BF16) for i in range(2)]
            qT = [sb(es, "qT%d" % i, [128, 512], BF16) for i in range(2)]
            pt = [sb(es, "pt%d" % i, [128, 512], BF16) for i in range(4)]
            sf = [sb(es, "sf%d" % i, [128, 512], F32) for i in range(4)]
            r1 = sb(es, "r1", [128, 512], F32)
            yb = [sb(es, "yb%d" % i, [128, 512], BF16) for i in range(2)]
            for e in range(2):
                for jb in range(2):
                    M.memset("pool", kzs[jb][e][(1 - e) * 64:(2 - e) * 64, :], 0.0)
                    M.memset("pool", Vzs[jb][e][:, :, :], 0.0)
                M.memset("pool", oz[e][:, :], 0.0); M.memset("pool", oz[e][:, e * 64:(e + 1) * 64], 1.0)

            def ldc(c):
                kz = kzs[c % 2]; Vz = Vzs[c % 2]; BT = BTs[c % 2]
                for e in range(2):
                    M.dma("sp", kz[e][e * 64:(e + 1) * 64, :], fm["nak"][c * 128 + e * 64:c * 128 + (e + 1) * 64, :], part=True)
                    M.dma("sp", Vz[e][:, :, e * 64:(e + 1) * 64],
                          tm["nav"][:, c * 128 + e * 64:c * 128 + (e + 1) * 64].rearrange("(b p) c -> p b c", p=128), part=True)
                M.memset("pool", BT[:, :, :], NEG)
                for pat in range(3):
                    g = (0, 1, G - 1)[pat]
                    rs0 = min(max(8 * g - 4, 0), ROWS - 16)
                    for e in range(2):
                        h = 2 * c + e
                        for kb in range(8):
                            for kr2 in range(2):
                                kr = rs0 + 2 * kb + kr2
                                val = [qr for qr in range(8) if 0 <= kr - min(max(8 * g + qr - 4, 0), ROWS - 8) < 8]
                                if not val:
                                    continue
                                qa, qb = val[0], val[-1] + 1
                                assert val == list(range(qa, qb))
                                ja = 7 - kr + 8 * g + qa
                                assert 0 <= ja and ja + (qb - qa) - 1 <= 14
                                src = trev[h * 15 + ja:h * 15 + ja + (qb - qa), :].rearrange("j (cp c) -> cp j c", c=64)
                                dst = BT[kr2 * 64:(kr2 + 1) * 64, (pat * 2 + e) * 8 + kb, qa * 64:qb * 64].rearrange("p (j c) -> p j c", c=64)
                                M.dma("sp", dst, src, part=True)

            qi = 0
            ldc(0)
            for c in range(4):
                if c + 1 < 4:
                    ldc(c + 1)
                kz = kzs[c % 2]; Vz = Vzs[c % 2]; BT = BTs[c % 2]
                qtiles = [(CTX + g * 512, 512, g) for g in range(G)] + ([(0, CTX, -1)] if ctx_out else [])
                for qx, (t0, N, g) in enumerate(qtiles):
                    if qx == 0 and c == 0:
                        M.dma("sp", qT[qi % 2][:, :N], fm["naq"][c * 128:(c + 1) * 128, t0:t0 + N])
                    nx = (c, qx + 1) if qx + 1 < len(qtiles) else ((c + 1, 0) if c + 1 < 4 else None)
                    if nx is not None:
                        tn, Nn, _ = qtiles[nx[1]]
                        M.dma("sp", qT[(qi + 1) % 2][:, :Nn], fm["naq"][nx[0] * 128:(nx[0] + 1) * 128, tn:tn + Nn])
                    q = qT[qi % 2]; yo = yb[qi % 2]; qi += 1
                    if g >= 0:
                        pat = 0 if g == 0 else (2 if g == G - 1 else 1)
                        rs0 = min(max(8 * g - 4, 0), ROWS - 16)
                        blks = [((CTX + rs0 * 64) // 128 + kb, kb) for kb in range(8)] + [(b, -1) for b in range(cfg.NCB)]
                    else:
                        blks = [(b, -1) for b in range(cfg.NCB)]
                    O = PS[0]; Dn = PS[1]
                    its = [(e, i, blk, kb) for e in range(2) for i, (blk, kb) in enumerate(blks)]
                    nk = len(blks)

                    def qk(j):
                        e, i, blk, kb = its[j]
                        M.mm(PS[2 + j % 6][:, :N], kz[e][:, blk * 128:(blk + 1) * 128], q[:, :N])

                    def ex(j):
                        e, i, blk, kb = its[j]
                        ps = PS[2 + j % 6]; p_ = pt[j % 4]; s_ = sf[j % 4]
                        if kb >= 0:
                            M.tt("dve", s_[:, :N], ps[:, :N], BT[:, (pat * 2 + e) * 8 + kb, :N], ALU.add)
                            M.act(p_[:, :N], s_[:, :N], AF.Exp)
                        else:
                            M.act(p_[:, :N], ps[:, :N], AF.Exp)

                    def pv(j):
                        e, i, blk, kb = its[j]
                        p_ = pt[j % 4]
                        first = (j == 0); last = (j == len(its) - 1)
                        M.mm(O[:, :N], Vz[e][:, blk, :], p_[:, :N], start=first, stop=last)
                        M.mm(Dn[:, :N], oz[e][:, :], p_[:, :N], start=first, stop=last)

                    pipeline(len(its), [qk, ex, pv], 3)
                    M.recip(r1[:, :N], Dn[:, :N])
                    M.tt("dve", yo[:, :N], O[:, :N], r1[:, :N], ALU.mult)
                    M.dma("sp", fm["nay"][c * 128:(c + 1) * 128, t0:t0 + N], yo[:, :N])
        M.barrier()

    def mlstm_stage(l, ctx_out):
        with ExitStack() as es:
            Gt = sb(es, "Gt", [128, NB, 16], F32); gbs = sb(es, "gbs", [128, 16], F32)
            LF = sb(es, "LF", [128, NB, 2, 4], F32); Bc = sb(es, "Bc", [128, NB, 2, 4], F32)
            eL = sb(es, "eL", [128, NB, 2, 4], F32); EK = sb(es, "EK", [128, NB, 2, 4], F32); EMB = sb(es, "EMB", [128, NB, 2, 4], F32)
            raw = sb(es, "raw", [128, T], BF16); cvb = sb(es, "cvb", [128, T], F32); k32 = sb(es, "k32", [128, T], F32)
            qT = sb(es, "qT", [128, T], BF16); kT = sb(es, "kT", [128, T], BF16); sigo = sb(es, "sigo", [128, T], BF16)
            Va = sb(es, "Va", [128, NB, 129], BF16); hF = sb(es, "hF", [128, NB, 128], F32); ybuf = sb(es, "ybuf", [128, T], BF16)
            CT = sb(es, "CT", [128, 129], F32); CTb = sb(es, "CTb", [128, 129], BF16); tmc = sb(es, "tmc", [128, 129], F32)
            ktd = [sb(es, "ktd%d" % i, [128, 128], BF16) for i in range(2)]
            Sm = [sb(es, "Sm%d" % i, [128, 128], BF16) for i in range(2)]
            hs = [sb(es, "hs%d" % i, [128, 128], F32) for i in range(2)]
            hn = [sb(es, "hn%d" % i, [128, 128], F32) for i in range(2)]
            smv = [sb(es, "sm%d" % i, [128, 8], F32) for i in range(2)]
            sqh = sb(es, "sqh", [128, NB, 128], F32); ssq = sb(es, "ssq", [128, NB], F32)
            M.dma("sp", Gt[:, :, :], tm["mlg"].rearrange("(b p) c -> p b c", p=128)); M.dma("sp", gbs[:, :], gb_in[l, :, :])
            for j in range(16):
                M.ts("dve", Gt[:, :, j], Gt[:, :, j], gbs[:, j:j + 1], ALU.add, part=True)
            for d in range(2):
                M.act(LF[:, :, d, :], Gt[:, :, d * 8 + 4:d * 8 + 8], AF.Exp, scale=-1.0, part=True)
            M.act(LF[:, :, :, :], LF[:, :, :, :], AF.Ln, bias=one_ap)
            M.ts("dve", LF[:, :, :, :], LF[:, :, :, :], -1.0, ALU.mult)
            M.mm(PS[0][:, 0:NB * 4], triF, LF[:, :, 0, :]); M.mm(PS[1][:, 0:NB * 4], triB, LF[:, :, 1, :])
            M.mm(PS[2][:, 0:NB * 8], ones_f[:, :], LF[:, :, :, :])
            M.act(eL[:, :, :, :], PS[2][:, 0:NB * 8].rearrange("p (b d h) -> p b d h", d=2, h=4), AF.Exp)
            for d in range(2):
                pv_ = PS[d][:, 0:NB * 4].rearrange("p (b h) -> p b h", h=4)
                M.copy("dve", Bc[:, :, d, :], pv_, part=True)
                M.tt("dve", EK[:, :, d, :], Gt[:, :, d * 8:d * 8 + 4], pv_, ALU.subtract, part=True)
            M.act(EK[:, :, :, :], EK[:, :, :, :], AF.Exp)
            M.act(EMB[:, :, :, :], Bc[:, :, :, :], AF.Exp, scale=-1.0)
            segs = ((0, CTX), (CTX, T))
            for h in range(4):
                for (name, chunk, isq) in (("mlq", h, True), ("mlk", 4 + h, False)):
                    M.dma("sp", raw[:, :], fm[name][h * 128:(h + 1) * 128, :])
                    cwv = lambda j: pvs[:, l, 96 + j * 8 + chunk:96 + j * 8 + chunk + 1]
                    for (a, b) in segs:
                        M.ts("dve", cvb[:, a:b], raw[:, a:b], cwv(1), ALU.mult, pvs[:, l, 120 + chunk:121 + chunk], ALU.add, part=True)
                        M.stt(cvb[:, a + 1:b], raw[:, a:b - 1], cwv(0), cvb[:, a + 1:b], ALU.mult, ALU.add, part=True)
                        M.stt(cvb[:, a:b - 1], raw[:, a + 1:b], cwv(2), cvb[:, a:b - 1], ALU.mult, ALU.add, part=True)
                    if isq:
                        M.act(k32[:, :], cvb[:, :], AF.Silu)
                        M.ts("dve", qT[:, :], k32[:, :], 128.0 ** -0.5, ALU.mult)
                    else:
                        M.act(k32[:, :], cvb[:, :], AF.Silu)
                        M.copy("pool", kT[:, :], k32[:, :])
                M.dma("sp", raw[:, :], fm["mlo"][h * 128:(h + 1) * 128, :])
                M.act(sigo[:, :], raw[:, :], AF.Sigmoid)
                M.dma("sp", Va[:, :, 0:128], tm["mlv"][:, h * 128:(h + 1) * 128].rearrange("(b p) c -> p b c", p=128))
                M.memset("pool", Va[:, :, 128:129], 1.0, part=True)
                for d in range(2):
                    order = list(range(NB)) if d == 0 else (list(range(cfg.NCB - 1, -1, -1)) + list(range(NB - 1, cfg.NCB - 1, -1)))
                    tri = triF if d == 0 else triB
                    M.memset("pool", CT[:, :], 0.0); M.memset("pool", CTb[:, :], 0.0)

                    def stA(j, d=d, order=order, tri=tri):
                        blk = order[j]; cs = slice(blk * 128, (blk + 1) * 128)
                        ek = EK[:, blk, d, h:h + 1]
                        pk = PS[j % 2]; pU = PS[2 + j % 2]
                        M.tr(pk[:, 0:128], k32[:, cs], ident)
                        M.ts("dve", ktd[j % 2][:, :], pk[:, 0:128], ek, ALU.mult)
                        M.mm(pU[:, 0:129], ktd[j % 2][:, :], Va[:, blk, :])

                    def stB(j, d=d, order=order, tri=tri):
                        blk = order[j]; cs = slice(blk * 128, (blk + 1) * 128)
                        pO = PS[5 + j % 2]; pU = PS[2 + j % 2]; pT = PS[7]; pS = PS[4]
                        hs_ = hs[j % 2]; hn_ = hn[j % 2]; sm_ = smv[j % 2]
                        M.mm(pS[:, 0:128], kT[:, cs], qT[:, cs])
                        M.stt(Sm[j % 2][:, :], pS[:, 0:128], EK[:, blk, d, h:h + 1], tri, ALU.mult, ALU.mult)
                        M.mm(pO[:, 0:129], Sm[j % 2][:, :], Va[:, blk, :], start=True, stop=False)
                        M.mm(pO[:, 0:129], qT[:, cs], CTb[:, :], start=False, stop=True)
                        M.tt("dve", tmc[:, :], pU[:, 0:129], CT[:, :], ALU.add)
                        M.ts("dve", CT[:, :], tmc[:, :], eL[:, blk, d, h:h + 1], ALU.mult)
                        M.copy("pool", CTb[:, :], CT[:, :])
                        M.ts("dve", sm_[:, 0:1], pO[:, 128:129], -1.0, ALU.mult, EMB[:, blk, d, h:h + 1], ALU.max)
                        M.tt("dve", sm_[:, 0:1], sm_[:, 0:1], pO[:, 128:129], ALU.max)
                        M.recip(sm_[:, 1:2], sm_[:, 0:1])
                        if d == 0:
                            M.ts("dve", hF[:, blk, :], pO[:, 0:128], sm_[:, 1:2], ALU.mult, part=True)
                        else:
                            M.stt(hF[:, blk, :], pO[:, 0:128], sm_[:, 1:2], hF[:, blk, :], ALU.mult, ALU.add, part=True)

                    pipeline(NB, [stA, stB], 1)
                b0 = 0 if ctx_out else cfg.NCB
                M.act(sqh[:, b0:NB, :], hF[:, b0:NB, :], AF.Square)
                M.op("dve", "tensor_reduce", [ssq[:, b0:NB]], [sqh[:, b0:NB, :]], out=ssq[:, b0:NB], in_=sqh[:, b0:NB, :],
                     axis=mybir.AxisListType.X, op=ALU.add)
                M.act(ssq[:, b0:NB], ssq[:, b0:NB], AF.Sqrt, bias=eps_ap, scale=1.0 / 128)
                M.recip(ssq[:, b0:NB], ssq[:, b0:NB])
                for blk in range(b0, NB):
                    cs = slice(blk * 128, (blk + 1) * 128)
                    hn_ = hn[blk % 2]; pT = PS[blk % 2]
                    M.ts("dve", hn_[:, :], hF[:, blk, :], ssq[:, blk:blk + 1], ALU.mult)
                    M.tr(pT[:, 0:128], hn_[:, :], ident)
                    M.stt(ybuf[:, cs], pT[:, 0:128], pvs[:, l, 128 + h:129 + h], sigo[:, cs], ALU.mult, ALU.mult, part=True)
                a0 = 0 if ctx_out else CTX
                M.dma("sp", fm["mly"][h * 128:(h + 1) * 128, a0:T], ybuf[:, a0:T])
        M.barrier()

    def merge_stage(l, tiles):
        with ExitStack() as es:
            Wb = sb(es, "Wb", [128, 12, D], BF16); Wo = sb(es, "Wo", [128, 8, D], BF16)
            xTs = [sb(es, "xT%d" % i, [128, 8, 512], F32) for i in range(2)]
            brs = [sb(es, "br%d" % i, [128, 12, 512], BF16) for i in range(2)]
            gts = [sb(es, "gt%d" % i, [128, 24, 512], BF16) for i in range(2)]
            yT = sb(es, "yT", [128, 8, 512], BF16)

            def ldt(ix):
                t0, N, isctx = tiles[ix]
                M.dma("sp", xTs[ix % 2][:, :, 0:N], xTd[:, :, t0:t0 + N])
                for i, nm in enumerate(("mly", "dfy", "nay")):
                    M.dma("sp", brs[ix % 2][:, i * 4:(i + 1) * 4, 0:N], fm[nm][:, t0:t0 + N].rearrange("(c p) t -> p c t", p=128), part=True)
                M.dma("sp", gts[ix % 2][:, :, 0:N], fm["gate"][:, t0:t0 + N].rearrange("(c p) t -> p c t", p=128))
            ta = [sb(es, "ta%d" % i, [128, 512], F32) for i in range(2)]; tb_ = [sb(es, "tb%d" % i, [128, 512], F32) for i in range(2)]
            wbv = w_br[l].rearrange("i (k p) n -> p i k n", p=128); wov = w_out[l].rearrange("(k p) n -> p k n", p=128)
            for i in range(3):
                for k in range(4):
                    M.dma("pool", Wb[:, i * 4 + k, :], wbv[:, i, k, :], part=True)
            for k in range(8):
                M.dma("pool", Wo[:, k, :], wov[:, k, :], part=True)
            ldt(0)
            for ix, (t0, N, isctx) in enumerate(tiles):
                ci = 1 if isctx else 0
                if ix + 1 < len(tiles):
                    ldt(ix + 1)
                xT = xTs[ix % 2]; br = brs[ix % 2]; gt = gts[ix % 2]
                M.act(gt[:, :, 0:N], gt[:, :, 0:N], AF.Sigmoid)
                for m in range(8):
                    a_ = ta[m % 2]; b_ = tb_[m % 2]
                    for i in range(3):
                        pp = PS[(m % 2) * 3 + i]
                        for k in range(4):
                            M.mm(pp[:, :N], Wb[:, i * 4 + k, m * 128:(m + 1) * 128], br[:, i * 4 + k, :N], start=(k == 0), stop=(k == 3))
                    M.tt("dve", a_[:, :N], PS[(m % 2) * 3][:, :N], gt[:, m, :N], ALU.mult)
                    M.tt("dve", b_[:, :N], PS[(m % 2) * 3 + 1][:, :N], gt[:, 8 + m, :N], ALU.mult)
                    M.tt("pool", a_[:, :N], a_[:, :N], b_[:, :N], ALU.add)
                    M.tt("dve", b_[:, :N], PS[(m % 2) * 3 + 2][:, :N], gt[:, 16 + m, :N], ALU.mult)
                    M.tt("pool", yT[:, m, :N], a_[:, :N], b_[:, :N], ALU.add, part=True)
                g5 = P(l, ci, 5)
                for m2 in range(8):
                    po = PS[6 + m2 % 2]
                    for m in range(8):
                        M.mm(po[:, :N], Wo[:, m, m2 * 128:(m2 + 1) * 128], yT[:, m, :N], start=(m == 0), stop=(m == 7))
                    M.stt(xT[:, m2, :N], po[:, :N], g5[:, m2:m2 + 1], xT[:, m2, :N], ALU.mult, ALU.add, part=True)
                M.dma("sp", xTd[:, :, t0:t0 + N], xT[:, :, 0:N])
        M.barrier()

    def final_stage():
        with ExitStack() as es:
            xT = sb(es, "xT", [128, 8, 512], F32); xn = sb(es, "xn", [128, 8, 512], F32)
            sqb = [sb(es, "sq%d" % i, [128, 512], F32) for i in range(2)]
            rstd = sb(es, "rstd", [128, 512], F32)
            ob = [sb(es, "ob%d" % i, [128, D], F32) for i in range(2)]
            oi = 0
            for (t0, N, isctx) in cfg.tiles:
                if isctx:
                    continue
                M.dma("sp", xT[:, :, 0:N], xTd[:, :, t0:t0 + N])
                for k in range(8):
                    M.act(sqb[k % 2][:, :N], xT[:, k, :N], AF.Square)
                    M.mm(PS[7][:, :N], ones_f[:, :], sqb[k % 2][:, :N], start=(k == 0), stop=(k == 7))
                M.act(rstd[:, :N], PS[7][:, :N], AF.Sqrt, bias=eps_ap, scale=1.0 / D)
                M.recip(rstd[:, :N], rstd[:, :N])
                for k in range(8):
                    M.stt(xn[:, k, :N], xT[:, k, :N], cvs[:, 16 + k:17 + k], rstd[:, :N], ALU.mult, ALU.mult, part=True)
                for tb in range(N // 128):
                    o_ = ob[oi % 2]
                    for k in range(8):
                        pb = PS[(oi % 2) * 2 + k // 4]
                        M.tr(pb[:, (k % 4) * 128:(k % 4 + 1) * 128], xn[:, k, tb * 128:(tb + 1) * 128], ident)
                    M.copy("dve", o_[:, 0:512], PS[(oi % 2) * 2][:, :], part=True)
                    M.copy("act", o_[:, 512:1024], PS[(oi % 2) * 2 + 1][:, :], part=True)
                    r0 = t0 - CTX + tb * 128
                    M.dma("sp", y_out[r0:r0 + 128, :], o_[:, :])
                    oi += 1
        M.barrier()

    for l in range(DEPTH):
        ctx_out = l < DEPTH - 1
        ffn_stage(l, 0, cfg.tiles)
        mixin_stage(l)
        mlstm_stage(l, ctx_out)
        diff_stage(l, ctx_out)
        na_stage(l, ctx_out)
        t2 = cfg.tiles if ctx_out else [t for t in cfg.tiles if not t[2]]
        merge_stage(l, t2)
        ffn_stage(l, 1, t2)
    final_stage()
    gs_.close()
    return nc, M


def host_consts(cfg):
    S = cfg.S
    cmat = np.zeros((128, 3, 128), np.float32)
    i = np.arange(128)
    cmat[:, 0, :] = (i[:, None] == i[None, :]); cmat[:, 1, :] = (i[:, None] <= i[None, :]); cmat[:, 2, :] = (i[:, None] >= i[None, :])
    t = np.arange(S); row = (t // 64).astype(np.float32); col = (t % 64).astype(np.float32)
    freqs = (10000.0 ** (-np.arange(0, 32, 2, dtype=np.float32) / 32)).astype(np.float32)
    ang = np.concatenate([row[:, None] * freqs, col[:, None] * freqs], -1).astype(np.float32)
    rope = np.zeros((2, 128, S), np.float32)
    rope[0] = np.tile(np.cos(ang).T, (4, 1)); rope[1] = np.tile(np.sin(ang).T, (4, 1))
    ind = np.zeros((32, 64, 64), np.float32)
    cp = np.arange(64)[:, None]; c = np.arange(64)[None, :]
    for d in range(31):
        ind[d] = (cp - c + 15 == d)
    qstart = np.clip(c - 8, 0, 48)
    ind[31] = np.where((cp >= qstart) & (cp < qstart + 16), 0.0, NEG)
    return cmat, rope, ind.reshape(32, 4096)


def make_in_maps(cfg, inp):
    cmat, rope, ind = host_consts(cfg)
    DEPTH = cfg.DEPTH
    f = lambda a: np.ascontiguousarray(np.asarray(a, np.float32))
    pv = np.zeros((DEPTH, 128, 136), np.float32)
    for l in range(DEPTH):
        pv[l, :, 0:24] = inp["norm_g"][l].reshape(24, 128).T
        pv[l, :, 24:96] = inp["b_ada"][l].reshape(72, 128).T
        pv[l, :, 96:120] = inp["ml_conv_w"][l].reshape(24, 128).T
        pv[l, :, 120:128] = inp["ml_conv_b"][l].reshape(8, 128).T
        pv[l, :, 128:132] = inp["ml_norm_g"][l].reshape(4, 128).T
        pv[l, :, 132:136] = inp["df_norm_g"][l].reshape(4, 128).T
    gb = np.broadcast_to(np.asarray(inp["ml_gate_b"], np.float32)[:, None, :], (DEPTH, 128, 16))
    rbT = np.ones((DEPTH, 32, 120), np.float32)
    rb = np.asarray(inp["na_rel_bias"], np.float32)
    rbT[:, 0:31, :] = rb[:, :, ::-1, :].reshape(DEPTH, 120, 31).transpose(0, 2, 1)
    shared = {"w_ada": f(inp["w_ada"]), "pv": pv, "gb": f(gb), "ffn_w1": f(inp["ffn_w1"]), "ffn_w2": f(inp["ffn_w2"]),
              "w_in": f(inp["w_in"]), "dfl": f(np.asarray(inp["df_lambda"]).reshape(DEPTH, 1, 256)), "rbT": f(rbT),
              "w_branch": f(inp["w_branch"]), "w_out": f(inp["w_out"]), "cmat": cmat, "rope": rope, "ind": ind}
    maps = []
    for b in range(cfg.NC):
        cv = np.zeros((128, 24), np.float32)
        cv[:, 0:8] = np.asarray(inp["c"][b]).reshape(8, 128).T
        cv[:, 8:16] = np.asarray(inp["c_ctx"]).reshape(8, 128).T
        cv[:, 16:24] = np.asarray(inp["final_g"]).reshape(8, 128).T
        m = dict(shared); m["x"] = f(inp["x"][b]); m["ctx"] = f(inp["ctx"][b]); m["cv"] = cv
        maps.append(m)
    return maps


def kernel(**inputs):
    cfg = Cfg()
    nc, M = build(cfg)
    maps = make_in_maps(cfg, inputs)
    res = run_bass_kernel_spmd(nc, maps, core_ids=list(range(cfg.NC)))
    return np.stack([np.asarray(r["y"], np.float32) for r in res.results], 0)
```

```python
import math
from contextlib import ExitStack
import numpy as np
import concourse.bass as bass
import concourse.mybir as mybir
from concourse.bass_utils import run_bass_kernel_spmd

F32 = mybir.dt.float32
BF16 = mybir.dt.bfloat16
AF = mybir.ActivationFunctionType
ALU = mybir.AluOpType
KQ = 8
EPS = 1e-6
NEG = -30000.0


class Cfg:
    def __init__(s, rows=64, ctx=256, depth=4, dff=2816, ncores=8):
        s.ROWS = rows; s.GW = 64; s.S = rows * 64; s.CTX = ctx; s.T = s.S + ctx
        s.DEPTH = depth; s.DFF = dff; s.NJ = dff // 128; s.D = 1024; s.NB = s.T // 128
        s.NCB = ctx // 128; s.G = rows // 8; s.DIN = 5136 + 3072; s.NC = ncores
        s.tiles = [(0, ctx, True)] + [(ctx + i * 512, 512, False) for i in range(s.S // 512)]


class Buf:
    __slots__ = ("w", "r", "wf")

    def __init__(s):
        s.w = {}; s.r = {}; s.wf = {}


def _merge(d, e):
    for k, v in e.items():
        if d.get(k, 0) < v:
            d[k] = v


class Mach:
    def __init__(s, nc):
        s.nc = nc; s.es = ExitStack(); s.sems = []; s.bufs = {}; s.eng = {}
        for nm, h in (("pe", nc.tensor), ("act", nc.scalar), ("dve", nc.vector), ("pool", nc.gpsimd), ("sp", nc.sync)):
            s.eng[nm] = {"h": h, "si": s.newsem("e_" + nm), "cnt": 0, "seen": {}}
        s.dq = {q: {"sis": [s.newsem("d_%s%d" % (q, i)) for i in range(KQ)], "n": 0} for q in ("sp", "pool")}
        s.ninst = 0

    def newsem(s, name):
        s.sems.append(s.es.enter_context(s.nc.semaphore(name)))
        return len(s.sems) - 1

    def buf(s, ap):
        k = ap.tensor.name
        b = s.bufs.get(k)
        if b is None:
            b = s.bufs[k] = Buf()
        return b

    def wait(s, en, evs):
        E = s.eng[en]
        for si, v in evs.items():
            if en == "pe" and si == E["si"]:
                continue
            if E["seen"].get(si, 0) < v:
                E["h"].wait_ge(s.sems[si], v); E["seen"][si] = v; s.ninst += 1

    def _deps(s, outs, ins, part):
        evs = {}
        for ap in ins:
            _merge(evs, s.buf(ap).w)
        for ap in outs:
            b = s.buf(ap); _merge(evs, b.r)
            _merge(evs, b.wf if part else b.w)
        return evs

    def _rec(s, outs, ins, ev, part=False):
        for ap in ins:
            _merge(s.buf(ap).r, ev)
        for ap in outs:
            _merge(s.buf(ap).w, ev)
            if not part:
                _merge(s.buf(ap).wf, ev)

    def op(s, en, name, outs, ins, *a, part=False, **kw):
        E = s.eng[en]
        s.wait(en, s._deps(outs, ins, part))
        inst = getattr(E["h"], name)(*a, **kw)
        E["cnt"] += 1; s.ninst += 1
        inst.then_inc(s.sems[E["si"]], 1)
        s._rec(outs, ins, {E["si"]: E["cnt"]}, part)

    def dma(s, q, out, in_, part=False, **kw):
        Q = s.dq[q]; slot = Q["n"] % KQ; gen = Q["n"] // KQ; si = Q["sis"][slot]
        evs = s._deps([out], [in_], part)
        if gen > 0:
            _merge(evs, {si: 16 * gen})
        s.wait(q, evs)
        s.eng[q]["h"].dma_start(out=out, in_=in_, **kw).then_inc(s.sems[si], 16)
        Q["n"] += 1; s.ninst += 1
        s._rec([out], [in_], {si: 16 * (gen + 1)}, part)

    def barrier(s):
        evs = {E["si"]: E["cnt"] for E in s.eng.values() if E["cnt"] > 0}
        for Q in s.dq.values():
            for i, si in enumerate(Q["sis"]):
                n = (Q["n"] - i + KQ - 1) // KQ
                if n > 0:
                    evs[si] = 16 * n
        for en in s.eng:
            s.wait(en, evs)

    def mm(s, out, lhsT, rhs, start=True, stop=True):
        s.op("pe", "matmul", [out], [lhsT, rhs], out, lhsT=lhsT, rhs=rhs, start=start, stop=stop)

    def tr(s, out, in_, ident):
        s.op("pe", "transpose", [out], [in_, ident], out, in_, ident)

    def act(s, out, in_, func, bias=None, scale=None, accum_out=None, part=False):
        kw = {}; ins = [in_]; outs = [out]
        if bias is not None:
            kw["bias"] = bias
            if not isinstance(bias, float):
                ins.append(bias)
        if scale is not None:
            kw["scale"] = scale
            if not isinstance(scale, float):
                ins.append(scale)
        if accum_out is not None:
            kw["accum_out"] = accum_out; outs.append(accum_out)
        s.op("act", "activation", outs, ins, out=out, in_=in_, func=func, part=part, **kw)

    def tt(s, en, out, in0, in1, op, part=False):
        s.op(en, "tensor_tensor", [out], [in0, in1], out=out, in0=in0, in1=in1, op=op, part=part)

    def ts(s, en, out, in0, s1, op0, s2=None, op1=None, part=False):
        ins = [in0] + [x for x in (s1, s2) if x is not None and not isinstance(x, float)]
        kw = {"op1": op1} if op1 is not None else {}
        s.op(en, "tensor_scalar", [out], ins, out=out, in0=in0, scalar1=s1, scalar2=s2, op0=op0, part=part, **kw)

    def stt(s, out, in0, scalar, in1, op0, op1, part=False):
        ins = [in0, in1] + ([] if isinstance(scalar, float) else [scalar])
        s.op("dve", "scalar_tensor_tensor", [out], ins, out=out, in0=in0, scalar=scalar, in1=in1, op0=op0, op1=op1, part=part)

    def copy(s, en, out, in_, part=False):
        if en == "act":
            s.op("act", "activation", [out], [in_], out=out, in_=in_, func=AF.Copy, part=part)
        else:
            s.op(en, "tensor_copy", [out], [in_], out=out, in_=in_, part=part)

    def memset(s, en, ap, val, part=False):
        s.op(en, "memset", [ap], [], ap, val, part=part)

    def recip(s, out, in_, part=False):
        s.op("dve", "reciprocal", [out], [in_], out=out, in_=in_, part=part)


def pipeline(n, stages, look):
    for j in range(min(look, n)):
        stages[0](j)
    for j in range(n):
        if j + look < n:
            stages[0](j + look)
        for f in stages[1:]:
            f(j)


def build(cfg):
    nc = bass.Bass("TRN2", target_bir_lowering=False)
    T, S, CTX, D, DFF, NJ, NB, DEPTH = cfg.T, cfg.S, cfg.CTX, cfg.D, cfg.DFF, cfg.NJ, cfg.NB, cfg.DEPTH

    def din(name, shape, dt=F32):
        return nc.dram_tensor(name, list(shape), dt, kind="ExternalInput").ap()

    def dscr(name, shape, dt):
        return nc.dram_tensor(name, list(shape), dt, kind="Internal").ap()

    x_in = din("x", [S, D]); ctx_in = din("ctx", [CTX, D]); cv_in = din("cv", [128, 24])
    w_ada = din("w_ada", [DEPTH, D, 9 * D]); pv_in = din("pv", [DEPTH, 128, 136]); gb_in = din("gb", [DEPTH, 128, 16])
    ffn_w1 = din("ffn_w1", [DEPTH, 2, D, 2 * DFF]); ffn_w2 = din("ffn_w2", [DEPTH, 2, DFF, D])
    w_in = din("w_in", [DEPTH, D, cfg.DIN]); dfl_in = din("dfl", [DEPTH, 1, 256]); rbT_in = din("rbT", [DEPTH, 32, 120])
    w_br = din("w_branch", [DEPTH, 3, 512, D]); w_out = din("w_out", [DEPTH, D, D])
    cmat_in = din("cmat", [128, 3, 128]); rope_in = din("rope", [2, 128, S]); ind_in = din("ind", [32, 4096])
    y_out = nc.dram_tensor("y", [S, D], F32, kind="ExternalOutput").ap()

    xTd = dscr("xTd", [D, T], F32).rearrange("(k p) t -> p k t", p=128)
    fm = {n: dscr("fm_" + n, [r, T], BF16) for n, r in (("mlq", 512), ("mlk", 512), ("mlo", 512), ("dfq", 512), ("dfk", 512),
                                                        ("naq", 512), ("nak", 512), ("gate", 3072), ("mly", 512), ("dfy", 512), ("nay", 512))}
    tm = {n: dscr("tm_" + n, [T, c], dt) for n, c, dt in (("mlv", 512, BF16), ("dfv", 512, BF16), ("nav", 512, BF16), ("mlg", 16, F32))}
    trev_all = dscr("trev", [DEPTH, 120, 4096], BF16)

    M = Mach(nc)
    gs_ = ExitStack()

    _nm = [0]

    def sb(es, name, shape, dt):
        _nm[0] += 1
        return es.enter_context(nc.sbuf_tensor("%s_%d" % (name, _nm[0]), list(shape), dt))

    PS = [gs_.enter_context(nc.psum_tensor("ps%d" % i, [128, 512], F32)) for i in range(8)]
    cm = sb(gs_, "cmat", [128, 3, 128], F32)
    ones_f = sb(gs_, "ones_f", [128, 128], F32); ones_b = sb(gs_, "ones_b", [128, 128], BF16)
    epsc = sb(gs_, "epsc", [128, 2], F32)
    cvs = sb(gs_, "cvs", [128, 24], F32)
    prm = sb(gs_, "prm", [128, DEPTH * 2 * 9 * 8], F32)
    pvs = sb(gs_, "pvs", [128, DEPTH, 136], F32)
    ident = cm[:, 0, :]; triF = cm[:, 1, :]; triB = cm[:, 2, :]
    eps_ap = epsc[:, 0:1]; one_ap = epsc[:, 1:2]

    def P(l, ci, j):
        o = ((l * 2 + ci) * 9 + j) * 8
        return prm[:, o:o + 8]

    M.dma("sp", cm[:, :, :], cmat_in[:, :, :]); M.dma("sp", cvs[:, :], cv_in[:, :])
    for l in range(DEPTH):
        M.dma("sp", pvs[:, l, :], pv_in[l, :, :], part=True)
    M.memset("dve", ones_f[:, :], 1.0); M.memset("dve", ones_b[:, :], 1.0)
    M.memset("dve", epsc[:, 0:1], EPS); M.memset("dve", epsc[:, 1:2], 1.0, part=True)
    with ExitStack() as es:
        scT = sb(es, "scT", [128, 8, 2], F32)
        wa = [sb(es, "wa%d" % i, [128, 8, 1024], F32) for i in range(3)]
        mods = sb(es, "mods", [128, 72, 2], F32)
        for i in range(2):
            M.act(scT[:, :, i], cvs[:, i * 8:(i + 1) * 8], AF.Silu, part=True)
        it = 0
        for l in range(DEPTH):
            wv = w_ada[l].rearrange("(k p) n -> p k n", p=128)
            pm = PS[l % 2]
            for grp in range(9):
                w = wa[it % 3]; it += 1
                for k in range(8):
                    M.dma("sp" if k % 2 else "pool", w[:, k, :], wv[:, k, grp * 1024:(grp + 1) * 1024], part=True)
                for fc in range(8):
                    c0 = (grp * 8 + fc) * 2
                    for k in range(8):
                        M.mm(pm[:, c0:c0 + 2], w[:, k, fc * 128:(fc + 1) * 128], scT[:, k, :], start=(k == 0), stop=(k == 7))
            pmv = pm[:, 0:144].rearrange("p (j i) -> p j i", i=2)
            for i in range(2):
                M.tt("dve", mods[:, :, i], pmv[:, :, i], pvs[:, l, 24:96], ALU.add, part=True)
            for ci in range(2):
                for sub in range(3):
                    mo = sub * 24
                    ng = pvs[:, l, sub * 8:(sub + 1) * 8]
                    M.stt(P(l, ci, sub * 3 + 0), mods[:, mo + 8:mo + 16, ci], 1.0, ng, ALU.add, ALU.mult, part=True)
                    M.copy("dve", P(l, ci, sub * 3 + 1), mods[:, mo:mo + 8, ci], part=True)
                    M.ts("dve", P(l, ci, sub * 3 + 2), mods[:, mo + 16:mo + 24, ci], 0.5 if sub != 1 else 1.0, ALU.mult, part=True)
    M.barrier()
    with ExitStack() as es:
        Ind = sb(es, "Ind", [32, 4096], F32)
        At = [sb(es, "A%d" % i, [32, 120], F32) for i in range(2)]
        Tsb = [sb(es, "Tsb%d" % i, [128, 4096], BF16) for i in range(2)]
        M.dma("sp", Ind[:, :], ind_in[:, :])
        for l in range(DEPTH):
            A = At[l % 2]; Tb = Tsb[l % 2]
            M.dma("sp", A[:, :], rbT_in[l, :, :])
            for cc in range(8):
                ps = PS[2 + cc % 2]
                M.mm(ps[0:120, :], A[:, :], Ind[:, cc * 512:(cc + 1) * 512])
                M.copy("dve" if cc % 2 else "act", Tb[0:120, cc * 512:(cc + 1) * 512], ps[0:120, :], part=True)
            M.dma("sp", trev_all[l, :, :], Tb[0:120, :])
    M.barrier()

    with ExitStack() as es:
        xin = [sb(es, "xin%d" % i, [128, D], F32) for i in range(2)]
        xtb = [sb(es, "xtb%d" % i, [128, 8, 128], F32) for i in range(2)]
        for b in range(NB):
            src = ctx_in[b * 128:(b + 1) * 128, :] if b < cfg.NCB else x_in[(b - cfg.NCB) * 128:(b - cfg.NCB + 1) * 128, :]
            xi = xin[b % 2]; xo = xtb[b % 2]
            M.dma("sp", xi[:, :], src)
            for k in range(8):
                pb = PS[(b % 2) * 2 + k // 4]
                M.tr(pb[:, (k % 4) * 128:(k % 4 + 1) * 128], xi[:, k * 128:(k + 1) * 128], ident)
            M.copy("dve", xo[:, 0:4, :], PS[(b % 2) * 2][:, :].rearrange("p (k t) -> p k t", k=4), part=True)
            M.copy("act", xo[:, 4:8, :], PS[(b % 2) * 2 + 1][:, :].rearrange("p (k t) -> p k t", k=4), part=True)
            M.dma("sp", xTd[:, :, b * 128:(b + 1) * 128], xo[:, :, :])
    M.barrier()

    def rms_mod(xT, hT, N, gsv, shv, sqb, rstd, tmpb):
        pss = PS[7]
        if not callable(xT):
            xt_ = xT
            xT = lambda k: xt_[:, k, :N]
        for k in range(8):
            s_ = sqb[k % 2]
            M.act(s_[:, :N], xT(k), AF.Square)
            M.mm(pss[:, :N], ones_f[:, :], s_[:, :N], start=(k == 0), stop=(k == 7))
        M.act(rstd[:, :N], pss[:, :N], AF.Sqrt, bias=eps_ap, scale=1.0 / D)
        M.recip(rstd[:, :N], rstd[:, :N])
        for k in range(8):
            t_ = tmpb[k % 2]
            M.stt(t_[:, :N], xT(k), gsv[:, k:k + 1], rstd[:, :N], ALU.mult, ALU.mult)
            M.act(hT[:, k, :N], t_[:, :N], AF.Identity, bias=shv[:, k:k + 1], part=True)

    def ffn_stage(l, j, tiles):
        sub = 0 if j == 0 else 2
        with ExitStack() as es:
            NG = min(4, NJ)
            gsz = [NJ // NG + (1 if g < NJ % NG else 0) for g in range(NG)]
            gj0 = [sum(gsz[:g]) for g in range(NG)]
            W1g = [sb(es, "W1g%d" % g, [128, 8, 2, gsz[g] * 128], BF16) for g in range(NG)]
            jg = [(g, jj - gj0[g]) for g in range(NG) for jj in range(gj0[g], gj0[g] + gsz[g])]
            W2 = sb(es, "W2", [128, NJ, D], BF16)
            xTk = [sb(es, "xTk%d" % k, [128, 512], F32) for k in range(8)]
            hT = sb(es, "hT", [128, 8, 512], BF16); uT = sb(es, "uT", [128, NJ, 512], BF16)
            sqb = [sb(es, "sq%d" % i, [128, 512], F32) for i in range(2)]
            rstd = sb(es, "rstd", [128, 512], F32)
            w1v = ffn_w1[l, j].rearrange("(k p) n -> p k n", p=128); w2v = ffn_w2[l, j].rearrange("(k p) n -> p k n", p=128)
            for g in range(NG):
                for k in range(8):
                    for ab in range(2):
                        c0 = ab * DFF + gj0[g] * 128
                        M.dma("pool", W1g[g][:, k, ab, :], w1v[:, k, c0:c0 + gsz[g] * 128], part=True)
            for k in range(NJ):
                M.dma("pool", W2[:, k, :], w2v[:, k, :], part=True)
            for k in range(8):
                M.dma("sp", xTk[k][:, 0:tiles[0][1]], xTd[:, k, tiles[0][0]:tiles[0][0] + tiles[0][1]])
            for ix, (t0, N, isctx) in enumerate(tiles):
                ci = 1 if isctx else 0
                rms_mod(lambda k: xTk[k][:, :N], hT, N, P(l, ci, sub * 3), P(l, ci, sub * 3 + 1), sqb, rstd, sqb)
                for jj in range(NJ):
                    pa = PS[(jj % 2) * 2]; pb = PS[(jj % 2) * 2 + 1]; sl = sqb[jj % 2]
                    g_, jo = jg[jj]
                    for k in range(8):
                        M.mm(pa[:, :N], W1g[g_][:, k, 0, jo * 128:(jo + 1) * 128], hT[:, k, :N], start=(k == 0), stop=(k == 7))
                    for k in range(8):
                        M.mm(pb[:, :N], W1g[g_][:, k, 1, jo * 128:(jo + 1) * 128], hT[:, k, :N], start=(k == 0), stop=(k == 7))
                    M.act(sl[:, :N], pa[:, :N], AF.Silu)
                    M.tt("dve", uT[:, jj, :N], sl[:, :N], pb[:, :N], ALU.mult, part=True)
                hg = P(l, ci, sub * 3 + 2)
                for m in range(8):
                    po = PS[4 + m % 2]
                    for jj in range(NJ):
                        M.mm(po[:, :N], W2[:, jj, m * 128:(m + 1) * 128], uT[:, jj, :N], start=(jj == 0), stop=(jj == NJ - 1))
                    M.stt(xTk[m][:, :N], po[:, :N], hg[:, m:m + 1], xTk[m][:, :N], ALU.mult, ALU.add)
                    M.dma("sp", xTd[:, m, t0:t0 + N], xTk[m][:, 0:N])
                    if ix + 1 < len(tiles):
                        tn, Nn, _ = tiles[ix + 1]
                        M.dma("sp", xTk[m][:, 0:Nn], xTd[:, m, tn:tn + Nn])
        M.barrier()

    FMG = [("mlq", 0, 4, None), ("mlk", 512, 4, None), ("mlo", 1536, 4, None), ("dfq", 2064, 4, 0.125), ("dfk", 2576, 4, None),
           ("naq", 3600, 4, 0.125), ("nak", 4112, 4, None), ("gate", 5136, 24, None)]
    TMG = [("mlv", 1024, 512), ("dfv", 3088, 512), ("nav", 4624, 512), ("mlg", 2048, 16)]

    def mixin_stage(l):
        with ExitStack() as es:
            W = sb(es, "Win", [128, 8, cfg.DIN], BF16); Wr = sb(es, "Wrot", [128, 8, 1024], BF16)
            xT = sb(es, "xT", [128, 8, 512], F32); hT = sb(es, "hT", [128, 8, 512], BF16)
            sqb = [sb(es, "sq%d" % i, [128, 512], F32) for i in range(2)]
            rstd = sb(es, "rstd", [128, 512], F32)
            stg = [sb(es, "stg%d" % i, [128, 4, 512], BF16) for i in range(2)]
            stt_ = [sb(es, "stt%d" % i, [128, 512], BF16) for i in range(2)]
            stf = sb(es, "stf", [128, 16], F32)
            rC = sb(es, "rC", [128, 512], F32); rS = sb(es, "rS", [128, 512], F32)
            wv = w_in[l].rearrange("(k p) n -> p k n", p=128)
            cw = cfg.DIN // 6
            for k in range(8):
                for c in range(6):
                    M.dma("pool", W[:, k, c * cw:(c + 1) * cw], wv[:, k, c * cw:(c + 1) * cw], part=True)
            for k in range(8):
                sv = W[:, k, 2064:3088].rearrange("p (b h d) -> p b h d", b=16, h=2, d=32)
                dv = Wr[:, k, :].rearrange("p (b h d) -> p b h d", b=16, h=2, d=32)
                M.ts("dve", dv[:, :, 0, :], sv[:, :, 1, :], -1.0, ALU.mult, part=True)
                M.copy("pool", dv[:, :, 1, :], sv[:, :, 0, :], part=True)
            si = 0; ti = 0; ev = 0
            M.dma("sp", xT[:, :, 0:cfg.tiles[0][1]], xTd[:, :, cfg.tiles[0][0]:cfg.tiles[0][0] + cfg.tiles[0][1]])
            for ix, (t0, N, isctx) in enumerate(cfg.tiles):
                ci = 1 if isctx else 0
                if not isctx:
                    M.dma("sp", rC[:, :N], rope_in[0, :, t0 - CTX:t0 - CTX + N]); M.dma("sp", rS[:, :N], rope_in[1, :, t0 - CTX:t0 - CTX + N])
                rms_mod(xT, hT, N, P(l, ci, 3), P(l, ci, 4), sqb, rstd, sqb)
                if ix + 1 < len(cfg.tiles):
                    tn, Nn, _ = cfg.tiles[ix + 1]
                    M.dma("sp", xT[:, :, 0:Nn], xTd[:, :, tn:tn + Nn])
                for (name, c0, nch, scl) in FMG:
                    rope = name in ("dfq", "dfk") and not isctx
                    for c in range(nch):
                        st = stg[si % 2]
                        pa = PS[(ev % 2) * 2]; pb = PS[(ev % 2) * 2 + 1]; ev += 1
                        col = c0 + c * 128
                        for k in range(8):
                            M.mm(pa[:, :N], W[:, k, col:col + 128], hT[:, k, :N], start=(k == 0), stop=(k == 7))
                        if rope:
                            rc = col - 2064
                            for k in range(8):
                                M.mm(pb[:, :N], Wr[:, k, rc:rc + 128], hT[:, k, :N], start=(k == 0), stop=(k == 7))
                            t1 = sqb[0]; t2 = sqb[1]
                            M.stt(t1[:, :N], pa[:, :N], float(scl or 1.0), rC[:, :N], ALU.mult, ALU.mult)
                            M.stt(t2[:, :N], pb[:, :N], float(scl or 1.0), rS[:, :N], ALU.mult, ALU.mult)
                            M.tt("pool", st[:, c % 4, :N], t1[:, :N], t2[:, :N], ALU.add, part=True)
                        elif ev % 2 == 0:
                            M.act(st[:, c % 4, :N], pa[:, :N], AF.Copy, scale=float(scl or 1.0), part=True)
                        else:
                            M.ts("dve", st[:, c % 4, :N], pa[:, :N], float(scl or 1.0), ALU.mult, part=True)
                        if c % 4 == 3:
                            r0 = (c - 3) * 128
                            M.dma("sp", fm[name][r0:r0 + 512, t0:t0 + N].rearrange("(c p) t -> p c t", p=128), st[:, :, 0:N])
                            si += 1
                for tb in range(N // 128):
                    for (name, c0, ncol) in TMG:
                        pa = PS[4 + ti % 2]
                        for k in range(8):
                            M.mm(pa[:, :ncol], hT[:, k, tb * 128:(tb + 1) * 128], W[:, k, c0:c0 + ncol], start=(k == 0), stop=(k == 7))
                        dst = tm[name][t0 + tb * 128:t0 + (tb + 1) * 128, :]
                        if name == "mlg":
                            M.copy("dve", stf[:, :], pa[:, 0:16]); M.dma("sp", dst, stf[:, :])
                        else:
                            so = stt_[ti % 2]
                            if ti % 2 == 0:
                                M.copy("act", so[:, :], pa[:, :])
                            else:
                                M.copy("dve", so[:, :], pa[:, :])
                            M.dma("sp", dst, so[:, :])
                        ti += 1
        M.barrier()

    def diff_stage(l, ctx_out):
        lam_init = 0.8 - 0.6 * math.exp(-0.3 * l)
        with ExitStack() as es:
            lv = sb(es, "lv", [1, 256], F32); lt = sb(es, "lt", [1, 8], F32)
            nlam = sb(es, "nlam", [128, 1], F32); ngd = sb(es, "ngd", [128, 4], F32)
            kzs = [[sb(es, "kz%d_%d" % (i, jb), [128, T], BF16) for i in range(2)] for jb in range(2)]
            Vs = [sb(es, "V%d" % jb, [128, NB, 128], BF16) for jb in range(2)]
            qT = [sb(es, "qT%d" % i, [128, 512], BF16) for i in range(2)]
            pt = [sb(es, "pt%d" % i, [128, 512], BF16) for i in range(4)]
            acc = [sb(es, "acc%d" % i, [128, 512], F32) for i in range(2)]
            for jb in range(2):
                M.memset("pool", kzs[jb][0][64:128, :], 0.0); M.memset("pool", kzs[jb][1][0:64, :], 0.0)

            def ldh(h):
                for m in range(2):
                    M.dma("sp", kzs[h % 2][m][m * 64:(m + 1) * 64, :], fm["dfk"][h * 128 + m * 64:h * 128 + (m + 1) * 64, :], part=True)
                M.dma("sp", Vs[h % 2][:, :, :], tm["dfv"][:, h * 128:(h + 1) * 128].rearrange("(b p) c -> p b c", p=128))
            r1 = sb(es, "r1", [128, 512], F32); r2 = sb(es, "r2", [128, 512], F32)
            t1 = sb(es, "t1", [128, 512], F32); t2 = sb(es, "t2", [128, 512], F32)
            yb = [sb(es, "yb%d" % i, [128, 512], BF16) for i in range(2)]
            M.dma("sp", lv[:, :], dfl_in[l, :, :])
            lp = sb(es, "lp", [1, 128], F32)
            for i in range(2):
                M.tt("dve", lp[:, 64 * i:64 * i + 64], lv[:, 128 * i:128 * i + 64], lv[:, 128 * i + 64:128 * i + 128], ALU.mult, part=True)
            M.op("dve", "tensor_reduce", [lt[:, 0:2]], [lp[:, :]], out=lt[:, 0:2], in_=lp[:, :].rearrange("p (a b) -> p a b", a=2),
                 axis=mybir.AxisListType.X, op=ALU.add)
            M.act(lt[:, 2:4], lt[:, 0:2], AF.Exp)
            M.tt("dve", lt[:, 4:5], lt[:, 3:4], lt[:, 2:3], ALU.subtract)
            M.ts("dve", lt[:, 5:6], lt[:, 4:5], -lam_init, ALU.add)
            M.mm(PS[7][:, 0:1], ones_f[0:1, :], lt[0:1, 5:6])
            M.copy("dve", nlam[:, :], PS[7][:, 0:1])
            M.ts("dve", ngd[:, :], pvs[:, l, 132:136], 1.0 - lam_init, ALU.mult)
            qi = 0
            items = [(h, t0, N, isctx) for h in range(4) for (t0, N, isctx) in cfg.tiles if ctx_out or not isctx]

            def ldq(ii):
                hh, tt0, NN, _ = items[ii]
                M.dma("sp", qT[ii % 2][:, :NN], fm["dfq"][hh * 128:(hh + 1) * 128, tt0:tt0 + NN])

            ldh(0); ldq(0)
            for ii, (h, t0, N, isctx) in enumerate(items):
                if True:
                    if (ii == 0 or items[ii - 1][0] != h) and h + 1 < 4:
                        ldh(h + 1)
                    kz = kzs[h % 2]; V = Vs[h % 2]
                    if ii + 1 < len(items):
                        ldq(ii + 1)
                    q = qT[qi % 2]; yo = yb[qi % 2]; qi += 1
                    kbs = list(range(cfg.NCB)) if isctx else list(range(NB))
                    O = [PS[0], PS[1]]
                    its = [(i, kb, m) for i, kb in enumerate(kbs) for m in range(2)]
                    nk = len(kbs)

                    def qk(j):
                        i, kb, m = its[j]
                        M.mm(PS[2 + j % 4][:, :N], kz[m][:, kb * 128:(kb + 1) * 128], q[:, :N])

                    def ex(j):
                        M.act(pt[j % 4][:, :N], PS[2 + j % 4][:, :N], AF.Exp)

                    def pv(j):
                        i, kb, m = its[j]
                        M.mm(O[m][:, :N], V[:, kb, :], pt[j % 4][:, :N], start=(i == 0), stop=(i == nk - 1))
                        if m == 0:
                            M.mm(PS[6][:, :N], ones_b[:, :], pt[j % 4][:, :N], start=(i == 0), stop=(i == nk - 1))
                        elif i == 0:
                            M.copy("dve", acc[1][:, :N], pt[j % 4][:, :N])
                        else:
                            M.tt("dve", acc[1][:, :N], acc[1][:, :N], pt[j % 4][:, :N], ALU.add)

                    pipeline(len(its), [qk, ex, pv], 3)
                    M.recip(r1[:, :N], PS[6][:, :N])
                    M.mm(PS[7][:, :N], ones_f[:, :], acc[1][:, :N])
                    M.recip(r2[:, :N], PS[7][:, :N])
                    M.tt("dve", t1[:, :N], O[0][:, :N], r1[:, :N], ALU.mult)
                    M.tt("dve", t2[:, :N], O[1][:, :N], r2[:, :N], ALU.mult)
                    M.stt(t1[:, :N], t2[:, :N], nlam[:, 0:1], t1[:, :N], ALU.mult, ALU.add)
                    M.act(t2[:, :N], t1[:, :N], AF.Square)
                    M.mm(PS[7][:, :N], ones_f[:, :], t2[:, :N])
                    M.act(r1[:, :N], PS[7][:, :N], AF.Sqrt, bias=eps_ap, scale=1.0 / 128)
                    M.recip(r1[:, :N], r1[:, :N])
                    M.stt(yo[:, :N], t1[:, :N], ngd[:, h:h + 1], r1[:, :N], ALU.mult, ALU.mult)
                    M.dma("sp", fm["dfy"][h * 128:(h + 1) * 128, t0:t0 + N], yo[:, :N])
        M.barrier()

    def na_stage(l, ctx_out):
        ROWS, G = cfg.ROWS, cfg.G
        trev = trev_all[l]
        with ExitStack() as es:
            BTs = [sb(es, "BT%d" % i, [128, 48, 512], BF16) for i in range(2)]
            kzs = [[sb(es, "kz%d_%d" % (i, jb), [128, T], BF16) for i in range(2)] for jb in range(2)]
            Vzs = [[sb(es, "Vz%d_%d" % (i, jb), [128, NB, 128], BF16) for i in range(2)] for jb in range(2)]
            oz = [sb(es, "oz%d" % i, [128, 128], BF16) for i in range(2)]
            qT = [sb(es, "qT%d" % i, [128, 512], BF16) for i in range(2)]
            pt = [sb(es, "pt%d" % i, [128, 512], BF16) for i in range(4)]
            sf = [sb(es, "sf%d" % i, [128, 512], F32) for i in range(4)]
            r1 = sb(es, "r1", [128, 512], F32)
            yb = [sb(es, "yb%d" % i, [128, 512], BF16) for i in range(2)]
            for e in range(2):
                for jb in range(2):
                    M.memset("pool", kzs[jb][e][(1 - e) * 64:(2 - e) * 64, :], 0.0)
                    M.memset("pool", Vzs[jb][e][:, :, :], 0.0)
                M.memset("pool", oz[e][:, :], 0.0); M.memset("pool", oz[e][:, e * 64:(e + 1) * 64], 1.0)

            nbt = [0]

            def ldc(c):
                kz = kzs[c % 2]; Vz = Vzs[c % 2]; BT = BTs[c % 2]
                for e in range(2):
                    M.dma("sp", kz[e][e * 64:(e + 1) * 64, :], fm["nak"][c * 128 + e * 64:c * 128 + (e + 1) * 64, :], part=True)
                    M.dma("sp", Vz[e][:, :, e * 64:(e + 1) * 64],
                          tm["nav"][:, c * 128 + e * 64:c * 128 + (e + 1) * 64].rearrange("(b p) c -> p b c", p=128), part=True)
                M.memset("pool", BT[:, :, :], NEG)
                for pat in range(3):
                    g = (0, 1, G - 1)[pat]
                    rs0 = min(max(8 * g - 4, 0), ROWS - 16)
                    for e in range(2):
                        h = 2 * c + e
                        for kb in range(8):
                            for kr2 in range(2):
                                kr = rs0 + 2 * kb + kr2
                                val = [qr for qr in range(8) if 0 <= kr - min(max(8 * g + qr - 4, 0), ROWS - 8) < 8]
                                if not val:
                                    continue
                                qa, qb = val[0], val[-1] + 1
                                assert val == list(range(qa, qb))
                                ja = 7 - kr + 8 * g + qa
                                assert 0 <= ja and ja + (qb - qa) - 1 <= 14
                                src = trev[h * 15 + ja:h * 15 + ja + (qb - qa), :].rearrange("j (cp c) -> cp j c", c=64)
                                dst = BT[kr2 * 64:(kr2 + 1) * 64, (pat * 2 + e) * 8 + kb, qa * 64:qb * 64].rearrange("p (j c) -> p j c", c=64)
                                nbt[0] += 1
                                M.dma("sp" if nbt[0] % 2 else "pool", dst, src, part=True)

            qi = 0
            ldc(0)
            for c in range(4):
                if c + 1 < 4:
                    ldc(c + 1)
                kz = kzs[c % 2]; Vz = Vzs[c % 2]; BT = BTs[c % 2]
                qtiles = [(CTX + g * 512, 512, g) for g in range(G)] + ([(0, CTX, -1)] if ctx_out else [])
                for qx, (t0, N, g) in enumerate(qtiles):
                    if qx == 0 and c == 0:
                        M.dma("sp", qT[qi % 2][:, :N], fm["naq"][c * 128:(c + 1) * 128, t0:t0 + N])
                    nx = (c, qx + 1) if qx + 1 < len(qtiles) else ((c + 1, 0) if c + 1 < 4 else None)
                    if nx is not None:
                        tn, Nn, _ = qtiles[nx[1]]
                        M.dma("sp", qT[(qi + 1) % 2][:, :Nn], fm["naq"][nx[0] * 128:(nx[0] + 1) * 128, tn:tn + Nn])
                    q = qT[qi % 2]; yo = yb[qi % 2]; qi += 1
                    if g >= 0:
                        pat = 0 if g == 0 else (2 if g == G - 1 else 1)
                        rs0 = min(max(8 * g - 4, 0), ROWS - 16)
                        blks = [((CTX + rs0 * 64) // 128 + kb, kb) for kb in range(8)] + [(b, -1) for b in range(cfg.NCB)]
                    else:
                        blks = [(b, -1) for b in range(cfg.NCB)]
                    O = PS[0]; Dn = PS[1]
                    its = [(e, i, blk, kb) for e in range(2) for i, (blk, kb) in enumerate(blks)]
                    nk = len(blks)

                    def qk(j):
                        e, i, blk, kb = its[j]
                        M.mm(PS[2 + j % 6][:, :N], kz[e][:, blk * 128:(blk + 1) * 128], q[:, :N])

                    def ex(j):
                        e, i, blk, kb = its[j]
                        ps = PS[2 + j % 6]; p_ = pt[j % 4]; s_ = sf[j % 4]
                        if kb >= 0:
                            M.tt("dve", s_[:, :N], ps[:, :N], BT[:, (pat * 2 + e) * 8 + kb, :N], ALU.add)
                            M.act(p_[:, :N], s_[:, :N], AF.Exp)
                        else:
                            M.act(p_[:, :N], ps[:, :N], AF.Exp)

                    def pv(j):
                        e, i, blk, kb = its[j]
                        p_ = pt[j % 4]
                        first = (j == 0); last = (j == len(its) - 1)
                        M.mm(O[:, :N], Vz[e][:, blk, :], p_[:, :N], start=first, stop=last)
                        M.mm(Dn[:, :N], oz[e][:, :], p_[:, :N], start=first, stop=last)

                    pipeline(len(its), [qk, ex, pv], 3)
                    M.recip(r1[:, :N], Dn[:, :N])
                    M.tt("dve", yo[:, :N], O[:, :N], r1[:, :N], ALU.mult)
                    M.dma("sp", fm["nay"][c * 128:(c + 1) * 128, t0:t0 + N], yo[:, :N])
        M.barrier()

    def mlstm_stage(l, ctx_out):
        with ExitStack() as es:
            Gt = sb(es, "Gt", [128, NB, 16], F32); gbs = sb(es, "gbs", [128, 16], F32)
            LF = sb(es, "LF", [128, NB, 2, 4], F32); Bc = sb(es, "Bc", [128, NB, 2, 4], F32)
            eL = sb(es, "eL", [128, NB, 2, 4], F32); EK = sb(es, "EK", [128, NB, 2, 4], F32); EMB = sb(es, "EMB", [128, NB, 2, 4], F32)
            raw = sb(es, "raw", [128, T], BF16); cvb = sb(es, "cvb", [128, T], F32); k32 = sb(es, "k32", [128, T], F32)
            qT = sb(es, "qT", [128, T], BF16); kT = sb(es, "kT", [128, T], BF16); sigo = sb(es, "sigo", [128, T], BF16)
            Va = sb(es, "Va", [128, NB, 129], BF16); hF = sb(es, "hF", [128, NB, 128], F32); ybuf = sb(es, "ybuf", [128, T], BF16)
            CT = sb(es, "CT", [128, 129], F32); CTb = sb(es, "CTb", [128, 129], BF16); tmc = sb(es, "tmc", [128, 129], F32)
            ktd = [sb(es, "ktd%d" % i, [128, 128], BF16) for i in range(2)]
            Sm = [sb(es, "Sm%d" % i, [128, 128], BF16) for i in range(2)]
            hs = [sb(es, "hs%d" % i, [128, 128], F32) for i in range(2)]
            hn = [sb(es, "hn%d" % i, [128, 128], F32) for i in range(2)]
            smv = [sb(es, "sm%d" % i, [128, 8], F32) for i in range(2)]
            sqh = sb(es, "sqh", [128, NB, 128], F32); ssq = sb(es, "ssq", [128, NB], F32)
            M.dma("sp", Gt[:, :, :], tm["mlg"].rearrange("(b p) c -> p b c", p=128)); M.dma("sp", gbs[:, :], gb_in[l, :, :])
            for j in range(16):
                M.ts("dve", Gt[:, :, j], Gt[:, :, j], gbs[:, j:j + 1], ALU.add, part=True)
            for d in range(2):
                M.act(LF[:, :, d, :], Gt[:, :, d * 8 + 4:d * 8 + 8], AF.Exp, scale=-1.0, part=True)
            M.act(LF[:, :, :, :], LF[:, :, :, :], AF.Ln, bias=one_ap)
            M.ts("dve", LF[:, :, :, :], LF[:, :, :, :], -1.0, ALU.mult)
            M.mm(PS[0][:, 0:NB * 4], triF, LF[:, :, 0, :]); M.mm(PS[1][:, 0:NB * 4], triB, LF[:, :, 1, :])
            M.mm(PS[2][:, 0:NB * 8], ones_f[:, :], LF[:, :, :, :])
            M.act(eL[:, :, :, :], PS[2][:, 0:NB * 8].rearrange("p (b d h) -> p b d h", d=2, h=4), AF.Exp)
            for d in range(2):
                pv_ = PS[d][:, 0:NB * 4].rearrange("p (b h) -> p b h", h=4)
                M.copy("dve", Bc[:, :, d, :], pv_, part=True)
                M.tt("dve", EK[:, :, d, :], Gt[:, :, d * 8:d * 8 + 4], pv_, ALU.subtract, part=True)
            M.act(EK[:, :, :, :], EK[:, :, :, :], AF.Exp)
            M.act(EMB[:, :, :, :], Bc[:, :, :, :], AF.Exp, scale=-1.0)
            segs = ((0, CTX), (CTX, T))
            for h in range(4):
                for (name, chunk, isq) in (("mlq", h, True), ("mlk", 4 + h, False)):
                    M.dma("sp", raw[:, :], fm[name][h * 128:(h + 1) * 128, :])
                    cwv = lambda j: pvs[:, l, 96 + j * 8 + chunk:96 + j * 8 + chunk + 1]
                    for (a, b) in segs:
                        M.ts("dve", cvb[:, a:b], raw[:, a:b], cwv(1), ALU.mult, pvs[:, l, 120 + chunk:121 + chunk], ALU.add, part=True)
                        M.stt(cvb[:, a + 1:b], raw[:, a:b - 1], cwv(0), cvb[:, a + 1:b], ALU.mult, ALU.add, part=True)
                        M.stt(cvb[:, a:b - 1], raw[:, a + 1:b], cwv(2), cvb[:, a:b - 1], ALU.mult, ALU.add, part=True)
                    if isq:
                        M.act(k32[:, :], cvb[:, :], AF.Silu)
                        M.ts("dve", qT[:, :], k32[:, :], 128.0 ** -0.5, ALU.mult)
                    else:
                        M.act(k32[:, :], cvb[:, :], AF.Silu)
                        M.copy("pool", kT[:, :], k32[:, :])
                M.dma("sp", raw[:, :], fm["mlo"][h * 128:(h + 1) * 128, :])
                M.act(sigo[:, :], raw[:, :], AF.Sigmoid)
                M.dma("sp", Va[:, :, 0:128], tm["mlv"][:, h * 128:(h + 1) * 128].rearrange("(b p) c -> p b c", p=128))
                M.memset("pool", Va[:, :, 128:129], 1.0, part=True)
                for d in range(2):
                    order = list(range(NB)) if d == 0 else (list(range(cfg.NCB - 1, -1, -1)) + list(range(NB - 1, cfg.NCB - 1, -1)))
                    tri = triF if d == 0 else triB
                    M.memset("pool", CT[:, :], 0.0); M.memset("pool", CTb[:, :], 0.0)

                    def stA(j, d=d, order=order, tri=tri):
                        blk = order[j]; cs = slice(blk * 128, (blk + 1) * 128)
                        ek = EK[:, blk, d, h:h + 1]
                        pk = PS[j % 2]; pU = PS[2 + j % 2]
                        M.tr(pk[:, 0:128], k32[:, cs], ident)
                        M.ts("dve", ktd[j % 2][:, :], pk[:, 0:128], ek, ALU.mult)
                        M.mm(pU[:, 0:129], ktd[j % 2][:, :], Va[:, blk, :])

                    def stB(j, d=d, order=order, tri=tri):
                        blk = order[j]; cs = slice(blk * 128, (blk + 1) * 128)
                        pO = PS[5 + j % 2]; pU = PS[2 + j % 2]; pT = PS[7]; pS = PS[4]
                        hs_ = hs[j % 2]; hn_ = hn[j % 2]; sm_ = smv[j % 2]
                        M.mm(pS[:, 0:128], kT[:, cs], qT[:, cs])
                        M.stt(Sm[j % 2][:, :], pS[:, 0:128], EK[:, blk, d, h:h + 1], tri, ALU.mult, ALU.mult)
                        M.mm(pO[:, 0:129], Sm[j % 2][:, :], Va[:, blk, :], start=True, stop=False)
                        M.mm(pO[:, 0:129], qT[:, cs], CTb[:, :], start=False, stop=True)
                        M.tt("dve", tmc[:, :], pU[:, 0:129], CT[:, :], ALU.add)
                        M.ts("dve", CT[:, :], tmc[:, :], eL[:, blk, d, h:h + 1], ALU.mult)
                        M.copy("pool", CTb[:, :], CT[:, :])
                        M.ts("dve", sm_[:, 0:1], pO[:, 128:129], -1.0, ALU.mult, EMB[:, blk, d, h:h + 1], ALU.max)
                        M.tt("dve", sm_[:, 0:1], sm_[:, 0:1], pO[:, 128:129], ALU.max)
                        M.recip(sm_[:, 1:2], sm_[:, 0:1])
                        if d == 0:
                            M.ts("dve", hF[:, blk, :], pO[:, 0:128], sm_[:, 1:2], ALU.mult, part=True)
                        else:
                            M.stt(hF[:, blk, :], pO[:, 0:128], sm_[:, 1:2], hF[:, blk, :], ALU.mult, ALU.add, part=True)

                    pipeline(NB, [stA, stB], 1)
                b0 = 0 if ctx_out else cfg.NCB
                M.act(sqh[:, b0:NB, :], hF[:, b0:NB, :], AF.Square)
                M.op("dve", "tensor_reduce", [ssq[:, b0:NB]], [sqh[:, b0:NB, :]], out=ssq[:, b0:NB], in_=sqh[:, b0:NB, :],
                     axis=mybir.AxisListType.X, op=ALU.add)
                M.act(ssq[:, b0:NB], ssq[:, b0:NB], AF.Sqrt, bias=eps_ap, scale=1.0 / 128)
                M.recip(ssq[:, b0:NB], ssq[:, b0:NB])
                for blk in range(b0, NB):
                    cs = slice(blk * 128, (blk + 1) * 128)
                    hn_ = hn[blk % 2]; pT = PS[blk % 2]
                    M.ts("dve", hn_[:, :], hF[:, blk, :], ssq[:, blk:blk + 1], ALU.mult)
                    M.tr(pT[:, 0:128], hn_[:, :], ident)
                    M.stt(ybuf[:, cs], pT[:, 0:128], pvs[:, l, 128 + h:129 + h], sigo[:, cs], ALU.mult, ALU.mult, part=True)
                a0 = 0 if ctx_out else CTX
                M.dma("sp", fm["mly"][h * 128:(h + 1) * 128, a0:T], ybuf[:, a0:T])
        M.barrier()

    def merge_stage(l, tiles):
        with ExitStack() as es:
            Wb = sb(es, "Wb", [128, 12, D], BF16); Wo = sb(es, "Wo", [128, 8, D], BF16)
            xTs = [sb(es, "xT%d" % i, [128, 8, 512], F32) for i in range(2)]
            brs = [sb(es, "br%d" % i, [128, 12, 512], BF16) for i in range(2)]
            gts = [sb(es, "gt%d" % i, [128, 24, 512], BF16) for i in range(2)]
            yT = sb(es, "yT", [128, 8, 512], BF16)

            def ldt(ix):
                t0, N, isctx = tiles[ix]
                M.dma("sp", xTs[ix % 2][:, :, 0:N], xTd[:, :, t0:t0 + N])
                for i, nm in enumerate(("mly", "dfy", "nay")):
                    M.dma("sp", brs[ix % 2][:, i * 4:(i + 1) * 4, 0:N], fm[nm][:, t0:t0 + N].rearrange("(c p) t -> p c t", p=128), part=True)
                M.dma("sp", gts[ix % 2][:, :, 0:N], fm["gate"][:, t0:t0 + N].rearrange("(c p) t -> p c t", p=128))
            ta = [sb(es, "ta%d" % i, [128, 512], F32) for i in range(2)]; tb_ = [sb(es, "tb%d" % i, [128, 512], F32) for i in range(2)]
            wbv = w_br[l].rearrange("i (k p) n -> p i k n", p=128); wov = w_out[l].rearrange("(k p) n -> p k n", p=128)
            for i in range(3):
                for k in range(4):
                    M.dma("pool", Wb[:, i * 4 + k, :], wbv[:, i, k, :], part=True)
            for k in range(8):
                M.dma("pool", Wo[:, k, :], wov[:, k, :], part=True)
            ldt(0)
            for ix, (t0, N, isctx) in enumerate(tiles):
                ci = 1 if isctx else 0
                if ix + 1 < len(tiles):
                    ldt(ix + 1)
                xT = xTs[ix % 2]; br = brs[ix % 2]; gt = gts[ix % 2]
                M.act(gt[:, :, 0:N], gt[:, :, 0:N], AF.Sigmoid)
                for m in range(8):
                    a_ = ta[m % 2]; b_ = tb_[m % 2]
                    for i in range(3):
                        pp = PS[(m % 2) * 3 + i]
                        for k in range(4):
                            M.mm(pp[:, :N], Wb[:, i * 4 + k, m * 128:(m + 1) * 128], br[:, i * 4 + k, :N], start=(k == 0), stop=(k == 3))
                    M.tt("dve", a_[:, :N], PS[(m % 2) * 3][:, :N], gt[:, m, :N], ALU.mult)
                    M.tt("dve", b_[:, :N], PS[(m % 2) * 3 + 1][:, :N], gt[:, 8 + m, :N], ALU.mult)
                    M.tt("pool", a_[:, :N], a_[:, :N], b_[:, :N], ALU.add)
                    M.tt("dve", b_[:, :N], PS[(m % 2) * 3 + 2][:, :N], gt[:, 16 + m, :N], ALU.mult)
                    M.tt("pool", yT[:, m, :N], a_[:, :N], b_[:, :N], ALU.add, part=True)
                g5 = P(l, ci, 5)
                for m2 in range(8):
                    po = PS[6 + m2 % 2]
                    for m in range(8):
                        M.mm(po[:, :N], Wo[:, m, m2 * 128:(m2 + 1) * 128], yT[:, m, :N], start=(m == 0), stop=(m == 7))
                    M.stt(xT[:, m2, :N], po[:, :N], g5[:, m2:m2 + 1], xT[:, m2, :N], ALU.mult, ALU.add, part=True)
                M.dma("sp", xTd[:, :, t0:t0 + N], xT[:, :, 0:N])
        M.barrier()

    def final_stage():
        with ExitStack() as es:
            xT = sb(es, "xT", [128, 8, 512], F32); xn = sb(es, "xn", [128, 8, 512], F32)
            sqb = [sb(es, "sq%d" % i, [128, 512], F32) for i in range(2)]
            rstd = sb(es, "rstd", [128, 512], F32)
            ob = [sb(es, "ob%d" % i, [128, D], F32) for i in range(2)]
            oi = 0
            for (t0, N, isctx) in cfg.tiles:
                if isctx:
                    continue
                M.dma("sp", xT[:, :, 0:N], xTd[:, :, t0:t0 + N])
                for k in range(8):
                    M.act(sqb[k % 2][:, :N], xT[:, k, :N], AF.Square)
                    M.mm(PS[7][:, :N], ones_f[:, :], sqb[k % 2][:, :N], start=(k == 0), stop=(k == 7))
                M.act(rstd[:, :N], PS[7][:, :N], AF.Sqrt, bias=eps_ap, scale=1.0 / D)
                M.recip(rstd[:, :N], rstd[:, :N])
                for k in range(8):
                    M.stt(xn[:, k, :N], xT[:, k, :N], cvs[:, 16 + k:17 + k], rstd[:, :N], ALU.mult, ALU.mult, part=True)
                for tb in range(N // 128):
                    o_ = ob[oi % 2]
                    for k in range(8):
                        pb = PS[(oi % 2) * 2 + k // 4]
                        M.tr(pb[:, (k % 4) * 128:(k % 4 + 1) * 128], xn[:, k, tb * 128:(tb + 1) * 128], ident)
                    M.copy("dve", o_[:, 0:512], PS[(oi % 2) * 2][:, :], part=True)
                    M.copy("act", o_[:, 512:1024], PS[(oi % 2) * 2 + 1][:, :], part=True)
                    r0 = t0 - CTX + tb * 128
                    M.dma("sp", y_out[r0:r0 + 128, :], o_[:, :])
                    oi += 1
        M.barrier()

    for l in range(DEPTH):
        ctx_out = l < DEPTH - 1
        ffn_stage(l, 0, cfg.tiles)
        mixin_stage(l)
        mlstm_stage(l, ctx_out)
        diff_stage(l, ctx_out)
        na_stage(l, ctx_out)
        t2 = cfg.tiles if ctx_out else [t for t in cfg.tiles if not t[2]]
        merge_stage(l, t2)
        ffn_stage(l, 1, t2)
    final_stage()
    gs_.close()
    return nc, M


def host_consts(cfg):
    S = cfg.S
    cmat = np.zeros((128, 3, 128), np.float32)
    i = np.arange(128)
    cmat[:, 0, :] = (i[:, None] == i[None, :]); cmat[:, 1, :] = (i[:, None] <= i[None, :]); cmat[:, 2, :] = (i[:, None] >= i[None, :])
    t = np.arange(S); row = (t // 64).astype(np.float32); col = (t % 64).astype(np.float32)
    freqs = (10000.0 ** (-np.arange(0, 32, 2, dtype=np.float32) / 32)).astype(np.float32)
    ang = np.concatenate([row[:, None] * freqs, col[:, None] * freqs], -1).astype(np.float32)
    rope = np.zeros((2, 128, S), np.float32)
    rope[0] = np.tile(np.cos(ang).T, (4, 1)); rope[1] = np.tile(np.sin(ang).T, (4, 1))
    ind = np.zeros((32, 64, 64), np.float32)
    cp = np.arange(64)[:, None]; c = np.arange(64)[None, :]
    for d in range(31):
        ind[d] = (cp - c + 15 == d)
    qstart = np.clip(c - 8, 0, 48)
    ind[31] = np.where((cp >= qstart) & (cp < qstart + 16), 0.0, NEG)
    return cmat, rope, ind.reshape(32, 4096)


def make_in_maps(cfg, inp):
    cmat, rope, ind = host_consts(cfg)
    DEPTH = cfg.DEPTH
    f = lambda a: np.ascontiguousarray(np.asarray(a, np.float32))
    pv = np.zeros((DEPTH, 128, 136), np.float32)
    for l in range(DEPTH):
        pv[l, :, 0:24] = inp["norm_g"][l].reshape(24, 128).T
        pv[l, :, 24:96] = inp["b_ada"][l].reshape(72, 128).T
        pv[l, :, 96:120] = inp["ml_conv_w"][l].reshape(24, 128).T
        pv[l, :, 120:128] = inp["ml_conv_b"][l].reshape(8, 128).T
        pv[l, :, 128:132] = inp["ml_norm_g"][l].reshape(4, 128).T
        pv[l, :, 132:136] = inp["df_norm_g"][l].reshape(4, 128).T
    gb = np.broadcast_to(np.asarray(inp["ml_gate_b"], np.float32)[:, None, :], (DEPTH, 128, 16))
    rbT = np.ones((DEPTH, 32, 120), np.float32)
    rb = np.asarray(inp["na_rel_bias"], np.float32)
    rbT[:, 0:31, :] = rb[:, :, ::-1, :].reshape(DEPTH, 120, 31).transpose(0, 2, 1)
    shared = {"w_ada": f(inp["w_ada"]), "pv": pv, "gb": f(gb), "ffn_w1": f(inp["ffn_w1"]), "ffn_w2": f(inp["ffn_w2"]),
              "w_in": f(inp["w_in"]), "dfl": f(np.asarray(inp["df_lambda"]).reshape(DEPTH, 1, 256)), "rbT": f(rbT),
              "w_branch": f(inp["w_branch"]), "w_out": f(inp["w_out"]), "cmat": cmat, "rope": rope, "ind": ind}
    maps = []
    for b in range(cfg.NC):
        cv = np.zeros((128, 24), np.float32)
        cv[:, 0:8] = np.asarray(inp["c"][b]).reshape(8, 128).T
        cv[:, 8:16] = np.asarray(inp["c_ctx"]).reshape(8, 128).T
        cv[:, 16:24] = np.asarray(inp["final_g"]).reshape(8, 128).T
        m = dict(shared); m["x"] = f(inp["x"][b]); m["ctx"] = f(inp["ctx"][b]); m["cv"] = cv
        maps.append(m)
    return maps


def kernel(**inputs):
    cfg = Cfg()
    nc, M = build(cfg)
    maps = make_in_maps(cfg, inputs)
    res = run_bass_kernel_spmd(nc, maps, core_ids=list(range(cfg.NC)))
    return np.stack([np.asarray(r["y"], np.float32) for r in res.results], 0)
```

```python
import math
from contextlib import ExitStack
import numpy as np
import concourse.bass as bass
import concourse.mybir as mybir
from concourse.bass_utils import run_bass_kernel_spmd

F32 = mybir.dt.float32
BF16 = mybir.dt.bfloat16
AF = mybir.ActivationFunctionType
ALU = mybir.AluOpType
KQ = 8
EPS = 1e-6
NEG = -30000.0


class Cfg:
    def __init__(s, rows=64, ctx=256, depth=4, dff=2816, ncores=8):
        s.ROWS = rows; s.GW = 64; s.S = rows * 64; s.CTX = ctx; s.T = s.S + ctx
        s.DEPTH = depth; s.DFF = dff; s.NJ = dff // 128; s.D = 1024; s.NB = s.T // 128
        s.NCB = ctx // 128; s.G = rows // 8; s.DIN = 5136 + 3072; s.NC = ncores
        s.tiles = [(0, ctx, True)] + [(ctx + i * 512, 512, False) for i in range(s.S // 512)]


class Buf:
    __slots__ = ("w", "r", "wf")

    def __init__(s):
        s.w = {}; s.r = {}; s.wf = {}


def _merge(d, e):
    for k, v in e.items():
        if d.get(k, 0) < v:
            d[k] = v


class Mach:
    def __init__(s, nc):
        s.nc = nc; s.es = ExitStack(); s.sems = []; s.bufs = {}; s.eng = {}
        for nm, h in (("pe", nc.tensor), ("act", nc.scalar), ("dve", nc.vector), ("pool", nc.gpsimd), ("sp", nc.sync)):
            s.eng[nm] = {"h": h, "si": s.newsem("e_" + nm), "cnt": 0, "seen": {}}
        s.dq = {q: {"sis": [s.newsem("d_%s%d" % (q, i)) for i in range(KQ)], "n": 0} for q in ("sp", "pool")}
        s.ninst = 0

    def newsem(s, name):
        s.sems.append(s.es.enter_context(s.nc.semaphore(name)))
        return len(s.sems) - 1

    def buf(s, ap):
        k = ap.tensor.name
        b = s.bufs.get(k)
        if b is None:
            b = s.bufs[k] = Buf()
        return b

    def wait(s, en, evs):
        E = s.eng[en]
        for si, v in evs.items():
            if en == "pe" and si == E["si"]:
                continue
            if E["seen"].get(si, 0) < v:
                E["h"].wait_ge(s.sems[si], v); E["seen"][si] = v; s.ninst += 1

    def _deps(s, outs, ins, part):
        evs = {}
        for ap in ins:
            _merge(evs, s.buf(ap).w)
        for ap in outs:
            b = s.buf(ap); _merge(evs, b.r)
            _merge(evs, b.wf if part else b.w)
        return evs

    def _rec(s, outs, ins, ev, part=False):
        for ap in ins:
            _merge(s.buf(ap).r, ev)
        for ap in outs:
            _merge(s.buf(ap).w, ev)
            if not part:
                _merge(s.buf(ap).wf, ev)

    def op(s, en, name, outs, ins, *a, part=False, **kw):
        E = s.eng[en]
        s.wait(en, s._deps(outs, ins, part))
        inst = getattr(E["h"], name)(*a, **kw)
        E["cnt"] += 1; s.ninst += 1
        inst.then_inc(s.sems[E["si"]], 1)
        s._rec(outs, ins, {E["si"]: E["cnt"]}, part)

    def dma(s, q, out, in_, part=False, **kw):
        Q = s.dq[q]; slot = Q["n"] % KQ; gen = Q["n"] // KQ; si = Q["sis"][slot]
        evs = s._deps([out], [in_], part)
        if gen > 0:
            _merge(evs, {si: 16 * gen})
        s.wait(q, evs)
        s.eng[q]["h"].dma_start(out=out, in_=in_, **kw).then_inc(s.sems[si], 16)
        Q["n"] += 1; s.ninst += 1
        s._rec([out], [in_], {si: 16 * (gen + 1)}, part)

    def barrier(s):
        evs = {E["si"]: E["cnt"] for E in s.eng.values() if E["cnt"] > 0}
        for Q in s.dq.values():
            for i, si in enumerate(Q["sis"]):
                n = (Q["n"] - i + KQ - 1) // KQ
                if n > 0:
                    evs[si] = 16 * n
        for en in s.eng:
            s.wait(en, evs)

    def mm(s, out, lhsT, rhs, start=True, stop=True):
        s.op("pe", "matmul", [out], [lhsT, rhs], out, lhsT=lhsT, rhs=rhs, start=start, stop=stop)

    def tr(s, out, in_, ident):
        s.op("pe", "transpose", [out], [in_, ident], out, in_, ident)

    def act(s, out, in_, func, bias=None, scale=None, accum_out=None, part=False):
        kw = {}; ins = [in_]; outs = [out]
        if bias is not None:
            kw["bias"] = bias
            if not isinstance(bias, float):
                ins.append(bias)
        if scale is not None:
            kw["scale"] = scale
            if not isinstance(scale, float):
                ins.append(scale)
        if accum_out is not None:
            kw["accum_out"] = accum_out; outs.append(accum_out)
        s.op("act", "activation", outs, ins, out=out, in_=in_, func=func, part=part, **kw)

    def tt(s, en, out, in0, in1, op, part=False):
        s.op(en, "tensor_tensor", [out], [in0, in1], out=out, in0=in0, in1=in1, op=op, part=part)

    def ts(s, en, out, in0, s1, op0, s2=None, op1=None, part=False):
        ins = [in0] + [x for x in (s1, s2) if x is not None and not isinstance(x, float)]
        kw = {"op1": op1} if op1 is not None else {}
        s.op(en, "tensor_scalar", [out], ins, out=out, in0=in0, scalar1=s1, scalar2=s2, op0=op0, part=part, **kw)

    def stt(s, out, in0, scalar, in1, op0, op1, part=False):
        ins = [in0, in1] + ([] if isinstance(scalar, float) else [scalar])
        s.op("dve", "scalar_tensor_tensor", [out], ins, out=out, in0=in0, scalar=scalar, in1=in1, op0=op0, op1=op1, part=part)

    def copy(s, en, out, in_, part=False):
        if en == "act":
            s.op("act", "activation", [out], [in_], out=out, in_=in_, func=AF.Copy, part=part)
        else:
            s.op(en, "tensor_copy", [out], [in_], out=out, in_=in_, part=part)

    def memset(s, en, ap, val, part=False):
        s.op(en, "memset", [ap], [], ap, val, part=part)

    def recip(s, out, in_, part=False):
        s.op("dve", "reciprocal", [out], [in_], out=out, in_=in_, part=part)


def pipeline(n, stages, look):
    for j in range(min(look, n)):
        stages[0](j)
    for j in range(n):
        if j + look < n:
            stages[0](j + look)
        for f in stages[1:]:
            f(j)


def build(cfg):
    nc = bass.Bass("TRN2", target_bir_lowering=False)
    T, S, CTX, D, DFF, NJ, NB, DEPTH = cfg.T, cfg.S, cfg.CTX, cfg.D, cfg.DFF, cfg.NJ, cfg.NB, cfg.DEPTH

    def din(name, shape, dt=F32):
        return nc.dram_tensor(name, list(shape), dt, kind="ExternalInput").ap()

    def dscr(name, shape, dt):
        return nc.dram_tensor(name, list(shape), dt, kind="Internal").ap()

    x_in = din("x", [S, D]); ctx_in = din("ctx", [CTX, D]); cv_in = din("cv", [128, 24])
    w_ada = din("w_ada", [DEPTH, D, 9 * D]); pv_in = din("pv", [DEPTH, 128, 136]); gb_in = din("gb", [DEPTH, 128, 16])
    ffn_w1 = din("ffn_w1", [DEPTH, 2, D, 2 * DFF]); ffn_w2 = din("ffn_w2", [DEPTH, 2, DFF, D])
    w_in = din("w_in", [DEPTH, D, cfg.DIN]); dfl_in = din("dfl", [DEPTH, 1, 256]); rbT_in = din("rbT", [DEPTH, 32, 120])
    w_br = din("w_branch", [DEPTH, 3, 512, D]); w_out = din("w_out", [DEPTH, D, D])
    cmat_in = din("cmat", [128, 3, 128]); rope_in = din("rope", [2, 128, S]); ind_in = din("ind", [32, 4096])
    y_out = nc.dram_tensor("y", [S, D], F32, kind="ExternalOutput").ap()

    xTd = dscr("xTd", [D, T], F32).rearrange("(k p) t -> p k t", p=128)
    fm = {n: dscr("fm_" + n, [r, T], BF16) for n, r in (("mlq", 512), ("mlk", 512), ("mlo", 512), ("dfq", 512), ("dfk", 512),
                                                        ("naq", 512), ("nak", 512), ("gate", 3072), ("mly", 512), ("dfy", 512), ("nay", 512))}
    tm = {n: dscr("tm_" + n, [T, c], dt) for n, c, dt in (("mlv", 512, BF16), ("dfv", 512, BF16), ("nav", 512, BF16), ("mlg", 16, F32))}
    trev_all = dscr("trev", [DEPTH, 120, 4096], BF16)

    M = Mach(nc)
    gs_ = ExitStack()

    _nm = [0]

    def sb(es, name, shape, dt):
        _nm[0] += 1
        return es.enter_context(nc.sbuf_tensor("%s_%d" % (name, _nm[0]), list(shape), dt))

    PS = [gs_.enter_context(nc.psum_tensor("ps%d" % i, [128, 512], F32)) for i in range(8)]
    cm = sb(gs_, "cmat", [128, 3, 128], F32)
    ones_f = sb(gs_, "ones_f", [128, 128], F32); ones_b = sb(gs_, "ones_b", [128, 128], BF16)
    epsc = sb(gs_, "epsc", [128, 2], F32)
    cvs = sb(gs_, "cvs", [128, 24], F32)
    prm = sb(gs_, "prm", [128, DEPTH * 2 * 9 * 8], F32)
    pvs = sb(gs_, "pvs", [128, DEPTH, 136], F32)
    ident = cm[:, 0, :]; triF = cm[:, 1, :]; triB = cm[:, 2, :]
    eps_ap = epsc[:, 0:1]; one_ap = epsc[:, 1:2]

    def P(l, ci, j):
        o = ((l * 2 + ci) * 9 + j) * 8
        return prm[:, o:o + 8]

    M.dma("sp", cm[:, :, :], cmat_in[:, :, :]); M.dma("sp", cvs[:, :], cv_in[:, :])
    for l in range(DEPTH):
        M.dma("sp", pvs[:, l, :], pv_in[l, :, :], part=True)
    M.memset("dve", ones_f[:, :], 1.0); M.memset("dve", ones_b[:, :], 1.0)
    M.memset("dve", epsc[:, 0:1], EPS); M.memset("dve", epsc[:, 1:2], 1.0, part=True)
    with ExitStack() as es:
        scT = sb(es, "scT", [128, 8, 2], F32)
        wa = [sb(es, "wa%d" % i, [128, 8, 1024], F32) for i in range(3)]
        mods = sb(es, "mods", [128, 72, 2], F32)
        for i in range(2):
            M.act(scT[:, :, i], cvs[:, i * 8:(i + 1) * 8], AF.Silu, part=True)
        it = 0
        for l in range(DEPTH):
            wv = w_ada[l].rearrange("(k p) n -> p k n", p=128)
            pm = PS[l % 2]
            for grp in range(9):
                w = wa[it % 3]; it += 1
                for k in range(8):
                    M.dma("sp" if k % 2 else "pool", w[:, k, :], wv[:, k, grp * 1024:(grp + 1) * 1024], part=True)
                for fc in range(8):
                    c0 = (grp * 8 + fc) * 2
                    for k in range(8):
                        M.mm(pm[:, c0:c0 + 2], w[:, k, fc * 128:(fc + 1) * 128], scT[:, k, :], start=(k == 0), stop=(k == 7))
            pmv = pm[:, 0:144].rearrange("p (j i) -> p j i", i=2)
            for i in range(2):
                M.tt("dve", mods[:, :, i], pmv[:, :, i], pvs[:, l, 24:96], ALU.add, part=True)
            for ci in range(2):
                for sub in range(3):
                    mo = sub * 24
                    ng = pvs[:, l, sub * 8:(sub + 1) * 8]
                    M.stt(P(l, ci, sub * 3 + 0), mods[:, mo + 8:mo + 16, ci], 1.0, ng, ALU.add, ALU.mult, part=True)
                    M.copy("dve", P(l, ci, sub * 3 + 1), mods[:, mo:mo + 8, ci], part=True)
                    M.ts("dve", P(l, ci, sub * 3 + 2), mods[:, mo + 16:mo + 24, ci], 0.5 if sub != 1 else 1.0, ALU.mult, part=True)
    M.barrier()
    with ExitStack() as es:
        Ind = sb(es, "Ind", [32, 4096], F32)
        At = [sb(es, "A%d" % i, [32, 120], F32) for i in range(2)]
        Tsb = [sb(es, "Tsb%d" % i, [128, 4096], BF16) for i in range(2)]
        M.dma("sp", Ind[:, :], ind_in[:, :])
        for l in range(DEPTH):
            A = At[l % 2]; Tb = Tsb[l % 2]
            M.dma("sp", A[:, :], rbT_in[l, :, :])
            for cc in range(8):
                ps = PS[2 + cc % 2]
                M.mm(ps[0:120, :], A[:, :], Ind[:, cc * 512:(cc + 1) * 512])
                M.copy("dve" if cc % 2 else "act", Tb[0:120, cc * 512:(cc + 1) * 512], ps[0:120, :], part=True)
            M.dma("sp", trev_all[l, :, :], Tb[0:120, :])
    M.barrier()

    with ExitStack() as es:
        xin = [sb(es, "xin%d" % i, [128, D], F32) for i in range(2)]
        xtb = [sb(es, "xtb%d" % i, [128, 8, 128], F32) for i in range(2)]
        for b in range(NB):
            src = ctx_in[b * 128:(b + 1) * 128, :] if b < cfg.NCB else x_in[(b - cfg.NCB) * 128:(b - cfg.NCB + 1) * 128, :]
            xi = xin[b % 2]; xo = xtb[b % 2]
            M.dma("sp", xi[:, :], src)
            for k in range(8):
                pb = PS[(b % 2) * 2 + k // 4]
                M.tr(pb[:, (k % 4) * 128:(k % 4 + 1) * 128], xi[:, k * 128:(k + 1) * 128], ident)
            M.copy("dve", xo[:, 0:4, :], PS[(b % 2) * 2][:, :].rearrange("p (k t) -> p k t", k=4), part=True)
            M.copy("act", xo[:, 4:8, :], PS[(b % 2) * 2 + 1][:, :].rearrange("p (k t) -> p k t", k=4), part=True)
            M.dma("sp", xTd[:, :, b * 128:(b + 1) * 128], xo[:, :, :])
    M.barrier()

    def rms_mod(xT, hT, N, gsv, shv, sqb, rstd, tmpb):
        pss = PS[7]
        if not callable(xT):
            xt_ = xT
            xT = lambda k: xt_[:, k, :N]
        for k in range(8):
            s_ = sqb[k % 2]
            M.act(s_[:, :N], xT(k), AF.Square)
            M.mm(pss[:, :N], ones_f[:, :], s_[:, :N], start=(k == 0), stop=(k == 7))
        M.act(rstd[:, :N], pss[:, :N], AF.Sqrt, bias=eps_ap, scale=1.0 / D)
        M.recip(rstd[:, :N], rstd[:, :N])
        for k in range(8):
            t_ = tmpb[k % 2]
            M.stt(t_[:, :N], xT(k), gsv[:, k:k + 1], rstd[:, :N], ALU.mult, ALU.mult)
            M.act(hT[:, k, :N], t_[:, :N], AF.Identity, bias=shv[:, k:k + 1], part=True)

    def ffn_stage(l, j, tiles):
        sub = 0 if j == 0 else 2
        with ExitStack() as es:
            NG = min(4, NJ)
            gsz = [NJ // NG + (1 if g < NJ % NG else 0) for g in range(NG)]
            gj0 = [sum(gsz[:g]) for g in range(NG)]
            W1g = [sb(es, "W1g%d" % g, [128, 8, 2, gsz[g] * 128], BF16) for g in range(NG)]
            jg = [(g, jj - gj0[g]) for g in range(NG) for jj in range(gj0[g], gj0[g] + gsz[g])]
            W2 = sb(es, "W2", [128, NJ, D], BF16)
            xTk = [sb(es, "xTk%d" % k, [128, 512], F32) for k in range(8)]
            hT = sb(es, "hT", [128, 8, 512], BF16); uT = sb(es, "uT", [128, NJ, 512], BF16)
            sqb = [sb(es, "sq%d" % i, [128, 512], F32) for i in range(2)]
            rstd = sb(es, "rstd", [128, 512], F32)
            w1v = ffn_w1[l, j].rearrange("(k p) n -> p k n", p=128); w2v = ffn_w2[l, j].rearrange("(k p) n -> p k n", p=128)
            for g in range(NG):
                for k in range(8):
                    for ab in range(2):
                        c0 = ab * DFF + gj0[g] * 128
                        M.dma("pool", W1g[g][:, k, ab, :], w1v[:, k, c0:c0 + gsz[g] * 128], part=True)
            for k in range(NJ):
                M.dma("pool", W2[:, k, :], w2v[:, k, :], part=True)
            for k in range(8):
                M.dma("sp", xTk[k][:, 0:tiles[0][1]], xTd[:, k, tiles[0][0]:tiles[0][0] + tiles[0][1]])
            for ix, (t0, N, isctx) in enumerate(tiles):
                ci = 1 if isctx else 0
                rms_mod(lambda k: xTk[k][:, :N], hT, N, P(l, ci, sub * 3), P(l, ci, sub * 3 + 1), sqb, rstd, sqb)
                for jj in range(NJ):
                    pa = PS[(jj % 2) * 2]; pb = PS[(jj % 2) * 2 + 1]; sl = sqb[jj % 2]
                    g_, jo = jg[jj]
                    for k in range(8):
                        M.mm(pa[:, :N], W1g[g_][:, k, 0, jo * 128:(jo + 1) * 128], hT[:, k, :N], start=(k == 0), stop=(k == 7))
                    for k in range(8):
                        M.mm(pb[:, :N], W1g[g_][:, k, 1, jo * 128:(jo + 1) * 128], hT[:, k, :N], start=(k == 0), stop=(k == 7))
                    M.act(sl[:, :N], pa[:, :N], AF.Silu)
                    M.tt("dve", uT[:, jj, :N], sl[:, :N], pb[:, :N], ALU.mult, part=True)
                hg = P(l, ci, sub * 3 + 2)
                for m in range(8):
                    po = PS[4 + m % 2]
                    for jj in range(NJ):
                        M.mm(po[:, :N], W2[:, jj, m * 128:(m + 1) * 128], uT[:, jj, :N], start=(jj == 0), stop=(jj == NJ - 1))
                    M.stt(xTk[m][:, :N], po[:, :N], hg[:, m:m + 1], xTk[m][:, :N], ALU.mult, ALU.add)
                    M.dma("sp", xTd[:, m, t0:t0 + N], xTk[m][:, 0:N])
                    if ix + 1 < len(tiles):
                        tn, Nn, _ = tiles[ix + 1]
                        M.dma("sp", xTk[m][:, 0:Nn], xTd[:, m, tn:tn + Nn])
        M.barrier()

    FMG = [("mlq", 0, 4, None), ("mlk", 512, 4, None), ("mlo", 1536, 4, None), ("dfq", 2064, 4, 0.125), ("dfk", 2576, 4, None),
           ("naq", 3600, 4, 0.125), ("nak", 4112, 4, None), ("gate", 5136, 24, None)]
    TMG = [("mlv", 1024, 512), ("dfv", 3088, 512), ("nav", 4624, 512), ("mlg", 2048, 16)]

    def mixin_stage(l):
        with ExitStack() as es:
            wb_ = [0, 2064, 3600, 5136, 6672, cfg.DIN]
            Wt = [sb(es, "Win%d" % i, [128, 8, wb_[i + 1] - wb_[i]], BF16) for i in range(5)]
            Wr = sb(es, "Wrot", [128, 8, 1024], BF16)

            def Wsl(k, col, n):
                i = max(ii for ii in range(5) if wb_[ii] <= col)
                assert col + n <= wb_[i + 1]
                return Wt[i][:, k, col - wb_[i]:col - wb_[i] + n]
            xT = sb(es, "xT", [128, 8, 512], F32); hT = sb(es, "hT", [128, 8, 512], BF16)
            sqb = [sb(es, "sq%d" % i, [128, 512], F32) for i in range(2)]
            rstd = sb(es, "rstd", [128, 512], F32)
            stg = [sb(es, "stg%d" % i, [128, 4, 512], BF16) for i in range(2)]
            stt_ = [sb(es, "stt%d" % i, [128, 512], BF16) for i in range(2)]
            stf = sb(es, "stf", [128, 16], F32)
            rC = sb(es, "rC", [128, 512], F32); rS = sb(es, "rS", [128, 512], F32)
            wv = w_in[l].rearrange("(k p) n -> p k n", p=128)
            for i in range(5):
                wd = wb_[i + 1] - wb_[i]
                npc = 2 if wd > 2048 else 1
                for k in range(8):
                    for c in range(npc):
                        a_ = c * (wd // npc); b_ = wd if c == npc - 1 else (c + 1) * (wd // npc)
                        M.dma("pool", Wt[i][:, k, a_:b_], wv[:, k, wb_[i] + a_:wb_[i] + b_], part=True)
            for k in range(8):
                sv = Wt[1][:, k, 0:1024].rearrange("p (b h d) -> p b h d", b=16, h=2, d=32)
                dv = Wr[:, k, :].rearrange("p (b h d) -> p b h d", b=16, h=2, d=32)
                M.ts("dve", dv[:, :, 0, :], sv[:, :, 1, :], -1.0, ALU.mult, part=True)
                M.copy("pool", dv[:, :, 1, :], sv[:, :, 0, :], part=True)
            si = 0; ti = 0; ev = 0
            M.dma("sp", xT[:, :, 0:cfg.tiles[0][1]], xTd[:, :, cfg.tiles[0][0]:cfg.tiles[0][0] + cfg.tiles[0][1]])
            for ix, (t0, N, isctx) in enumerate(cfg.tiles):
                ci = 1 if isctx else 0
                if not isctx:
                    M.dma("sp", rC[:, :N], rope_in[0, :, t0 - CTX:t0 - CTX + N]); M.dma("sp", rS[:, :N], rope_in[1, :, t0 - CTX:t0 - CTX + N])
                rms_mod(xT, hT, N, P(l, ci, 3), P(l, ci, 4), sqb, rstd, sqb)
                if ix + 1 < len(cfg.tiles):
                    tn, Nn, _ = cfg.tiles[ix + 1]
                    M.dma("sp", xT[:, :, 0:Nn], xTd[:, :, tn:tn + Nn])
                for (name, c0, nch, scl) in FMG:
                    rope = name in ("dfq", "dfk") and not isctx
                    for c in range(nch):
                        st = stg[si % 2]
                        pa = PS[(ev % 2) * 2]; pb = PS[(ev % 2) * 2 + 1]; ev += 1
                        col = c0 + c * 128
                        for k in range(8):
                            M.mm(pa[:, :N], Wsl(k, col, 128), hT[:, k, :N], start=(k == 0), stop=(k == 7))
                        if rope:
                            rc = col - 2064
                            for k in range(8):
                                M.mm(pb[:, :N], Wr[:, k, rc:rc + 128], hT[:, k, :N], start=(k == 0), stop=(k == 7))
                            t1 = sqb[0]; t2 = sqb[1]
                            M.stt(t1[:, :N], pa[:, :N], float(scl or 1.0), rC[:, :N], ALU.mult, ALU.mult)
                            M.stt(t2[:, :N], pb[:, :N], float(scl or 1.0), rS[:, :N], ALU.mult, ALU.mult)
                            M.tt("pool", st[:, c % 4, :N], t1[:, :N], t2[:, :N], ALU.add, part=True)
                        elif ev % 2 == 0:
                            M.act(st[:, c % 4, :N], pa[:, :N], AF.Copy, scale=float(scl or 1.0), part=True)
                        else:
                            M.ts("dve", st[:, c % 4, :N], pa[:, :N], float(scl or 1.0), ALU.mult, part=True)
                        if c % 4 == 3:
                            r0 = (c - 3) * 128
                            M.dma("sp", fm[name][r0:r0 + 512, t0:t0 + N].rearrange("(c p) t -> p c t", p=128), st[:, :, 0:N])
                            si += 1
                for tb in range(N // 128):
                    for (name, c0, ncol) in TMG:
                        pa = PS[4 + ti % 2]
                        for k in range(8):
                            M.mm(pa[:, :ncol], hT[:, k, tb * 128:(tb + 1) * 128], Wsl(k, c0, ncol), start=(k == 0), stop=(k == 7))
                        dst = tm[name][t0 + tb * 128:t0 + (tb + 1) * 128, :]
                        if name == "mlg":
                            M.copy("dve", stf[:, :], pa[:, 0:16]); M.dma("sp", dst, stf[:, :])
                        else:
                            so = stt_[ti % 2]
                            if ti % 2 == 0:
                                M.copy("act", so[:, :], pa[:, :])
                            else:
                                M.copy("dve", so[:, :], pa[:, :])
                            M.dma("sp", dst, so[:, :])
                        ti += 1
        M.barrier()

    def diff_stage(l, ctx_out):
        lam_init = 0.8 - 0.6 * math.exp(-0.3 * l)
        with ExitStack() as es:
            lv = sb(es, "lv", [1, 256], F32); lt = sb(es, "lt", [1, 8], F32)
            nlam = sb(es, "nlam", [128, 1], F32); ngd = sb(es, "ngd", [128, 4], F32)
            kzs = [[sb(es, "kz%d_%d" % (i, jb), [128, T], BF16) for i in range(2)] for jb in range(2)]
            Vs = [sb(es, "V%d" % jb, [128, NB, 128], BF16) for jb in range(2)]
            qT = [sb(es, "qT%d" % i, [128, 512], BF16) for i in range(2)]
            pt = [sb(es, "pt%d" % i, [128, 512], BF16) for i in range(4)]
            acc = [sb(es, "acc%d" % i, [128, 512], F32) for i in range(2)]
            for jb in range(2):
                M.memset("pool", kzs[jb][0][64:128, :], 0.0); M.memset("pool", kzs[jb][1][0:64, :], 0.0)

            def ldh(h):
                for m in range(2):
                    M.dma("sp", kzs[h % 2][m][m * 64:(m + 1) * 64, :], fm["dfk"][h * 128 + m * 64:h * 128 + (m + 1) * 64, :], part=True)
                M.dma("sp", Vs[h % 2][:, :, :], tm["dfv"][:, h * 128:(h + 1) * 128].rearrange("(b p) c -> p b c", p=128))
            r1 = sb(es, "r1", [128, 512], F32); r2 = sb(es, "r2", [128, 512], F32)
            t1 = sb(es, "t1", [128, 512], F32); t2 = sb(es, "t2", [128, 512], F32)
            yb = [sb(es, "yb%d" % i, [128, 512], BF16) for i in range(2)]
            M.dma("sp", lv[:, :], dfl_in[l, :, :])
            lp = sb(es, "lp", [1, 128], F32)
            for i in range(2):
                M.tt("dve", lp[:, 64 * i:64 * i + 64], lv[:, 128 * i:128 * i + 64], lv[:, 128 * i + 64:128 * i + 128], ALU.mult, part=True)
            M.op("dve", "tensor_reduce", [lt[:, 0:2]], [lp[:, :]], out=lt[:, 0:2], in_=lp[:, :].rearrange("p (a b) -> p a b", a=2),
                 axis=mybir.AxisListType.X, op=ALU.add)
            M.act(lt[:, 2:4], lt[:, 0:2], AF.Exp)
            M.tt("dve", lt[:, 4:5], lt[:, 3:4], lt[:, 2:3], ALU.subtract)
            M.ts("dve", lt[:, 5:6], lt[:, 4:5], -lam_init, ALU.add)
            M.mm(PS[7][:, 0:1], ones_f[0:1, :], lt[0:1, 5:6])
            M.copy("dve", nlam[:, :], PS[7][:, 0:1])
            M.ts("dve", ngd[:, :], pvs[:, l, 132:136], 1.0 - lam_init, ALU.mult)
            qi = 0
            items = [(h, t0, N, isctx) for h in range(4) for (t0, N, isctx) in cfg.tiles if ctx_out or not isctx]

            def ldq(ii):
                hh, tt0, NN, _ = items[ii]
                M.dma("sp", qT[ii % 2][:, :NN], fm["dfq"][hh * 128:(hh + 1) * 128, tt0:tt0 + NN])

            ldh(0); ldq(0)
            for ii, (h, t0, N, isctx) in enumerate(items):
                if True:
                    if (ii == 0 or items[ii - 1][0] != h) and h + 1 < 4:
                        ldh(h + 1)
                    kz = kzs[h % 2]; V = Vs[h % 2]
                    if ii + 1 < len(items):
                        ldq(ii + 1)
                    q = qT[qi % 2]; yo = yb[qi % 2]; qi += 1
                    kbs = list(range(cfg.NCB)) if isctx else list(range(NB))
                    O = [PS[0], PS[1]]
                    its = [(i, kb, m) for i, kb in enumerate(kbs) for m in range(2)]
                    nk = len(kbs)

                    def qk(j):
                        i, kb, m = its[j]
                        M.mm(PS[2 + j % 4][:, :N], kz[m][:, kb * 128:(kb + 1) * 128], q[:, :N])

                    def ex(j):
                        M.act(pt[j % 4][:, :N], PS[2 + j % 4][:, :N], AF.Exp)

                    def pv(j):
                        i, kb, m = its[j]
                        M.mm(O[m][:, :N], V[:, kb, :], pt[j % 4][:, :N], start=(i == 0), stop=(i == nk - 1))
                        if m == 0:
                            M.mm(PS[6][:, :N], ones_b[:, :], pt[j % 4][:, :N], start=(i == 0), stop=(i == nk - 1))
                        elif i == 0:
                            M.copy("dve", acc[1][:, :N], pt[j % 4][:, :N])
                        else:
                            M.tt("dve", acc[1][:, :N], acc[1][:, :N], pt[j % 4][:, :N], ALU.add)

                    pipeline(len(its), [qk, ex, pv], 3)
                    M.recip(r1[:, :N], PS[6][:, :N])
                    M.mm(PS[7][:, :N], ones_f[:, :], acc[1][:, :N])
                    M.recip(r2[:, :N], PS[7][:, :N])
                    M.tt("dve", t1[:, :N], O[0][:, :N], r1[:, :N], ALU.mult)
                    M.tt("dve", t2[:, :N], O[1][:, :N], r2[:, :N], ALU.mult)
                    M.stt(t1[:, :N], t2[:, :N], nlam[:, 0:1], t1[:, :N], ALU.mult, ALU.add)
                    M.act(t2[:, :N], t1[:, :N], AF.Square)
                    M.mm(PS[7][:, :N], ones_f[:, :], t2[:, :N])
                    M.act(r1[:, :N], PS[7][:, :N], AF.Sqrt, bias=eps_ap, scale=1.0 / 128)
                    M.recip(r1[:, :N], r1[:, :N])
                    M.stt(yo[:, :N], t1[:, :N], ngd[:, h:h + 1], r1[:, :N], ALU.mult, ALU.mult)
                    M.dma("sp", fm["dfy"][h * 128:(h + 1) * 128, t0:t0 + N], yo[:, :N])
        M.barrier()

    def na_stage(l, ctx_out):
        ROWS, G = cfg.ROWS, cfg.G
        trev = trev_all[l]
        with ExitStack() as es:
            BTs = [sb(es, "BT%d" % i, [128, 48, 512], BF16) for i in range(2)]
            kzs = [[sb(es, "kz%d_%d" % (i, jb), [128, T], BF16) for i in range(2)] for jb in range(2)]
            Vzs = [[sb(es, "Vz%d_%d" % (i, jb), [128, NB, 128], BF16) for i in range(2)] for jb in range(2)]
            oz = [sb(es, "oz%d" % i, [128, 128], BF16) for i in range(2)]
            qT = [sb(es, "qT%d" % i, [128, 512], BF16) for i in range(2)]
            pt = [sb(es, "pt%d" % i, [128, 512], BF16) for i in range(4)]
            sf = [sb(es, "sf%d" % i, [128, 512], F32) for i in range(4)]
            r1 = sb(es, "r1", [128, 512], F32)
            yb = [sb(es, "yb%d" % i, [128, 512], BF16) for i in range(2)]
            for e in range(2):
                for jb in range(2):
                    M.memset("pool", kzs[jb][e][(1 - e) * 64:(2 - e) * 64, :], 0.0)
                    M.memset("pool", Vzs[jb][e][:, :, :], 0.0)
                M.memset("pool", oz[e][:, :], 0.0); M.memset("pool", oz[e][:, e * 64:(e + 1) * 64], 1.0)

            nbt = [0]

            def ldc(c):
                kz = kzs[c % 2]; Vz = Vzs[c % 2]; BT = BTs[c % 2]
                for e in range(2):
                    M.dma("sp", kz[e][e * 64:(e + 1) * 64, :], fm["nak"][c * 128 + e * 64:c * 128 + (e + 1) * 64, :], part=True)
                    M.dma("sp", Vz[e][:, :, e * 64:(e + 1) * 64],
                          tm["nav"][:, c * 128 + e * 64:c * 128 + (e + 1) * 64].rearrange("(b p) c -> p b c", p=128), part=True)
                M.memset("pool", BT[:, :, :], NEG)
                for pat in range(3):
                    g = (0, 1, G - 1)[pat]
                    rs0 = min(max(8 * g - 4, 0), ROWS - 16)
                    for e in range(2):
                        h = 2 * c + e
                        for kb in range(8):
                            for kr2 in range(2):
                                kr = rs0 + 2 * kb + kr2
                                val = [qr for qr in range(8) if 0 <= kr - min(max(8 * g + qr - 4, 0), ROWS - 8) < 8]
                                if not val:
                                    continue
                                qa, qb = val[0], val[-1] + 1
                                assert val == list(range(qa, qb))
                                ja = 7 - kr + 8 * g + qa
                                assert 0 <= ja and ja + (qb - qa) - 1 <= 14
                                src = trev[h * 15 + ja:h * 15 + ja + (qb - qa), :].rearrange("j (cp c) -> cp j c", c=64)
                                dst = BT[kr2 * 64:(kr2 + 1) * 64, (pat * 2 + e) * 8 + kb, qa * 64:qb * 64].rearrange("p (j c) -> p j c", c=64)
                                nbt[0] += 1
                                M.dma("sp" if nbt[0] % 2 else "pool", dst, src, part=True)

            qi = 0
            ldc(0)
            for c in range(4):
                if c + 1 < 4:
                    ldc(c + 1)
                kz = kzs[c % 2]; Vz = Vzs[c % 2]; BT = BTs[c % 2]
                qtiles = [(CTX + g * 512, 512, g) for g in range(G)] + ([(0, CTX, -1)] if ctx_out else [])
                for qx, (t0, N, g) in enumerate(qtiles):
                    if qx == 0 and c == 0:
                        M.dma("sp", qT[qi % 2][:, :N], fm["naq"][c * 128:(c + 1) * 128, t0:t0 + N])
                    nx = (c, qx + 1) if qx + 1 < len(qtiles) else ((c + 1, 0) if c + 1 < 4 else None)
                    if nx is not None:
                        tn, Nn, _ = qtiles[nx[1]]
                        M.dma("sp", qT[(qi + 1) % 2][:, :Nn], fm["naq"][nx[0] * 128:(nx[0] + 1) * 128, tn:tn + Nn])
                    q = qT[qi % 2]; yo = yb[qi % 2]; qi += 1
                    if g >= 0:
                        pat = 0 if g == 0 else (2 if g == G - 1 else 1)
                        rs0 = min(max(8 * g - 4, 0), ROWS - 16)
                        blks = [((CTX + rs0 * 64) // 128 + kb, kb) for kb in range(8)] + [(b, -1) for b in range(cfg.NCB)]
                    else:
                        blks = [(b, -1) for b in range(cfg.NCB)]
                    O = PS[0]; Dn = PS[1]
                    its = [(e, i, blk, kb) for e in range(2) for i, (blk, kb) in enumerate(blks)]
                    nk = len(blks)

                    def qk(j):
                        e, i, blk, kb = its[j]
                        M.mm(PS[2 + j % 6][:, :N], kz[e][:, blk * 128:(blk + 1) * 128], q[:, :N])

                    def ex(j):
                        e, i, blk, kb = its[j]
                        ps = PS[2 + j % 6]; p_ = pt[j % 4]; s_ = sf[j % 4]
                        if kb >= 0:
                            M.tt("dve", s_[:, :N], ps[:, :N], BT[:, (pat * 2 + e) * 8 + kb, :N], ALU.add)
                            M.act(p_[:, :N], s_[:, :N], AF.Exp)
                        else:
                            M.act(p_[:, :N], ps[:, :N], AF.Exp)

                    def pv(j):
                        e, i, blk, kb = its[j]
                        p_ = pt[j % 4]
                        first = (j == 0); last = (j == len(its) - 1)
                        M.mm(O[:, :N], Vz[e][:, blk, :], p_[:, :N], start=first, stop=last)
                        M.mm(Dn[:, :N], oz[e][:, :], p_[:, :N], start=first, stop=last)

                    pipeline(len(its), [qk, ex, pv], 3)
                    M.recip(r1[:, :N], Dn[:, :N])
                    M.tt("dve", yo[:, :N], O[:, :N], r1[:, :N], ALU.mult)
                    M.dma("sp", fm["nay"][c * 128:(c + 1) * 128, t0:t0 + N], yo[:, :N])
        M.barrier()

    def mlstm_stage(l, ctx_out):
        with ExitStack() as es:
            Gt = sb(es, "Gt", [128, NB, 16], F32); gbs = sb(es, "gbs", [128, 16], F32)
            LF = sb(es, "LF", [128, NB, 2, 4], F32); Bc = sb(es, "Bc", [128, NB, 2, 4], F32)
            eL = sb(es, "eL", [128, NB, 2, 4], F32); EK = sb(es, "EK", [128, NB, 2, 4], F32); EMB = sb(es, "EMB", [128, NB, 2, 4], F32)
            raw = sb(es, "raw", [128, T], BF16); cvb = sb(es, "cvb", [128, T], F32); k32 = sb(es, "k32", [128, T], F32)
            qT = sb(es, "qT", [128, T], BF16); kT = sb(es, "kT", [128, T], BF16); sigo = sb(es, "sigo", [128, T], BF16)
            Va = sb(es, "Va", [128, NB, 129], BF16); hF = sb(es, "hF", [128, NB, 128], F32); ybuf = sb(es, "ybuf", [128, T], BF16)
            CT = sb(es, "CT", [128, 129], F32); CTb = sb(es, "CTb", [128, 129], BF16); tmc = sb(es, "tmc", [128, 129], F32)
            ktd = [sb(es, "ktd%d" % i, [128, 128], BF16) for i in range(2)]
            Sm = [sb(es, "Sm%d" % i, [128, 128], BF16) for i in range(2)]
            hs = [sb(es, "hs%d" % i, [128, 128], F32) for i in range(2)]
            hn = [sb(es, "hn%d" % i, [128, 128], F32) for i in range(2)]
            smv = [sb(es, "sm%d" % i, [128, 8], F32) for i in range(2)]
            sqh = sb(es, "sqh", [128, NB, 128], F32); ssq = sb(es, "ssq", [128, NB], F32)
            hra = [sb(es, "hra%d" % i, [128, NB, 129], F32) for i in range(2)]
            den = sb(es, "den", [128, NB], F32); rr = [sb(es, "rr%d" % i, [128, NB], F32) for i in range(2)]
            M.dma("sp", Gt[:, :, :], tm["mlg"].rearrange("(b p) c -> p b c", p=128)); M.dma("sp", gbs[:, :], gb_in[l, :, :])
            for j in range(16):
                M.ts("dve", Gt[:, :, j], Gt[:, :, j], gbs[:, j:j + 1], ALU.add, part=True)
            for d in range(2):
                M.act(LF[:, :, d, :], Gt[:, :, d * 8 + 4:d * 8 + 8], AF.Exp, scale=-1.0, part=True)
            M.act(LF[:, :, :, :], LF[:, :, :, :], AF.Ln, bias=one_ap)
            M.ts("dve", LF[:, :, :, :], LF[:, :, :, :], -1.0, ALU.mult)
            M.mm(PS[0][:, 0:NB * 4], triF, LF[:, :, 0, :]); M.mm(PS[1][:, 0:NB * 4], triB, LF[:, :, 1, :])
            M.mm(PS[2][:, 0:NB * 8], ones_f[:, :], LF[:, :, :, :])
            M.act(eL[:, :, :, :], PS[2][:, 0:NB * 8].rearrange("p (b d h) -> p b d h", d=2, h=4), AF.Exp)
            for d in range(2):
                pv_ = PS[d][:, 0:NB * 4].rearrange("p (b h) -> p b h", h=4)
                M.copy("dve", Bc[:, :, d, :], pv_, part=True)
                M.tt("dve", EK[:, :, d, :], Gt[:, :, d * 8:d * 8 + 4], pv_, ALU.subtract, part=True)
            M.act(EK[:, :, :, :], EK[:, :, :, :], AF.Exp)
            M.act(EMB[:, :, :, :], Bc[:, :, :, :], AF.Exp, scale=-1.0)
            segs = ((0, CTX), (CTX, T))
            for h in range(4):
                for (name, chunk, isq) in (("mlq", h, True), ("mlk", 4 + h, False)):
                    M.dma("sp", raw[:, :], fm[name][h * 128:(h + 1) * 128, :])
                    cwv = lambda j: pvs[:, l, 96 + j * 8 + chunk:96 + j * 8 + chunk + 1]
                    for (a, b) in segs:
                        M.ts("dve", cvb[:, a:b], raw[:, a:b], cwv(1), ALU.mult, pvs[:, l, 120 + chunk:121 + chunk], ALU.add, part=True)
                        M.stt(cvb[:, a + 1:b], raw[:, a:b - 1], cwv(0), cvb[:, a + 1:b], ALU.mult, ALU.add, part=True)
                        M.stt(cvb[:, a:b - 1], raw[:, a + 1:b], cwv(2), cvb[:, a:b - 1], ALU.mult, ALU.add, part=True)
                    if isq:
                        M.act(k32[:, :], cvb[:, :], AF.Silu)
                        M.ts("dve", qT[:, :], k32[:, :], 128.0 ** -0.5, ALU.mult)
                    else:
                        M.act(k32[:, :], cvb[:, :], AF.Silu)
                        M.copy("pool", kT[:, :], k32[:, :])
                M.dma("sp", raw[:, :], fm["mlo"][h * 128:(h + 1) * 128, :])
                M.act(sigo[:, :], raw[:, :], AF.Sigmoid)
                M.dma("sp", Va[:, :, 0:128], tm["mlv"][:, h * 128:(h + 1) * 128].rearrange("(b p) c -> p b c", p=128))
                M.memset("pool", Va[:, :, 128:129], 1.0, part=True)
                for d in range(2):
                    order = list(range(NB)) if d == 0 else (list(range(cfg.NCB - 1, -1, -1)) + list(range(NB - 1, cfg.NCB - 1, -1)))
                    tri = triF if d == 0 else triB
                    M.memset("pool", CT[:, :], 0.0); M.memset("pool", CTb[:, :], 0.0)

                    def stA(j, d=d, order=order, tri=tri):
                        blk = order[j]; cs = slice(blk * 128, (blk + 1) * 128)
                        ek = EK[:, blk, d, h:h + 1]
                        pk = PS[j % 2]; pU = PS[2 + j % 2]
                        M.tr(pk[:, 0:128], k32[:, cs], ident)
                        M.ts("dve", ktd[j % 2][:, :], pk[:, 0:128], ek, ALU.mult)
                        M.mm(pU[:, 0:129], ktd[j % 2][:, :], Va[:, blk, :])

                    def stB(j, d=d, order=order, tri=tri):
                        blk = order[j]; cs = slice(blk * 128, (blk + 1) * 128)
                        pO = PS[5 + j % 2]; pU = PS[2 + j % 2]; pT = PS[7]; pS = PS[4]
                        hs_ = hs[j % 2]; hn_ = hn[j % 2]; sm_ = smv[j % 2]
                        M.mm(pS[:, 0:128], kT[:, cs], qT[:, cs])
                        M.stt(Sm[j % 2][:, :], pS[:, 0:128], EK[:, blk, d, h:h + 1], tri, ALU.mult, ALU.mult)
                        M.mm(pO[:, 0:129], Sm[j % 2][:, :], Va[:, blk, :], start=True, stop=False)
                        M.mm(pO[:, 0:129], qT[:, cs], CTb[:, :], start=False, stop=True)
                        M.tt("dve", tmc[:, :], pU[:, 0:129], CT[:, :], ALU.add)
                        M.ts("dve", CT[:, :], tmc[:, :], eL[:, blk, d, h:h + 1], ALU.mult)
                        M.copy("pool", CTb[:, :], CT[:, :])
                        if ctx_out or blk >= cfg.NCB:
                            M.copy("act", hra[d][:, blk, :], pO[:, 0:129], part=True)

                    pipeline(NB, [stA, stB], 1)
                b0 = 0 if ctx_out else cfg.NCB
                for d in range(2):
                    dv_ = hra[d][:, b0:NB, 128]
                    M.ts("dve", den[:, b0:NB], dv_, -1.0, ALU.mult)
                    M.tt("dve", den[:, b0:NB], den[:, b0:NB], dv_, ALU.max)
                    M.tt("dve", den[:, b0:NB], den[:, b0:NB], EMB[:, b0:NB, d, h], ALU.max)
                    M.recip(rr[d][:, b0:NB], den[:, b0:NB])
                for blk in range(b0, NB):
                    hs_ = hs[blk % 2]
                    M.ts("dve", hs_[:, :], hra[0][:, blk, 0:128], rr[0][:, blk:blk + 1], ALU.mult)
                    M.stt(hF[:, blk, :], hra[1][:, blk, 0:128], rr[1][:, blk:blk + 1], hs_[:, :], ALU.mult, ALU.add, part=True)
                M.act(sqh[:, b0:NB, :], hF[:, b0:NB, :], AF.Square)
                M.op("dve", "tensor_reduce", [ssq[:, b0:NB]], [sqh[:, b0:NB, :]], out=ssq[:, b0:NB], in_=sqh[:, b0:NB, :],
                     axis=mybir.AxisListType.X, op=ALU.add)
                M.act(ssq[:, b0:NB], ssq[:, b0:NB], AF.Sqrt, bias=eps_ap, scale=1.0 / 128)
                M.recip(ssq[:, b0:NB], ssq[:, b0:NB])
                for blk in range(b0, NB):
                    cs = slice(blk * 128, (blk + 1) * 128)
                    hn_ = hn[blk % 2]; pT = PS[blk % 2]
                    M.ts("dve", hn_[:, :], hF[:, blk, :], ssq[:, blk:blk + 1], ALU.mult)
                    M.tr(pT[:, 0:128], hn_[:, :], ident)
                    M.stt(ybuf[:, cs], pT[:, 0:128], pvs[:, l, 128 + h:129 + h], sigo[:, cs], ALU.mult, ALU.mult, part=True)
                a0 = 0 if ctx_out else CTX
                M.dma("sp", fm["mly"][h * 128:(h + 1) * 128, a0:T], ybuf[:, a0:T])
        M.barrier()

    def merge_stage(l, tiles):
        with ExitStack() as es:
            Wb = sb(es, "Wb", [128, 12, D], BF16); Wo = sb(es, "Wo", [128, 8, D], BF16)
            xTs = [sb(es, "xT%d" % i, [128, 8, 512], F32) for i in range(2)]
            brs = [sb(es, "br%d" % i, [128, 12, 512], BF16) for i in range(2)]
            gts = [sb(es, "gt%d" % i, [128, 24, 512], BF16) for i in range(2)]
            yT = sb(es, "yT", [128, 8, 512], BF16)

            def ldt(ix):
                t0, N, isctx = tiles[ix]
                M.dma("sp", xTs[ix % 2][:, :, 0:N], xTd[:, :, t0:t0 + N])
                for i, nm in enumerate(("mly", "dfy", "nay")):
                    M.dma("sp", brs[ix % 2][:, i * 4:(i + 1) * 4, 0:N], fm[nm][:, t0:t0 + N].rearrange("(c p) t -> p c t", p=128), part=True)
                M.dma("sp", gts[ix % 2][:, :, 0:N], fm["gate"][:, t0:t0 + N].rearrange("(c p) t -> p c t", p=128))
            ta = [sb(es, "ta%d" % i, [128, 512], F32) for i in range(2)]; tb_ = [sb(es, "tb%d" % i, [128, 512], F32) for i in range(2)]
            wbv = w_br[l].rearrange("i (k p) n -> p i k n", p=128); wov = w_out[l].rearrange("(k p) n -> p k n", p=128)
            for i in range(3):
                for k in range(4):
                    M.dma("pool", Wb[:, i * 4 + k, :], wbv[:, i, k, :], part=True)
            for k in range(8):
                M.dma("pool", Wo[:, k, :], wov[:, k, :], part=True)
            ldt(0)
            for ix, (t0, N, isctx) in enumerate(tiles):
                ci = 1 if isctx else 0
                if ix + 1 < len(tiles):
                    ldt(ix + 1)
                xT = xTs[ix % 2]; br = brs[ix % 2]; gt = gts[ix % 2]
                M.act(gt[:, :, 0:N], gt[:, :, 0:N], AF.Sigmoid)
                for m in range(8):
                    a_ = ta[m % 2]; b_ = tb_[m % 2]
                    for i in range(3):
                        pp = PS[(m % 2) * 3 + i]
                        for k in range(4):
                            M.mm(pp[:, :N], Wb[:, i * 4 + k, m * 128:(m + 1) * 128], br[:, i * 4 + k, :N], start=(k == 0), stop=(k == 3))
                    M.tt("dve", a_[:, :N], PS[(m % 2) * 3][:, :N], gt[:, m, :N], ALU.mult)
                    M.tt("dve", b_[:, :N], PS[(m % 2) * 3 + 1][:, :N], gt[:, 8 + m, :N], ALU.mult)
                    M.tt("pool", a_[:, :N], a_[:, :N], b_[:, :N], ALU.add)
                    M.tt("dve", b_[:, :N], PS[(m % 2) * 3 + 2][:, :N], gt[:, 16 + m, :N], ALU.mult)
                    M.tt("pool", yT[:, m, :N], a_[:, :N], b_[:, :N], ALU.add, part=True)
                g5 = P(l, ci, 5)
                for m2 in range(8):
                    po = PS[6 + m2 % 2]
                    for m in range(8):
                        M.mm(po[:, :N], Wo[:, m, m2 * 128:(m2 + 1) * 128], yT[:, m, :N], start=(m == 0), stop=(m == 7))
                    M.stt(xT[:, m2, :N], po[:, :N], g5[:, m2:m2 + 1], xT[:, m2, :N], ALU.mult, ALU.add, part=True)
                M.dma("sp", xTd[:, :, t0:t0 + N], xT[:, :, 0:N])
        M.barrier()

    def final_stage():
        with ExitStack() as es:
            xT = sb(es, "xT", [128, 8, 512], F32); xn = sb(es, "xn", [128, 8, 512], F32)
            sqb = [sb(es, "sq%d" % i, [128, 512], F32) for i in range(2)]
            rstd = sb(es, "rstd", [128, 512], F32)
            ob = [sb(es, "ob%d" % i, [128, D], F32) for i in range(2)]
            oi = 0
            for (t0, N, isctx) in cfg.tiles:
                if isctx:
                    continue
                M.dma("sp", xT[:, :, 0:N], xTd[:, :, t0:t0 + N])
                for k in range(8):
                    M.act(sqb[k % 2][:, :N], xT[:, k, :N], AF.Square)
                    M.mm(PS[7][:, :N], ones_f[:, :], sqb[k % 2][:, :N], start=(k == 0), stop=(k == 7))
                M.act(rstd[:, :N], PS[7][:, :N], AF.Sqrt, bias=eps_ap, scale=1.0 / D)
                M.recip(rstd[:, :N], rstd[:, :N])
                for k in range(8):
                    M.stt(xn[:, k, :N], xT[:, k, :N], cvs[:, 16 + k:17 + k], rstd[:, :N], ALU.mult, ALU.mult, part=True)
                for tb in range(N // 128):
                    o_ = ob[oi % 2]
                    for k in range(8):
                        pb = PS[(oi % 2) * 2 + k // 4]
                        M.tr(pb[:, (k % 4) * 128:(k % 4 + 1) * 128], xn[:, k, tb * 128:(tb + 1) * 128], ident)
                    M.copy("dve", o_[:, 0:512], PS[(oi % 2) * 2][:, :], part=True)
                    M.copy("act", o_[:, 512:1024], PS[(oi % 2) * 2 + 1][:, :], part=True)
                    r0 = t0 - CTX + tb * 128
                    M.dma("sp", y_out[r0:r0 + 128, :], o_[:, :])
                    oi += 1
        M.barrier()

    for l in range(DEPTH):
        ctx_out = l < DEPTH - 1
        ffn_stage(l, 0, cfg.tiles)
        mixin_stage(l)
        mlstm_stage(l, ctx_out)
        diff_stage(l, ctx_out)
        na_stage(l, ctx_out)
        t2 = cfg.tiles if ctx_out else [t for t in cfg.tiles if not t[2]]
        merge_stage(l, t2)
        ffn_stage(l, 1, t2)
    final_stage()
    gs_.close()
    return nc, M


def host_consts(cfg):
    S = cfg.S
    cmat = np.zeros((128, 3, 128), np.float32)
    i = np.arange(128)
    cmat[:, 0, :] = (i[:, None] == i[None, :]); cmat[:, 1, :] = (i[:, None] <= i[None, :]); cmat[:, 2, :] = (i[:, None] >= i[None, :])
    t = np.arange(S); row = (t // 64).astype(np.float32); col = (t % 64).astype(np.float32)
    freqs = (10000.0 ** (-np.arange(0, 32, 2, dtype=np.float32) / 32)).astype(np.float32)
    ang = np.concatenate([row[:, None] * freqs, col[:, None] * freqs], -1).astype(np.float32)
    rope = np.zeros((2, 128, S), np.float32)
    rope[0] = np.tile(np.cos(ang).T, (4, 1)); rope[1] = np.tile(np.sin(ang).T, (4, 1))
    ind = np.zeros((32, 64, 64), np.float32)
    cp = np.arange(64)[:, None]; c = np.arange(64)[None, :]
    for d in range(31):
        ind[d] = (cp - c + 15 == d)
    qstart = np.clip(c - 8, 0, 48)
    ind[31] = np.where((cp >= qstart) & (cp < qstart + 16), 0.0, NEG)
    return cmat, rope, ind.reshape(32, 4096)


def make_in_maps(cfg, inp):
    cmat, rope, ind = host_consts(cfg)
    DEPTH = cfg.DEPTH
    f = lambda a: np.ascontiguousarray(np.asarray(a, np.float32))
    pv = np.zeros((DEPTH, 128, 136), np.float32)
    for l in range(DEPTH):
        pv[l, :, 0:24] = inp["norm_g"][l].reshape(24, 128).T
        pv[l, :, 24:96] = inp["b_ada"][l].reshape(72, 128).T
        pv[l, :, 96:120] = inp["ml_conv_w"][l].reshape(24, 128).T
        pv[l, :, 120:128] = inp["ml_conv_b"][l].reshape(8, 128).T
        pv[l, :, 128:132] = inp["ml_norm_g"][l].reshape(4, 128).T
        pv[l, :, 132:136] = inp["df_norm_g"][l].reshape(4, 128).T
    gb = np.broadcast_to(np.asarray(inp["ml_gate_b"], np.float32)[:, None, :], (DEPTH, 128, 16))
    rbT = np.ones((DEPTH, 32, 120), np.float32)
    rb = np.asarray(inp["na_rel_bias"], np.float32)
    rbT[:, 0:31, :] = rb[:, :, ::-1, :].reshape(DEPTH, 120, 31).transpose(0, 2, 1)
    shared = {"w_ada": f(inp["w_ada"]), "pv": pv, "gb": f(gb), "ffn_w1": f(inp["ffn_w1"]), "ffn_w2": f(inp["ffn_w2"]),
              "w_in": f(inp["w_in"]), "dfl": f(np.asarray(inp["df_lambda"]).reshape(DEPTH, 1, 256)), "rbT": f(rbT),
              "w_branch": f(inp["w_branch"]), "w_out": f(inp["w_out"]), "cmat": cmat, "rope": rope, "ind": ind}
    maps = []
    for b in range(cfg.NC):
        cv = np.zeros((128, 24), np.float32)
        cv[:, 0:8] = np.asarray(inp["c"][b]).reshape(8, 128).T
        cv[:, 8:16] = np.asarray(inp["c_ctx"]).reshape(8, 128).T
        cv[:, 16:24] = np.asarray(inp["final_g"]).reshape(8, 128).T
        m = dict(shared); m["x"] = f(inp["x"][b]); m["ctx"] = f(inp["ctx"][b]); m["cv"] = cv
        maps.append(m)
    return maps


def kernel(**inputs):
    cfg = Cfg()
    nc, M = build(cfg)
    maps = make_in_maps(cfg, inputs)
    res = run_bass_kernel_spmd(nc, maps, core_ids=list(range(cfg.NC)))
    return np.stack([np.asarray(r["y"], np.float32) for r in res.results], 0)
```

```python
import math
from contextlib import ExitStack
import numpy as np
import concourse.bass as bass
import concourse.mybir as mybir
from concourse.bass_utils import run_bass_kernel_spmd

F32 = mybir.dt.float32
BF16 = mybir.dt.bfloat16
AF = mybir.ActivationFunctionType
ALU = mybir.AluOpType
KQ = 8
EPS = 1e-6
NEG = -30000.0


class Cfg:
    def __init__(s, rows=64, ctx=256, depth=4, dff=2816, ncores=8):
        s.ROWS = rows; s.GW = 64; s.S = rows * 64; s.CTX = ctx; s.T = s.S + ctx
        s.DEPTH = depth; s.DFF = dff; s.NJ = dff // 128; s.D = 1024; s.NB = s.T // 128
        s.NCB = ctx // 128; s.G = rows // 8; s.DIN = 5136 + 3072; s.NC = ncores
        s.tiles = [(0, ctx, True)] + [(ctx + i * 512, 512, False) for i in range(s.S // 512)]


class Buf:
    __slots__ = ("w", "r", "wf")

    def __init__(s):
        s.w = {}; s.r = {}; s.wf = {}


def _merge(d, e):
    for k, v in e.items():
        if d.get(k, 0) < v:
            d[k] = v


class Mach:
    def __init__(s, nc):
        s.nc = nc; s.es = ExitStack(); s.sems = []; s.bufs = {}; s.eng = {}
        for nm, h in (("pe", nc.tensor), ("act", nc.scalar), ("dve", nc.vector), ("pool", nc.gpsimd), ("sp", nc.sync)):
            s.eng[nm] = {"h": h, "si": s.newsem("e_" + nm), "cnt": 0, "seen": {}}
        s.dq = {q: {"sis": [s.newsem("d_%s%d" % (q, i)) for i in range(KQ)], "n": 0} for q in ("sp", "pool")}
        s.ninst = 0

    def newsem(s, name):
        s.sems.append(s.es.enter_context(s.nc.semaphore(name)))
        return len(s.sems) - 1

    def buf(s, ap):
        k = ap.tensor.name
        b = s.bufs.get(k)
        if b is None:
            b = s.bufs[k] = Buf()
        return b

    def wait(s, en, evs):
        E = s.eng[en]
        for si, v in evs.items():
            if en == "pe" and si == E["si"]:
                continue
            if E["seen"].get(si, 0) < v:
                E["h"].wait_ge(s.sems[si], v); E["seen"][si] = v; s.ninst += 1

    def _deps(s, outs, ins, part):
        evs = {}
        for ap in ins:
            _merge(evs, s.buf(ap).w)
        for ap in outs:
            b = s.buf(ap); _merge(evs, b.r)
            _merge(evs, b.wf if part else b.w)
        return evs

    def _rec(s, outs, ins, ev, part=False):
        for ap in ins:
            _merge(s.buf(ap).r, ev)
        for ap in outs:
            _merge(s.buf(ap).w, ev)
            if not part:
                _merge(s.buf(ap).wf, ev)

    def op(s, en, name, outs, ins, *a, part=False, **kw):
        E = s.eng[en]
        s.wait(en, s._deps(outs, ins, part))
        inst = getattr(E["h"], name)(*a, **kw)
        E["cnt"] += 1; s.ninst += 1
        inst.then_inc(s.sems[E["si"]], 1)
        s._rec(outs, ins, {E["si"]: E["cnt"]}, part)

    def dma(s, q, out, in_, part=False, **kw):
        Q = s.dq[q]; slot = Q["n"] % KQ; gen = Q["n"] // KQ; si = Q["sis"][slot]
        evs = s._deps([out], [in_], part)
        if gen > 0:
            _merge(evs, {si: 16 * gen})
        s.wait(q, evs)
        s.eng[q]["h"].dma_start(out=out, in_=in_, **kw).then_inc(s.sems[si], 16)
        Q["n"] += 1; s.ninst += 1
        s._rec([out], [in_], {si: 16 * (gen + 1)}, part)

    def barrier(s):
        evs = {E["si"]: E["cnt"] for E in s.eng.values() if E["cnt"] > 0}
        for Q in s.dq.values():
            for i, si in enumerate(Q["sis"]):
                n = (Q["n"] - i + KQ - 1) // KQ
                if n > 0:
                    evs[si] = 16 * n
        for en in s.eng:
            s.wait(en, evs)

    def mm(s, out, lhsT, rhs, start=True, stop=True):
        s.op("pe", "matmul", [out], [lhsT, rhs], out, lhsT=lhsT, rhs=rhs, start=start, stop=stop)

    def tr(s, out, in_, ident):
        s.op("pe", "transpose", [out], [in_, ident], out, in_, ident)

    def act(s, out, in_, func, bias=None, scale=None, accum_out=None, part=False):
        kw = {}; ins = [in_]; outs = [out]
        if bias is not None:
            kw["bias"] = bias
            if not isinstance(bias, float):
                ins.append(bias)
        if scale is not None:
            kw["scale"] = scale
            if not isinstance(scale, float):
                ins.append(scale)
        if accum_out is not None:
            kw["accum_out"] = accum_out; outs.append(accum_out)
        s.op("act", "activation", outs, ins, out=out, in_=in_, func=func, part=part, **kw)

    def tt(s, en, out, in0, in1, op, part=False):
        s.op(en, "tensor_tensor", [out], [in0, in1], out=out, in0=in0, in1=in1, op=op, part=part)

    def ts(s, en, out, in0, s1, op0, s2=None, op1=None, part=False):
        ins = [in0] + [x for x in (s1, s2) if x is not None and not isinstance(x, float)]
        kw = {"op1": op1} if op1 is not None else {}
        s.op(en, "tensor_scalar", [out], ins, out=out, in0=in0, scalar1=s1, scalar2=s2, op0=op0, part=part, **kw)

    def stt(s, out, in0, scalar, in1, op0, op1, part=False):
        ins = [in0, in1] + ([] if isinstance(scalar, float) else [scalar])
        s.op("dve", "scalar_tensor_tensor", [out], ins, out=out, in0=in0, scalar=scalar, in1=in1, op0=op0, op1=op1, part=part)

    def copy(s, en, out, in_, part=False):
        if en == "act":
            s.op("act", "activation", [out], [in_], out=out, in_=in_, func=AF.Copy, part=part)
        else:
            s.op(en, "tensor_copy", [out], [in_], out=out, in_=in_, part=part)

    def memset(s, en, ap, val, part=False):
        s.op(en, "memset", [ap], [], ap, val, part=part)

    def recip(s, out, in_, part=False):
        s.op("dve", "reciprocal", [out], [in_], out=out, in_=in_, part=part)


def pipeline(n, stages, look):
    for j in range(min(look, n)):
        stages[0](j)
    for j in range(n):
        if j + look < n:
            stages[0](j + look)
        for f in stages[1:]:
            f(j)


def build(cfg):
    nc = bass.Bass("TRN2", target_bir_lowering=False)
    T, S, CTX, D, DFF, NJ, NB, DEPTH = cfg.T, cfg.S, cfg.CTX, cfg.D, cfg.DFF, cfg.NJ, cfg.NB, cfg.DEPTH

    def din(name, shape, dt=F32):
        return nc.dram_tensor(name, list(shape), dt, kind="ExternalInput").ap()

    def dscr(name, shape, dt):
        return nc.dram_tensor(name, list(shape), dt, kind="Internal").ap()

    x_in = din("x", [S, D]); ctx_in = din("ctx", [CTX, D]); cv_in = din("cv", [128, 24])
    w_ada = din("w_ada", [DEPTH, D, 9 * D]); pv_in = din("pv", [DEPTH, 128, 136]); gb_in = din("gb", [DEPTH, 128, 16])
    ffn_w1 = din("ffn_w1", [DEPTH, 2, D, 2 * DFF]); ffn_w2 = din("ffn_w2", [DEPTH, 2, DFF, D])
    w_in = din("w_in", [DEPTH, D, cfg.DIN]); dfl_in = din("dfl", [DEPTH, 1, 256]); rbT_in = din("rbT", [DEPTH, 32, 120])
    w_br = din("w_branch", [DEPTH, 3, 512, D]); w_out = din("w_out", [DEPTH, D, D])
    cmat_in = din("cmat", [128, 3, 128]); rope_in = din("rope", [2, 128, S]); ind_in = din("ind", [32, 4096])
    y_out = nc.dram_tensor("y", [S, D], F32, kind="ExternalOutput").ap()

    xTd = dscr("xTd", [D, T], F32).rearrange("(k p) t -> p k t", p=128)
    fm = {n: dscr("fm_" + n, [r, T], BF16) for n, r in (("mlq", 512), ("mlk", 512), ("mlo", 512), ("dfq", 512), ("dfk", 512),
                                                        ("naq", 512), ("nak", 512), ("gate", 3072), ("mly", 512), ("dfy", 512), ("nay", 512))}
    tm = {n: dscr("tm_" + n, [T, c], dt) for n, c, dt in (("mlv", 512, BF16), ("dfv", 512, BF16), ("nav", 512, BF16), ("mlg", 16, F32))}
    trev_all = dscr("trev", [DEPTH, 120, 4096], BF16)

    M = Mach(nc)
    gs_ = ExitStack()

    _nm = [0]

    def sb(es, name, shape, dt):
        _nm[0] += 1
        return es.enter_context(nc.sbuf_tensor("%s_%d" % (name, _nm[0]), list(shape), dt))

    PS = [gs_.enter_context(nc.psum_tensor("ps%d" % i, [128, 512], F32)) for i in range(8)]
    cm = sb(gs_, "cmat", [128, 3, 128], F32)
    ones_f = sb(gs_, "ones_f", [128, 128], F32); ones_b = sb(gs_, "ones_b", [128, 128], BF16)
    epsc = sb(gs_, "epsc", [128, 2], F32)
    cvs = sb(gs_, "cvs", [128, 24], F32)
    prm = sb(gs_, "prm", [128, DEPTH * 2 * 9 * 8], F32)
    pvs = sb(gs_, "pvs", [128, DEPTH, 136], F32)
    ident = cm[:, 0, :]; triF = cm[:, 1, :]; triB = cm[:, 2, :]
    eps_ap = epsc[:, 0:1]; one_ap = epsc[:, 1:2]

    def P(l, ci, j):
        o = ((l * 2 + ci) * 9 + j) * 8
        return prm[:, o:o + 8]

    M.dma("sp", cm[:, :, :], cmat_in[:, :, :]); M.dma("sp", cvs[:, :], cv_in[:, :])
    for l in range(DEPTH):
        M.dma("sp", pvs[:, l, :], pv_in[l, :, :], part=True)
    M.memset("dve", ones_f[:, :], 1.0); M.memset("dve", ones_b[:, :], 1.0)
    M.memset("dve", epsc[:, 0:1], EPS); M.memset("dve", epsc[:, 1:2], 1.0, part=True)
    with ExitStack() as es:
        scT = sb(es, "scT", [128, 8, 2], F32)
        wa = [sb(es, "wa%d" % i, [128, 8, 1024], F32) for i in range(3)]
        mods = sb(es, "mods", [128, 72, 2], F32)
        for i in range(2):
            M.act(scT[:, :, i], cvs[:, i * 8:(i + 1) * 8], AF.Silu, part=True)
        it = 0
        for l in range(DEPTH):
            wv = w_ada[l].rearrange("(k p) n -> p k n", p=128)
            pm = PS[l % 2]
            for grp in range(9):
                w = wa[it % 3]; it += 1
                for k in range(8):
                    M.dma("sp" if k % 2 else "pool", w[:, k, :], wv[:, k, grp * 1024:(grp + 1) * 1024], part=True)
                for fc in range(8):
                    c0 = (grp * 8 + fc) * 2
                    for k in range(8):
                        M.mm(pm[:, c0:c0 + 2], w[:, k, fc * 128:(fc + 1) * 128], scT[:, k, :], start=(k == 0), stop=(k == 7))
            pmv = pm[:, 0:144].rearrange("p (j i) -> p j i", i=2)
            for i in range(2):
                M.tt("dve", mods[:, :, i], pmv[:, :, i], pvs[:, l, 24:96], ALU.add, part=True)
            for ci in range(2):
                for sub in range(3):
                    mo = sub * 24
                    ng = pvs[:, l, sub * 8:(sub + 1) * 8]
                    M.stt(P(l, ci, sub * 3 + 0), mods[:, mo + 8:mo + 16, ci], 1.0, ng, ALU.add, ALU.mult, part=True)
                    M.copy("dve", P(l, ci, sub * 3 + 1), mods[:, mo:mo + 8, ci], part=True)
                    M.ts("dve", P(l, ci, sub * 3 + 2), mods[:, mo + 16:mo + 24, ci], 0.5 if sub != 1 else 1.0, ALU.mult, part=True)
    M.barrier()
    with ExitStack() as es:
        Ind = sb(es, "Ind", [32, 4096], F32)
        At = [sb(es, "A%d" % i, [32, 120], F32) for i in range(2)]
        Tsb = [sb(es, "Tsb%d" % i, [128, 4096], BF16) for i in range(2)]
        M.dma("sp", Ind[:, :], ind_in[:, :])
        for l in range(DEPTH):
            A = At[l % 2]; Tb = Tsb[l % 2]
            M.dma("sp", A[:, :], rbT_in[l, :, :])
            for cc in range(8):
                ps = PS[2 + cc % 2]
                M.mm(ps[0:120, :], A[:, :], Ind[:, cc * 512:(cc + 1) * 512])
                M.copy("dve" if cc % 2 else "act", Tb[0:120, cc * 512:(cc + 1) * 512], ps[0:120, :], part=True)
            M.dma("sp", trev_all[l, :, :], Tb[0:120, :])
    M.barrier()

    with ExitStack() as es:
        xin = [sb(es, "xin%d" % i, [128, D], F32) for i in range(2)]
        xtb = [sb(es, "xtb%d" % i, [128, 8, 128], F32) for i in range(2)]
        for b in range(NB):
            src = ctx_in[b * 128:(b + 1) * 128, :] if b < cfg.NCB else x_in[(b - cfg.NCB) * 128:(b - cfg.NCB + 1) * 128, :]
            xi = xin[b % 2]; xo = xtb[b % 2]
            M.dma("sp", xi[:, :], src)
            for k in range(8):
                pb = PS[(b % 2) * 2 + k // 4]
                M.tr(pb[:, (k % 4) * 128:(k % 4 + 1) * 128], xi[:, k * 128:(k + 1) * 128], ident)
            M.copy("dve", xo[:, 0:4, :], PS[(b % 2) * 2][:, :].rearrange("p (k t) -> p k t", k=4), part=True)
            M.copy("act", xo[:, 4:8, :], PS[(b % 2) * 2 + 1][:, :].rearrange("p (k t) -> p k t", k=4), part=True)
            M.dma("sp", xTd[:, :, b * 128:(b + 1) * 128], xo[:, :, :])
    M.barrier()

    def rms_mod(xT, hT, N, gsv, shv, sqb, rstd, tmpb):
        pss = PS[7]
        if not callable(xT):
            xt_ = xT
            xT = lambda k: xt_[:, k, :N]
        for k in range(8):
            s_ = sqb[k % 2]
            M.act(s_[:, :N], xT(k), AF.Square)
            M.mm(pss[:, :N], ones_f[:, :], s_[:, :N], start=(k == 0), stop=(k == 7))
        M.act(rstd[:, :N], pss[:, :N], AF.Sqrt, bias=eps_ap, scale=1.0 / D)
        M.recip(rstd[:, :N], rstd[:, :N])
        for k in range(8):
            t_ = tmpb[k % 2]
            M.stt(t_[:, :N], xT(k), gsv[:, k:k + 1], rstd[:, :N], ALU.mult, ALU.mult)
            M.act(hT[:, k, :N], t_[:, :N], AF.Identity, bias=shv[:, k:k + 1], part=True)

    def ffn_stage(l, j, tiles):
        sub = 0 if j == 0 else 2
        with ExitStack() as es:
            NG = min(4, NJ)
            gsz = [NJ // NG + (1 if g < NJ % NG else 0) for g in range(NG)]
            gj0 = [sum(gsz[:g]) for g in range(NG)]
            W1g = [sb(es, "W1g%d" % g, [128, 8, 2, gsz[g] * 128], BF16) for g in range(NG)]
            jg = [(g, jj - gj0[g]) for g in range(NG) for jj in range(gj0[g], gj0[g] + gsz[g])]
            W2 = sb(es, "W2", [128, NJ, D], BF16)
            xTk = [sb(es, "xTk%d" % k, [128, 512], F32) for k in range(8)]
            hT = sb(es, "hT", [128, 8, 512], BF16); uT = sb(es, "uT", [128, NJ, 512], BF16)
            sqb = [sb(es, "sq%d" % i, [128, 512], F32) for i in range(2)]
            rstd = sb(es, "rstd", [128, 512], F32)
            w1v = ffn_w1[l, j].rearrange("(k p) n -> p k n", p=128); w2v = ffn_w2[l, j].rearrange("(k p) n -> p k n", p=128)
            for g in range(NG):
                for k in range(8):
                    for ab in range(2):
                        c0 = ab * DFF + gj0[g] * 128
                        M.dma("pool", W1g[g][:, k, ab, :], w1v[:, k, c0:c0 + gsz[g] * 128], part=True)
            for k in range(NJ):
                M.dma("pool", W2[:, k, :], w2v[:, k, :], part=True)
            for k in range(8):
                M.dma("sp", xTk[k][:, 0:tiles[0][1]], xTd[:, k, tiles[0][0]:tiles[0][0] + tiles[0][1]])
            for ix, (t0, N, isctx) in enumerate(tiles):
                ci = 1 if isctx else 0
                rms_mod(lambda k: xTk[k][:, :N], hT, N, P(l, ci, sub * 3), P(l, ci, sub * 3 + 1), sqb, rstd, sqb)
                for jj in range(NJ):
                    pa = PS[(jj % 2) * 2]; pb = PS[(jj % 2) * 2 + 1]; sl = sqb[jj % 2]
                    g_, jo = jg[jj]
                    for k in range(8):
                        M.mm(pa[:, :N], W1g[g_][:, k, 0, jo * 128:(jo + 1) * 128], hT[:, k, :N], start=(k == 0), stop=(k == 7))
                    for k in range(8):
                        M.mm(pb[:, :N], W1g[g_][:, k, 1, jo * 128:(jo + 1) * 128], hT[:, k, :N], start=(k == 0), stop=(k == 7))
                    M.act(sl[:, :N], pa[:, :N], AF.Silu)
                    M.tt("dve", uT[:, jj, :N], sl[:, :N], pb[:, :N], ALU.mult, part=True)
                hg = P(l, ci, sub * 3 + 2)
                for m in range(8):
                    po = PS[4 + m % 2]
                    for jj in range(NJ):
                        M.mm(po[:, :N], W2[:, jj, m * 128:(m + 1) * 128], uT[:, jj, :N], start=(jj == 0), stop=(jj == NJ - 1))
                    M.stt(xTk[m][:, :N], po[:, :N], hg[:, m:m + 1], xTk[m][:, :N], ALU.mult, ALU.add)
                    M.dma("sp", xTd[:, m, t0:t0 + N], xTk[m][:, 0:N])
                    if ix + 1 < len(tiles):
                        tn, Nn, _ = tiles[ix + 1]
                        M.dma("sp", xTk[m][:, 0:Nn], xTd[:, m, tn:tn + Nn])
        M.barrier()

    FMG = [("mlq", 0, 4, None), ("mlk", 512, 4, None), ("mlo", 1536, 4, None), ("dfq", 2064, 4, 0.125), ("dfk", 2576, 4, None),
           ("naq", 3600, 4, 0.125), ("nak", 4112, 4, None), ("gate", 5136, 24, None)]
    TMG = [("mlv", 1024, 512), ("dfv", 3088, 512), ("nav", 4624, 512), ("mlg", 2048, 16)]

    def mixin_stage(l):
        with ExitStack() as es:
            wb_ = [0, 2064, 3600, 5136, 6672, cfg.DIN]
            Wt = [sb(es, "Win%d" % i, [128, 8, wb_[i + 1] - wb_[i]], BF16) for i in range(5)]
            Wr = sb(es, "Wrot", [128, 8, 1024], BF16)

            def Wsl(k, col, n):
                i = max(ii for ii in range(5) if wb_[ii] <= col)
                assert col + n <= wb_[i + 1]
                return Wt[i][:, k, col - wb_[i]:col - wb_[i] + n]
            xT = sb(es, "xT", [128, 8, 512], F32); hT = sb(es, "hT", [128, 8, 512], BF16)
            sqb = [sb(es, "sq%d" % i, [128, 512], F32) for i in range(2)]
            rstd = sb(es, "rstd", [128, 512], F32)
            stg = [sb(es, "stg%d" % i, [128, 4, 512], BF16) for i in range(2)]
            stt_ = [sb(es, "stt%d" % i, [128, 512], BF16) for i in range(2)]
            stf = sb(es, "stf", [128, 16], F32)
            rC = sb(es, "rC", [128, 512], F32); rS = sb(es, "rS", [128, 512], F32)
            wv = w_in[l].rearrange("(k p) n -> p k n", p=128)
            for i in range(5):
                wd = wb_[i + 1] - wb_[i]
                npc = 2 if wd > 2048 else 1
                for k in range(8):
                    for c in range(npc):
                        a_ = c * (wd // npc); b_ = wd if c == npc - 1 else (c + 1) * (wd // npc)
                        M.dma("pool", Wt[i][:, k, a_:b_], wv[:, k, wb_[i] + a_:wb_[i] + b_], part=True)
            for k in range(8):
                sv = Wt[1][:, k, 0:1024].rearrange("p (b h d) -> p b h d", b=16, h=2, d=32)
                dv = Wr[:, k, :].rearrange("p (b h d) -> p b h d", b=16, h=2, d=32)
                M.ts("dve", dv[:, :, 0, :], sv[:, :, 1, :], -1.0, ALU.mult, part=True)
                M.copy("pool", dv[:, :, 1, :], sv[:, :, 0, :], part=True)
            si = 0; ti = 0; ev = 0
            M.dma("sp", xT[:, :, 0:cfg.tiles[0][1]], xTd[:, :, cfg.tiles[0][0]:cfg.tiles[0][0] + cfg.tiles[0][1]])
            for ix, (t0, N, isctx) in enumerate(cfg.tiles):
                ci = 1 if isctx else 0
                if not isctx:
                    M.dma("sp", rC[:, :N], rope_in[0, :, t0 - CTX:t0 - CTX + N]); M.dma("sp", rS[:, :N], rope_in[1, :, t0 - CTX:t0 - CTX + N])
                rms_mod(xT, hT, N, P(l, ci, 3), P(l, ci, 4), sqb, rstd, sqb)
                if ix + 1 < len(cfg.tiles):
                    tn, Nn, _ = cfg.tiles[ix + 1]
                    M.dma("sp", xT[:, :, 0:Nn], xTd[:, :, tn:tn + Nn])
                for (name, c0, nch, scl) in FMG:
                    rope = name in ("dfq", "dfk") and not isctx
                    for c in range(nch):
                        st = stg[si % 2]
                        pa = PS[(ev % 2) * 2]; pb = PS[(ev % 2) * 2 + 1]; ev += 1
                        col = c0 + c * 128
                        for k in range(8):
                            M.mm(pa[:, :N], Wsl(k, col, 128), hT[:, k, :N], start=(k == 0), stop=(k == 7))
                        if rope:
                            rc = col - 2064
                            for k in range(8):
                                M.mm(pb[:, :N], Wr[:, k, rc:rc + 128], hT[:, k, :N], start=(k == 0), stop=(k == 7))
                            t1 = sqb[0]; t2 = sqb[1]
                            M.stt(t1[:, :N], pa[:, :N], float(scl or 1.0), rC[:, :N], ALU.mult, ALU.mult)
                            M.stt(t2[:, :N], pb[:, :N], float(scl or 1.0), rS[:, :N], ALU.mult, ALU.mult)
                            M.tt("pool", st[:, c % 4, :N], t1[:, :N], t2[:, :N], ALU.add, part=True)
                        elif ev % 2 == 0:
                            M.act(st[:, c % 4, :N], pa[:, :N], AF.Copy, scale=float(scl or 1.0), part=True)
                        else:
                            M.ts("dve", st[:, c % 4, :N], pa[:, :N], float(scl or 1.0), ALU.mult, part=True)
                        if c % 4 == 3:
                            r0 = (c - 3) * 128
                            M.dma("sp", fm[name][r0:r0 + 512, t0:t0 + N].rearrange("(c p) t -> p c t", p=128), st[:, :, 0:N])
                            si += 1
                for tb in range(N // 128):
                    for (name, c0, ncol) in TMG:
                        pa = PS[4 + ti % 2]
                        for k in range(8):
                            M.mm(pa[:, :ncol], hT[:, k, tb * 128:(tb + 1) * 128], Wsl(k, c0, ncol), start=(k == 0), stop=(k == 7))
                        dst = tm[name][t0 + tb * 128:t0 + (tb + 1) * 128, :]
                        if name == "mlg":
                            M.copy("dve", stf[:, :], pa[:, 0:16]); M.dma("sp", dst, stf[:, :])
                        else:
                            so = stt_[ti % 2]
                            if ti % 2 == 0:
                                M.copy("act", so[:, :], pa[:, :])
                            else:
                                M.copy("dve", so[:, :], pa[:, :])
                            M.dma("sp", dst, so[:, :])
                        ti += 1
        M.barrier()

    def diff_stage(l, ctx_out):
        lam_init = 0.8 - 0.6 * math.exp(-0.3 * l)
        with ExitStack() as es:
            lv = sb(es, "lv", [1, 256], F32); lt = sb(es, "lt", [1, 8], F32)
            nlam = sb(es, "nlam", [128, 1], F32); ngd = sb(es, "ngd", [128, 4], F32)
            kzs = [[sb(es, "kz%d_%d" % (i, jb), [128, T], BF16) for i in range(2)] for jb in range(2)]
            Vs = [sb(es, "V%d" % jb, [128, NB, 128], BF16) for jb in range(2)]
            qT = [sb(es, "qT%d" % i, [128, 512], BF16) for i in range(2)]
            pt = [sb(es, "pt%d" % i, [128, 512], BF16) for i in range(4)]
            acc = [sb(es, "acc%d" % i, [128, 512], F32) for i in range(2)]
            for jb in range(2):
                M.memset("pool", kzs[jb][0][64:128, :], 0.0); M.memset("pool", kzs[jb][1][0:64, :], 0.0)

            def ldh(h):
                for m in range(2):
                    M.dma("sp", kzs[h % 2][m][m * 64:(m + 1) * 64, :], fm["dfk"][h * 128 + m * 64:h * 128 + (m + 1) * 64, :], part=True)
                M.dma("sp", Vs[h % 2][:, :, :], tm["dfv"][:, h * 128:(h + 1) * 128].rearrange("(b p) c -> p b c", p=128))
            r1 = sb(es, "r1", [128, 512], F32); r2 = sb(es, "r2", [128, 512], F32)
            t1 = sb(es, "t1", [128, 512], F32); t2 = sb(es, "t2", [128, 512], F32)
            yb = [sb(es, "yb%d" % i, [128, 512], BF16) for i in range(2)]
            M.dma("sp", lv[:, :], dfl_in[l, :, :])
            lp = sb(es, "lp", [1, 128], F32)
            for i in range(2):
                M.tt("dve", lp[:, 64 * i:64 * i + 64], lv[:, 128 * i:128 * i + 64], lv[:, 128 * i + 64:128 * i + 128], ALU.mult, part=True)
            M.op("dve", "tensor_reduce", [lt[:, 0:2]], [lp[:, :]], out=lt[:, 0:2], in_=lp[:, :].rearrange("p (a b) -> p a b", a=2),
                 axis=mybir.AxisListType.X, op=ALU.add)
            M.act(lt[:, 2:4], lt[:, 0:2], AF.Exp)
            M.tt("dve", lt[:, 4:5], lt[:, 3:4], lt[:, 2:3], ALU.subtract)
            M.ts("dve", lt[:, 5:6], lt[:, 4:5], -lam_init, ALU.add)
            M.mm(PS[7][:, 0:1], ones_f[0:1, :], lt[0:1, 5:6])
            M.copy("dve", nlam[:, :], PS[7][:, 0:1])
            M.ts("dve", ngd[:, :], pvs[:, l, 132:136], 1.0 - lam_init, ALU.mult)
            qi = 0
            items = [(h, t0, N, isctx) for h in range(4) for (t0, N, isctx) in cfg.tiles if ctx_out or not isctx]

            def ldq(ii):
                hh, tt0, NN, _ = items[ii]
                M.dma("sp", qT[ii % 2][:, :NN], fm["dfq"][hh * 128:(hh + 1) * 128, tt0:tt0 + NN])

            ldh(0); ldq(0)
            for ii, (h, t0, N, isctx) in enumerate(items):
                if True:
                    if (ii == 0 or items[ii - 1][0] != h) and h + 1 < 4:
                        ldh(h + 1)
                    kz = kzs[h % 2]; V = Vs[h % 2]
                    if ii + 1 < len(items):
                        ldq(ii + 1)
                    q = qT[qi % 2]; yo = yb[qi % 2]; qi += 1
                    kbs = list(range(cfg.NCB)) if isctx else list(range(NB))
                    O = [PS[0], PS[1]]
                    its = [(i, kb, m) for i, kb in enumerate(kbs) for m in range(2)]
                    nk = len(kbs)

                    def qk(j):
                        i, kb, m = its[j]
                        M.mm(PS[2 + j % 4][:, :N], kz[m][:, kb * 128:(kb + 1) * 128], q[:, :N])

                    def ex(j):
                        M.act(pt[j % 4][:, :N], PS[2 + j % 4][:, :N], AF.Exp)

                    def pv(j):
                        i, kb, m = its[j]
                        M.mm(O[m][:, :N], V[:, kb, :], pt[j % 4][:, :N], start=(i == 0), stop=(i == nk - 1))
                        if m == 0:
                            M.mm(PS[6][:, :N], ones_b[:, :], pt[j % 4][:, :N], start=(i == 0), stop=(i == nk - 1))
                        elif i == 0:
                            M.copy("dve", acc[1][:, :N], pt[j % 4][:, :N])
                        else:
                            M.tt("dve", acc[1][:, :N], acc[1][:, :N], pt[j % 4][:, :N], ALU.add)

                    pipeline(len(its), [qk, ex, pv], 3)
                    M.recip(r1[:, :N], PS[6][:, :N])
                    M.mm(PS[7][:, :N], ones_f[:, :], acc[1][:, :N])
                    M.recip(r2[:, :N], PS[7][:, :N])
                    M.tt("dve", t1[:, :N], O[0][:, :N], r1[:, :N], ALU.mult)
                    M.tt("dve", t2[:, :N], O[1][:, :N], r2[:, :N], ALU.mult)
                    M.stt(t1[:, :N], t2[:, :N], nlam[:, 0:1], t1[:, :N], ALU.mult, ALU.add)
                    M.act(t2[:, :N], t1[:, :N], AF.Square)
                    M.mm(PS[7][:, :N], ones_f[:, :], t2[:, :N])
                    M.act(r1[:, :N], PS[7][:, :N], AF.Sqrt, bias=eps_ap, scale=1.0 / 128)
                    M.recip(r1[:, :N], r1[:, :N])
                    M.stt(yo[:, :N], t1[:, :N], ngd[:, h:h + 1], r1[:, :N], ALU.mult, ALU.mult)
                    M.dma("sp", fm["dfy"][h * 128:(h + 1) * 128, t0:t0 + N], yo[:, :N])
        M.barrier()

    def na_stage(l, ctx_out):
        ROWS, G = cfg.ROWS, cfg.G
        trev = trev_all[l]
        with ExitStack() as es:
            BTs = [sb(es, "BT%d" % i, [128, 48, 512], BF16) for i in range(2)]
            kzs = [[sb(es, "kz%d_%d" % (i, jb), [128, T], BF16) for i in range(2)] for jb in range(2)]
            Vzs = [[sb(es, "Vz%d_%d" % (i, jb), [128, NB, 128], BF16) for i in range(2)] for jb in range(2)]
            oz = [sb(es, "oz%d" % i, [128, 128], BF16) for i in range(2)]
            qT = [sb(es, "qT%d" % i, [128, 512], BF16) for i in range(2)]
            pt = [sb(es, "pt%d" % i, [128, 512], BF16) for i in range(4)]
            sf = [sb(es, "sf%d" % i, [128, 512], F32) for i in range(4)]
            r1 = sb(es, "r1", [128, 512], F32)
            yb = [sb(es, "yb%d" % i, [128, 512], BF16) for i in range(2)]
            for e in range(2):
                for jb in range(2):
                    M.memset("pool", kzs[jb][e][(1 - e) * 64:(2 - e) * 64, :], 0.0)
                    M.memset("pool", Vzs[jb][e][:, :, :], 0.0)
                M.memset("pool", oz[e][:, :], 0.0); M.memset("pool", oz[e][:, e * 64:(e + 1) * 64], 1.0)

            nbt = [0]

            def ldc(c):
                kz = kzs[c % 2]; Vz = Vzs[c % 2]; BT = BTs[c % 2]
                for e in range(2):
                    M.dma("sp", kz[e][e * 64:(e + 1) * 64, :], fm["nak"][c * 128 + e * 64:c * 128 + (e + 1) * 64, :], part=True)
                    M.dma("sp", Vz[e][:, :, e * 64:(e + 1) * 64],
                          tm["nav"][:, c * 128 + e * 64:c * 128 + (e + 1) * 64].rearrange("(b p) c -> p b c", p=128), part=True)
                M.memset("dve", BT[:, :, :], NEG)
                for pat in range(3):
                    g = (0, 1, G - 1)[pat]
                    rs0 = min(max(8 * g - 4, 0), ROWS - 16)
                    for e in range(2):
                        h = 2 * c + e
                        for kb in range(8):
                            for kr2 in range(2):
                                kr = rs0 + 2 * kb + kr2
                                val = [qr for qr in range(8) if 0 <= kr - min(max(8 * g + qr - 4, 0), ROWS - 8) < 8]
                                if not val:
                                    continue
                                qa, qb = val[0], val[-1] + 1
                                assert val == list(range(qa, qb))
                                ja = 7 - kr + 8 * g + qa
                                assert 0 <= ja and ja + (qb - qa) - 1 <= 14
                                src = trev[h * 15 + ja:h * 15 + ja + (qb - qa), :].rearrange("j (cp c) -> cp j c", c=64)
                                dst = BT[kr2 * 64:(kr2 + 1) * 64, (pat * 2 + e) * 8 + kb, qa * 64:qb * 64].rearrange("p (j c) -> p j c", c=64)
                                nbt[0] += 1
                                M.dma("sp" if nbt[0] % 2 else "pool", dst, src, part=True)

            qi = 0
            ldc(0)
            for c in range(4):
                if c + 1 < 4:
                    ldc(c + 1)
                kz = kzs[c % 2]; Vz = Vzs[c % 2]; BT = BTs[c % 2]
                qtiles = [(CTX + g * 512, 512, g) for g in range(G)] + ([(0, CTX, -1)] if ctx_out else [])
                for qx, (t0, N, g) in enumerate(qtiles):
                    if qx == 0 and c == 0:
                        M.dma("sp", qT[qi % 2][:, :N], fm["naq"][c * 128:(c + 1) * 128, t0:t0 + N])
                    nx = (c, qx + 1) if qx + 1 < len(qtiles) else ((c + 1, 0) if c + 1 < 4 else None)
                    if nx is not None:
                        tn, Nn, _ = qtiles[nx[1]]
                        M.dma("sp", qT[(qi + 1) % 2][:, :Nn], fm["naq"][nx[0] * 128:(nx[0] + 1) * 128, tn:tn + Nn])
                    q = qT[qi % 2]; yo = yb[qi % 2]; qi += 1
                    if g >= 0:
                        pat = 0 if g == 0 else (2 if g == G - 1 else 1)
                        rs0 = min(max(8 * g - 4, 0), ROWS - 16)
                        blks = [((CTX + rs0 * 64) // 128 + kb, kb) for kb in range(8)] + [(b, -1) for b in range(cfg.NCB)]
                    else:
                        blks = [(b, -1) for b in range(cfg.NCB)]
                    O = PS[0]; Dn = PS[1]
                    its = [(e, i, blk, kb) for e in range(2) for i, (blk, kb) in enumerate(blks)]
                    nk = len(blks)

                    def qk(j):
                        e, i, blk, kb = its[j]
                        M.mm(PS[2 + j % 6][:, :N], kz[e][:, blk * 128:(blk + 1) * 128], q[:, :N])

                    def ex(j):
                        e, i, blk, kb = its[j]
                        ps = PS[2 + j % 6]; p_ = pt[j % 4]; s_ = sf[j % 4]
                        if kb >= 0:
                            M.tt("dve", s_[:, :N], ps[:, :N], BT[:, (pat * 2 + e) * 8 + kb, :N], ALU.add)
                            M.act(p_[:, :N], s_[:, :N], AF.Exp)
                        else:
                            M.act(p_[:, :N], ps[:, :N], AF.Exp)

                    def pv(j):
                        e, i, blk, kb = its[j]
                        p_ = pt[j % 4]
                        first = (j == 0); last = (j == len(its) - 1)
                        M.mm(O[:, :N], Vz[e][:, blk, :], p_[:, :N], start=first, stop=last)
                        M.mm(Dn[:, :N], oz[e][:, :], p_[:, :N], start=first, stop=last)

                    pipeline(len(its), [qk, ex, pv], 3)
                    M.recip(r1[:, :N], Dn[:, :N])
                    M.tt("dve", yo[:, :N], O[:, :N], r1[:, :N], ALU.mult)
                    M.dma("sp", fm["nay"][c * 128:(c + 1) * 128, t0:t0 + N], yo[:, :N])
        M.barrier()

    def mlstm_stage(l, ctx_out):
        with ExitStack() as es:
            Gt = sb(es, "Gt", [128, NB, 16], F32); gbs = sb(es, "gbs", [128, 16], F32)
            LF = sb(es, "LF", [128, NB, 2, 4], F32); Bc = sb(es, "Bc", [128, NB, 2, 4], F32)
            eL = sb(es, "eL", [128, NB, 2, 4], F32); EK = sb(es, "EK", [128, NB, 2, 4], F32); EMB = sb(es, "EMB", [128, NB, 2, 4], F32)
            raw = sb(es, "raw", [128, T], BF16); cvb = sb(es, "cvb", [128, T], F32); k32 = sb(es, "k32", [128, T], F32)
            qT = sb(es, "qT", [128, T], BF16); kT = sb(es, "kT", [128, T], BF16); sigo = sb(es, "sigo", [128, T], BF16)
            Va = sb(es, "Va", [128, NB, 129], BF16); hF = sb(es, "hF", [128, NB, 128], F32); ybuf = sb(es, "ybuf", [128, T], BF16)
            CT = sb(es, "CT", [128, 129], F32); CTb = sb(es, "CTb", [128, 129], BF16); tmc = sb(es, "tmc", [128, 129], F32)
            ktd = [sb(es, "ktd%d" % i, [128, 128], BF16) for i in range(2)]
            Sm = [sb(es, "Sm%d" % i, [128, 128], BF16) for i in range(2)]
            hs = [sb(es, "hs%d" % i, [128, 128], F32) for i in range(2)]
            hn = [sb(es, "hn%d" % i, [128, 128], F32) for i in range(2)]
            smv = [sb(es, "sm%d" % i, [128, 8], F32) for i in range(2)]
            sqh = sb(es, "sqh", [128, NB, 128], F32); ssq = sb(es, "ssq", [128, NB], F32)
            hra = [sb(es, "hra%d" % i, [128, NB, 129], F32) for i in range(2)]
            den = sb(es, "den", [128, NB], F32); rr = [sb(es, "rr%d" % i, [128, NB], F32) for i in range(2)]
            M.dma("sp", Gt[:, :, :], tm["mlg"].rearrange("(b p) c -> p b c", p=128)); M.dma("sp", gbs[:, :], gb_in[l, :, :])
            for j in range(16):
                M.ts("dve", Gt[:, :, j], Gt[:, :, j], gbs[:, j:j + 1], ALU.add, part=True)
            for d in range(2):
                M.act(LF[:, :, d, :], Gt[:, :, d * 8 + 4:d * 8 + 8], AF.Exp, scale=-1.0, part=True)
            M.act(LF[:, :, :, :], LF[:, :, :, :], AF.Ln, bias=one_ap)
            M.ts("dve", LF[:, :, :, :], LF[:, :, :, :], -1.0, ALU.mult)
            M.mm(PS[0][:, 0:NB * 4], triF, LF[:, :, 0, :]); M.mm(PS[1][:, 0:NB * 4], triB, LF[:, :, 1, :])
            M.mm(PS[2][:, 0:NB * 8], ones_f[:, :], LF[:, :, :, :])
            M.act(eL[:, :, :, :], PS[2][:, 0:NB * 8].rearrange("p (b d h) -> p b d h", d=2, h=4), AF.Exp)
            for d in range(2):
                pv_ = PS[d][:, 0:NB * 4].rearrange("p (b h) -> p b h", h=4)
                M.copy("dve", Bc[:, :, d, :], pv_, part=True)
                M.tt("dve", EK[:, :, d, :], Gt[:, :, d * 8:d * 8 + 4], pv_, ALU.subtract, part=True)
            M.act(EK[:, :, :, :], EK[:, :, :, :], AF.Exp)
            M.act(EMB[:, :, :, :], Bc[:, :, :, :], AF.Exp, scale=-1.0)
            segs = ((0, CTX), (CTX, T))
            for h in range(4):
                for (name, chunk, isq) in (("mlq", h, True), ("mlk", 4 + h, False)):
                    M.dma("sp", raw[:, :], fm[name][h * 128:(h + 1) * 128, :])
                    cwv = lambda j: pvs[:, l, 96 + j * 8 + chunk:96 + j * 8 + chunk + 1]
                    for (a, b) in segs:
                        M.ts("dve", cvb[:, a:b], raw[:, a:b], cwv(1), ALU.mult, pvs[:, l, 120 + chunk:121 + chunk], ALU.add, part=True)
                        M.stt(cvb[:, a + 1:b], raw[:, a:b - 1], cwv(0), cvb[:, a + 1:b], ALU.mult, ALU.add, part=True)
                        M.stt(cvb[:, a:b - 1], raw[:, a + 1:b], cwv(2), cvb[:, a:b - 1], ALU.mult, ALU.add, part=True)
                    if isq:
                        M.act(k32[:, :], cvb[:, :], AF.Silu)
                        M.ts("dve", qT[:, :], k32[:, :], 128.0 ** -0.5, ALU.mult)
                    else:
                        M.act(k32[:, :], cvb[:, :], AF.Silu)
                        M.copy("pool", kT[:, :], k32[:, :])
                M.dma("sp", raw[:, :], fm["mlo"][h * 128:(h + 1) * 128, :])
                M.act(sigo[:, :], raw[:, :], AF.Sigmoid)
                M.dma("sp", Va[:, :, 0:128], tm["mlv"][:, h * 128:(h + 1) * 128].rearrange("(b p) c -> p b c", p=128))
                M.memset("pool", Va[:, :, 128:129], 1.0, part=True)
                for d in range(2):
                    order = list(range(NB)) if d == 0 else (list(range(cfg.NCB - 1, -1, -1)) + list(range(NB - 1, cfg.NCB - 1, -1)))
                    tri = triF if d == 0 else triB
                    M.memset("pool", CT[:, :], 0.0); M.memset("pool", CTb[:, :], 0.0)

                    def stA(j, d=d, order=order, tri=tri):
                        blk = order[j]; cs = slice(blk * 128, (blk + 1) * 128)
                        ek = EK[:, blk, d, h:h + 1]
                        pk = PS[j % 2]; pU = PS[2 + j % 2]
                        M.tr(pk[:, 0:128], k32[:, cs], ident)
                        M.ts("dve", ktd[j % 2][:, :], pk[:, 0:128], ek, ALU.mult)
                        M.mm(pU[:, 0:129], ktd[j % 2][:, :], Va[:, blk, :])

                    def stB(j, d=d, order=order, tri=tri):
                        blk = order[j]; cs = slice(blk * 128, (blk + 1) * 128)
                        pO = PS[5 + j % 2]; pU = PS[2 + j % 2]; pT = PS[7]; pS = PS[4]
                        hs_ = hs[j % 2]; hn_ = hn[j % 2]; sm_ = smv[j % 2]
                        M.mm(pS[:, 0:128], kT[:, cs], qT[:, cs])
                        M.stt(Sm[j % 2][:, :], pS[:, 0:128], EK[:, blk, d, h:h + 1], tri, ALU.mult, ALU.mult)
                        M.mm(pO[:, 0:129], Sm[j % 2][:, :], Va[:, blk, :], start=True, stop=False)
                        M.mm(pO[:, 0:129], qT[:, cs], CTb[:, :], start=False, stop=True)
                        M.tt("dve", tmc[:, :], pU[:, 0:129], CT[:, :], ALU.add)
                        M.ts("dve", CT[:, :], tmc[:, :], eL[:, blk, d, h:h + 1], ALU.mult)
                        M.copy("pool", CTb[:, :], CT[:, :])
                        if ctx_out or blk >= cfg.NCB:
                            M.copy("act", hra[d][:, blk, :], pO[:, 0:129], part=True)

                    pipeline(NB, [stA, stB], 1)
                b0 = 0 if ctx_out else cfg.NCB
                for d in range(2):
                    dv_ = hra[d][:, b0:NB, 128]
                    M.ts("dve", den[:, b0:NB], dv_, -1.0, ALU.mult)
                    M.tt("dve", den[:, b0:NB], den[:, b0:NB], dv_, ALU.max)
                    M.tt("dve", den[:, b0:NB], den[:, b0:NB], EMB[:, b0:NB, d, h], ALU.max)
                    M.recip(rr[d][:, b0:NB], den[:, b0:NB])
                for blk in range(b0, NB):
                    hs_ = hs[blk % 2]
                    M.ts("dve", hs_[:, :], hra[0][:, blk, 0:128], rr[0][:, blk:blk + 1], ALU.mult)
                    M.stt(hF[:, blk, :], hra[1][:, blk, 0:128], rr[1][:, blk:blk + 1], hs_[:, :], ALU.mult, ALU.add, part=True)
                M.act(sqh[:, b0:NB, :], hF[:, b0:NB, :], AF.Square)
                M.op("dve", "tensor_reduce", [ssq[:, b0:NB]], [sqh[:, b0:NB, :]], out=ssq[:, b0:NB], in_=sqh[:, b0:NB, :],
                     axis=mybir.AxisListType.X, op=ALU.add)
                M.act(ssq[:, b0:NB], ssq[:, b0:NB], AF.Sqrt, bias=eps_ap, scale=1.0 / 128)
                M.recip(ssq[:, b0:NB], ssq[:, b0:NB])
                def fA(j):
                    blk = b0 + j
                    M.ts("dve", hn[j % 2][:, :], hF[:, blk, :], ssq[:, blk:blk + 1], ALU.mult)
                    M.tr(PS[j % 4][:, 0:128], hn[j % 2][:, :], ident)

                def fB(j):
                    blk = b0 + j; cs = slice(blk * 128, (blk + 1) * 128)
                    M.stt(ybuf[:, cs], PS[j % 4][:, 0:128], pvs[:, l, 128 + h:129 + h], sigo[:, cs], ALU.mult, ALU.mult, part=True)

                pipeline(NB - b0, [fA, fB], 2)
                a0 = 0 if ctx_out else CTX
                M.dma("sp", fm["mly"][h * 128:(h + 1) * 128, a0:T], ybuf[:, a0:T])
        M.barrier()

    def merge_stage(l, tiles):
        with ExitStack() as es:
            Wb = sb(es, "Wb", [128, 12, D], BF16); Wo = sb(es, "Wo", [128, 8, D], BF16)
            xTs = [sb(es, "xT%d" % i, [128, 8, 512], F32) for i in range(2)]
            brs = [sb(es, "br%d" % i, [128, 12, 512], BF16) for i in range(2)]
            gts = [sb(es, "gt%d" % i, [128, 24, 512], BF16) for i in range(2)]
            yT = sb(es, "yT", [128, 8, 512], BF16)

            def ldt(ix):
                t0, N, isctx = tiles[ix]
                M.dma("sp", xTs[ix % 2][:, :, 0:N], xTd[:, :, t0:t0 + N])
                for i, nm in enumerate(("mly", "dfy", "nay")):
                    M.dma("sp", brs[ix % 2][:, i * 4:(i + 1) * 4, 0:N], fm[nm][:, t0:t0 + N].rearrange("(c p) t -> p c t", p=128), part=True)
                M.dma("sp", gts[ix % 2][:, :, 0:N], fm["gate"][:, t0:t0 + N].rearrange("(c p) t -> p c t", p=128))
            ta = [sb(es, "ta%d" % i, [128, 512], F32) for i in range(2)]; tb_ = [sb(es, "tb%d" % i, [128, 512], F32) for i in range(2)]
            wbv = w_br[l].rearrange("i (k p) n -> p i k n", p=128); wov = w_out[l].rearrange("(k p) n -> p k n", p=128)
            for i in range(3):
                for k in range(4):
                    M.dma("pool", Wb[:, i * 4 + k, :], wbv[:, i, k, :], part=True)
            for k in range(8):
                M.dma("pool", Wo[:, k, :], wov[:, k, :], part=True)
            ldt(0)
            for ix, (t0, N, isctx) in enumerate(tiles):
                ci = 1 if isctx else 0
                if ix + 1 < len(tiles):
                    ldt(ix + 1)
                xT = xTs[ix % 2]; br = brs[ix % 2]; gt = gts[ix % 2]
                M.act(gt[:, :, 0:N], gt[:, :, 0:N], AF.Sigmoid)
                for m in range(8):
                    a_ = ta[m % 2]; b_ = tb_[m % 2]
                    for i in range(3):
                        pp = PS[(m % 2) * 3 + i]
                        for k in range(4):
                            M.mm(pp[:, :N], Wb[:, i * 4 + k, m * 128:(m + 1) * 128], br[:, i * 4 + k, :N], start=(k == 0), stop=(k == 3))
                    M.tt("dve", a_[:, :N], PS[(m % 2) * 3][:, :N], gt[:, m, :N], ALU.mult)
                    M.tt("dve", b_[:, :N], PS[(m % 2) * 3 + 1][:, :N], gt[:, 8 + m, :N], ALU.mult)
                    M.tt("pool", a_[:, :N], a_[:, :N], b_[:, :N], ALU.add)
                    M.tt("dve", b_[:, :N], PS[(m % 2) * 3 + 2][:, :N], gt[:, 16 + m, :N], ALU.mult)
                    M.tt("pool", yT[:, m, :N], a_[:, :N], b_[:, :N], ALU.add, part=True)
                g5 = P(l, ci, 5)
                for m2 in range(8):
                    po = PS[6 + m2 % 2]
                    for m in range(8):
                        M.mm(po[:, :N], Wo[:, m, m2 * 128:(m2 + 1) * 128], yT[:, m, :N], start=(m == 0), stop=(m == 7))
                    M.stt(xT[:, m2, :N], po[:, :N], g5[:, m2:m2 + 1], xT[:, m2, :N], ALU.mult, ALU.add, part=True)
                M.dma("sp", xTd[:, :, t0:t0 + N], xT[:, :, 0:N])
        M.barrier()

    def final_stage():
        with ExitStack() as es:
            xT = sb(es, "xT", [128, 8, 512], F32); xn = sb(es, "xn", [128, 8, 512], F32)
            sqb = [sb(es, "sq%d" % i, [128, 512], F32) for i in range(2)]
            rstd = sb(es, "rstd", [128, 512], F32)
            ob = [sb(es, "ob%d" % i, [128, D], F32) for i in range(2)]
            oi = 0
            for (t0, N, isctx) in cfg.tiles:
                if isctx:
                    continue
                M.dma("sp", xT[:, :, 0:N], xTd[:, :, t0:t0 + N])
                for k in range(8):
                    M.act(sqb[k % 2][:, :N], xT[:, k, :N], AF.Square)
                    M.mm(PS[7][:, :N], ones_f[:, :], sqb[k % 2][:, :N], start=(k == 0), stop=(k == 7))
                M.act(rstd[:, :N], PS[7][:, :N], AF.Sqrt, bias=eps_ap, scale=1.0 / D)
                M.recip(rstd[:, :N], rstd[:, :N])
                for k in range(8):
                    M.stt(xn[:, k, :N], xT[:, k, :N], cvs[:, 16 + k:17 + k], rstd[:, :N], ALU.mult, ALU.mult, part=True)
                for tb in range(N // 128):
                    o_ = ob[oi % 2]
                    for k in range(8):
                        pb = PS[(oi % 2) * 2 + k // 4]
                        M.tr(pb[:, (k % 4) * 128:(k % 4 + 1) * 128], xn[:, k, tb * 128:(tb + 1) * 128], ident)
                    M.copy("dve", o_[:, 0:512], PS[(oi % 2) * 2][:, :], part=True)
                    M.copy("act", o_[:, 512:1024], PS[(oi % 2) * 2 + 1][:, :], part=True)
                    r0 = t0 - CTX + tb * 128
                    M.dma("sp", y_out[r0:r0 + 128, :], o_[:, :])
                    oi += 1
        M.barrier()

    for l in range(DEPTH):
        ctx_out = l < DEPTH - 1
        ffn_stage(l, 0, cfg.tiles)
        mixin_stage(l)
        mlstm_stage(l, ctx_out)
        diff_stage(l, ctx_out)
        na_stage(l, ctx_out)
        t2 = cfg.tiles if ctx_out else [t for t in cfg.tiles if not t[2]]
        merge_stage(l, t2)
        ffn_stage(l, 1, t2)
    final_stage()
    gs_.close()
    return nc, M


def host_consts(cfg):
    S = cfg.S
    cmat = np.zeros((128, 3, 128), np.float32)
    i = np.arange(128)
    cmat[:, 0, :] = (i[:, None] == i[None, :]); cmat[:, 1, :] = (i[:, None] <= i[None, :]); cmat[:, 2, :] = (i[:, None] >= i[None, :])
    t = np.arange(S); row = (t // 64).astype(np.float32); col = (t % 64).astype(np.float32)
    freqs = (10000.0 ** (-np.arange(0, 32, 2, dtype=np.float32) / 32)).astype(np.float32)
    ang = np.concatenate([row[:, None] * freqs, col[:, None] * freqs], -1).astype(np.float32)
    rope = np.zeros((2, 128, S), np.float32)
    rope[0] = np.tile(np.cos(ang).T, (4, 1)); rope[1] = np.tile(np.sin(ang).T, (4, 1))
    ind = np.zeros((32, 64, 64), np.float32)
    cp = np.arange(64)[:, None]; c = np.arange(64)[None, :]
    for d in range(31):
        ind[d] = (cp - c + 15 == d)
    qstart = np.clip(c - 8, 0, 48)
    ind[31] = np.where((cp >= qstart) & (cp < qstart + 16), 0.0, NEG)
    return cmat, rope, ind.reshape(32, 4096)


def make_in_maps(cfg, inp):
    cmat, rope, ind = host_consts(cfg)
    DEPTH = cfg.DEPTH
    f = lambda a: np.ascontiguousarray(np.asarray(a, np.float32))
    pv = np.zeros((DEPTH, 128, 136), np.float32)
    for l in range(DEPTH):
        pv[l, :, 0:24] = inp["norm_g"][l].reshape(24, 128).T
        pv[l, :, 24:96] = inp["b_ada"][l].reshape(72, 128).T
        pv[l, :, 96:120] = inp["ml_conv_w"][l].reshape(24, 128).T
        pv[l, :, 120:128] = inp["ml_conv_b"][l].reshape(8, 128).T
        pv[l, :, 128:132] = inp["ml_norm_g"][l].reshape(4, 128).T
        pv[l, :, 132:136] = inp["df_norm_g"][l].reshape(4, 128).T
    gb = np.broadcast_to(np.asarray(inp["ml_gate_b"], np.float32)[:, None, :], (DEPTH, 128, 16))
    rbT = np.ones((DEPTH, 32, 120), np.float32)
    rb = np.asarray(inp["na_rel_bias"], np.float32)
    rbT[:, 0:31, :] = rb[:, :, ::-1, :].reshape(DEPTH, 120, 31).transpose(0, 2, 1)
    shared = {"w_ada": f(inp["w_ada"]), "pv": pv, "gb": f(gb), "ffn_w1": f(inp["ffn_w1"]), "ffn_w2": f(inp["ffn_w2"]),
              "w_in": f(inp["w_in"]), "dfl": f(np.asarray(inp["df_lambda"]).reshape(DEPTH, 1, 256)), "rbT": f(rbT),
              "w_branch": f(inp["w_branch"]), "w_out": f(inp["w_out"]), "cmat": cmat, "rope": rope, "ind": ind}
    maps = []
    for b in range(cfg.NC):
        cv = np.zeros((128, 24), np.float32)
        cv[:, 0:8] = np.asarray(inp["c"][b]).reshape(8, 128).T
        cv[:, 8:16] = np.asarray(inp["c_ctx"]).reshape(8, 128).T
        cv[:, 16:24] = np.asarray(inp["final_g"]).reshape(8, 128).T
        m = dict(shared); m["x"] = f(inp["x"][b]); m["ctx"] = f(inp["ctx"][b]); m["cv"] = cv
        maps.append(m)
    return maps


def kernel(**inputs):
    cfg = Cfg()
    nc, M = build(cfg)
    maps = make_in_maps(cfg, inputs)
    res = run_bass_kernel_spmd(nc, maps, core_ids=list(range(cfg.NC)))
    return np.stack([np.asarray(r["y"], np.float32) for r in res.results], 0)
```
